# Optimizing a Trainium2 kernel written in Bass

```python
import math
import jax, jax.numpy as jnp
from jax import lax
import numpy as np

D_MODEL = 1024
BATCH = 8
SEQ = 2048
DEPTH = 4
DEC_BATCH = 128
DEC_SEQ = 4
PAST_LEN = 16384
PAGE_SIZE = 128

D_A = D_MODEL // 2
K_A = 3
D_B = D_MODEL // 2
K_B = 31
H_C = 4
DH_C = D_MODEL // H_C
D_C = H_C * DH_C
MLSTM_CHUNK = 128
N_MEM = 256
HX = 4
DX = D_MODEL // HX
D_FF = -(-8 * D_MODEL // (3 * 256)) * 256
EPS = 1e-6
IN_SPLITS = (D_A, D_A, D_A, D_B, D_B, D_C, D_C, D_C, D_C, H_C, H_C, D_MODEL, D_MODEL, D_MODEL)
IN_WIDTH = 3 * D_A + 2 * D_B + 4 * D_C + 2 * H_C + 3 * D_MODEL

kernel_name = 'hybrid_conv_mlstm_memxattn_step'


def _split(z, sizes):
    idx, acc = [], 0
    for s in sizes[:-1]:
        acc += s
        idx.append(acc)
    return jnp.split(z, idx, axis=-1)


def rmsnorm(x, g):
    xf = x.astype(jnp.float32)
    y = xf * lax.rsqrt(jnp.mean(xf * xf, axis=-1, keepdims=True) + EPS)
    return (y * g.astype(jnp.float32)).astype(x.dtype)


def layernorm(x, g, b):
    xf = x.astype(jnp.float32)
    mu = jnp.mean(xf, axis=-1, keepdims=True)
    xc = xf - mu
    y = xc * lax.rsqrt(jnp.mean(xc * xc, axis=-1, keepdims=True) + EPS)
    return (y * g.astype(jnp.float32) + b.astype(jnp.float32)).astype(x.dtype)


def causal_dwconv(x, prev, w):
    K, C = w.shape
    xp = jnp.concatenate([prev.astype(x.dtype), x], axis=1)
    y = lax.conv_general_dilated(xp, w[:, None, :].astype(x.dtype), window_strides=(1,), padding='VALID',
                                 dimension_numbers=('NWC', 'WIO', 'NWC'), feature_group_count=C)
    return y, xp[:, xp.shape[1] - (K - 1):]


def mlstm_chunkwise(q, k, v, ig, fg, c0, n0, m0):
    B, T, H, DH = q.shape
    L = math.gcd(T, MLSTM_CHUNK)
    NC = T // L
    logf = jax.nn.log_sigmoid(fg)

    def to_chunks(a):
        return jnp.moveaxis(a.reshape((B, NC, L) + a.shape[2:]), 1, 0)

    qc, kc, vc, ic, lfc = (to_chunks(a) for a in (q, k, v, ig, logf))
    causal = jnp.tril(jnp.ones((L, L), dtype=bool))

    def step(carry, inp):
        c, n, m = carry
        qq, kk, vv, ii, lf = inp
        bt = jnp.moveaxis(jnp.cumsum(lf, axis=1), 1, 2)
        it = jnp.moveaxis(ii, 1, 2)
        dlog = bt[:, :, :, None] - bt[:, :, None, :] + it[:, :, None, :]
        dlog = jnp.where(causal, dlog, -jnp.inf)
        inter = bt + m[:, :, None]
        m_t = jnp.maximum(inter, jnp.max(dlog, axis=-1))
        w_intra = jnp.exp(dlog - m_t[..., None])
        w_inter = jnp.moveaxis(jnp.exp(inter - m_t), 1, 2)
        s = jnp.einsum('bthd,bshd->bhts', qq, kk) * w_intra
        num = jnp.einsum('bhts,bshd->bthd', s, vv) + jnp.einsum('bhed,bthd->bthe', c, qq) * w_inter[..., None]
        den = jnp.moveaxis(jnp.sum(s, axis=-1), 1, 2) + w_inter * jnp.einsum('bhd,bthd->bth', n, qq)
        floor = jnp.exp(-jnp.moveaxis(m_t, 1, 2))
        h = num / jnp.maximum(jnp.abs(den), floor)[..., None]
        m_new = m_t[:, :, -1]
        w_last = jnp.exp(dlog[:, :, -1, :] - m_new[..., None])
        w_prev = jnp.exp(inter[:, :, -1] - m_new)
        c_new = w_prev[..., None, None] * c + jnp.einsum('bhs,bshe,bshd->bhed', w_last, vv, kk)
        n_new = w_prev[..., None] * n + jnp.einsum('bhs,bshd->bhd', w_last, kk)
        return (c_new, n_new, m_new), h

    (c, n, m), hs = lax.scan(step, (c0, n0, m0), (qc, kc, vc, ic, lfc))
    h = jnp.moveaxis(hs, 0, 1).reshape(B, T, H, DH)
    return h, (c, n, m)


def mixer(h, conv_a_prev, conv_b_prev, c0, n0, m0, p):
    B, T, _ = h.shape
    dt = h.dtype
    f32 = jnp.float32
    z = h @ p['w_in']
    a_b, a_c, a_x, b_val, b_gt, q, k, v, o, ig, fg, g_a, g_b, g_c = _split(z, IN_SPLITS)
    a_conv, sa = causal_dwconv(a_c * a_x, conv_a_prev, p['conv_a_w'])
    y_a = (a_b * a_conv) @ p['w_out_a']
    b_conv, sb = causal_dwconv(b_val * jax.nn.sigmoid(b_gt), conv_b_prev, p['conv_b_w'])
    b_conv = b_conv + p['conv_b_b']
    y_b = jax.nn.silu(layernorm(b_conv, p['ln_b_g'], p['ln_b_b'])) @ p['w_out_b']
    gates = jnp.concatenate([ig, fg], axis=-1).astype(f32) + p['b_if'].astype(f32)
    qh = q.reshape(B, T, H_C, DH_C).astype(f32)
    kh = k.reshape(B, T, H_C, DH_C).astype(f32) * (DH_C ** -0.5)
    vh = v.reshape(B, T, H_C, DH_C).astype(f32)
    hc, (c, n, m) = mlstm_chunkwise(qh, kh, vh, gates[..., :H_C], gates[..., H_C:],
                                    c0.astype(f32), n0.astype(f32), m0.astype(f32))
    hc = hc * lax.rsqrt(jnp.mean(hc * hc, axis=-1, keepdims=True) + EPS)
    hc = hc.reshape(B, T, D_C) * p['mlstm_norm_g'].astype(f32)
    y_c = (jax.nn.sigmoid(o) * hc.astype(dt)) @ p['w_out_c']
    u = jax.nn.sigmoid(g_a) * y_a + jax.nn.sigmoid(g_b) * y_b + jax.nn.sigmoid(g_c) * y_c
    return u @ p['w_o'], (sa, sb, c.astype(dt), n.astype(dt), m.astype(dt))


def memory_kv(mem, g, w_kv):
    B, M, _ = mem.shape
    kv = rmsnorm(mem, g) @ w_kv
    k, v = jnp.split(kv, 2, axis=-1)
    return k.reshape(B, M, HX, DX), v.reshape(B, M, HX, DX)


def cross_attn(h, mk, mv, w_q, w_o):
    B, T, _ = h.shape
    q = (h @ w_q).reshape(B, T, HX, DX)
    s = jnp.einsum('bthd,bmhd->bhtm', q, mk.astype(h.dtype)).astype(jnp.float32) * (DX ** -0.5)
    pr = jax.nn.softmax(s, axis=-1).astype(h.dtype)
    out = jnp.einsum('bhtm,bmhd->bthd', pr, mv.astype(h.dtype)).reshape(B, T, HX * DX)
    return out @ w_o


def swiglu(h, w_in, w_out):
    gate, up = jnp.split(h @ w_in, 2, axis=-1)
    return (jax.nn.silu(gate) * up) @ w_out


def layer(x, mk, mv, conv_a_prev, conv_b_prev, c0, n0, m0, p):
    mix, st = mixer(rmsnorm(x, p['norm_mix_g']), conv_a_prev, conv_b_prev, c0, n0, m0, p)
    x = x + mix
    x = x + cross_attn(rmsnorm(x, p['norm_x_g']), mk, mv, p['w_xq'], p['w_xo'])
    x = x + swiglu(rmsnorm(x, p['norm_ffn_g']), p['w_ffn_in'], p['w_ffn_out'])
    return x, st


def setup_inputs(seed: int = 0) -> dict:
    key = jax.random.key(seed)
    ks = iter(jax.random.split(key, 48))

    def nrm(shape, scale):
        return jax.random.normal(next(ks), shape, jnp.float32) * scale

    def gain(shape):
        return 1.0 + nrm(shape, 0.01)

    b_if = jnp.concatenate([nrm((DEPTH, H_C), 0.1),
                            3.0 + 3.0 * jax.random.uniform(next(ks), (DEPTH, H_C), jnp.float32)], axis=-1)
    return {
        'x_prompt': nrm((BATCH, SEQ, D_MODEL), 1.0),
        'x_sample': nrm((DEC_BATCH, DEC_SEQ, D_MODEL), 1.0),
        'state_conv_a': nrm((DEPTH, DEC_BATCH, K_A - 1, D_A), 1.0),
        'state_conv_b': nrm((DEPTH, DEC_BATCH, K_B - 1, D_B), 0.5),
        'state_mlstm_c': nrm((DEPTH, DEC_BATCH, H_C, DH_C, DH_C), 0.3),
        'state_mlstm_n': nrm((DEPTH, DEC_BATCH, H_C, DH_C), 1.0),
        'state_mlstm_m': nrm((DEPTH, DEC_BATCH, H_C), 1.0),
        'cache_mem_k': nrm((DEPTH, DEC_BATCH, N_MEM, HX, DX), 1.0),
        'cache_mem_v': nrm((DEPTH, DEC_BATCH, N_MEM, HX, DX), 1.0),
        'mem_prompt': nrm((BATCH, N_MEM, D_MODEL), 1.0),
        'norm_mix_g': gain((DEPTH, D_MODEL)),
        'w_in': nrm((DEPTH, D_MODEL, IN_WIDTH), D_MODEL ** -0.5),
        'b_if': b_if,
        'conv_a_w': nrm((DEPTH, K_A, D_A), K_A ** -0.5),
        'w_out_a': nrm((DEPTH, D_A, D_MODEL), D_A ** -0.5),
        'conv_b_w': nrm((DEPTH, K_B, D_B), K_B ** -0.5),
        'conv_b_b': nrm((DEPTH, D_B), 0.01),
        'ln_b_g': gain((DEPTH, D_B)),
        'ln_b_b': nrm((DEPTH, D_B), 0.01),
        'w_out_b': nrm((DEPTH, D_B, D_MODEL), D_B ** -0.5),
        'mlstm_norm_g': gain((DEPTH, D_C)),
        'w_out_c': nrm((DEPTH, D_C, D_MODEL), D_C ** -0.5),
        'w_o': nrm((DEPTH, D_MODEL, D_MODEL), D_MODEL ** -0.5),
        'norm_x_g': gain((DEPTH, D_MODEL)),
        'norm_mem_g': gain((DEPTH, D_MODEL)),
        'w_xq': nrm((DEPTH, D_MODEL, HX * DX), D_MODEL ** -0.5),
        'w_xkv': nrm((DEPTH, D_MODEL, 2 * HX * DX), D_MODEL ** -0.5),
        'w_xo': nrm((DEPTH, HX * DX, D_MODEL), (HX * DX) ** -0.5),
        'norm_ffn_g': gain((DEPTH, D_MODEL)),
        'w_ffn_in': nrm((DEPTH, D_MODEL, 2 * D_FF), D_MODEL ** -0.5),
        'w_ffn_out': nrm((DEPTH, D_FF, D_MODEL), D_FF ** -0.5),
        'final_norm_g': gain((D_MODEL,)),
    }


def reference(x_prompt, x_sample, state_conv_a, state_conv_b, state_mlstm_c, state_mlstm_n, state_mlstm_m,
              cache_mem_k, cache_mem_v, mem_prompt, norm_mix_g, w_in, b_if, conv_a_w, w_out_a, conv_b_w,
              conv_b_b, ln_b_g, ln_b_b, w_out_b, mlstm_norm_g, w_out_c, w_o, norm_x_g, norm_mem_g, w_xq,
              w_xkv, w_xo, norm_ffn_g, w_ffn_in, w_ffn_out, final_norm_g):
    bp = x_prompt.shape[0]
    dt = x_prompt.dtype
    za = jnp.zeros((bp, K_A - 1, D_A), dt)
    zb = jnp.zeros((bp, K_B - 1, D_B), dt)
    zc = jnp.zeros((bp, H_C, DH_C, DH_C), jnp.float32)
    zn = jnp.zeros((bp, H_C, DH_C), jnp.float32)
    zm = jnp.zeros((bp, H_C), jnp.float32)
    xp, xs = x_prompt, x_sample
    pa, pb, pc, pn, pm, pk, pv = [], [], [], [], [], [], []
    sa, sb, sc, sn, sm = [], [], [], [], []
    for l in range(DEPTH):
        p = {'norm_mix_g': norm_mix_g[l], 'w_in': w_in[l], 'b_if': b_if[l], 'conv_a_w': conv_a_w[l],
             'w_out_a': w_out_a[l], 'conv_b_w': conv_b_w[l], 'conv_b_b': conv_b_b[l], 'ln_b_g': ln_b_g[l],
             'ln_b_b': ln_b_b[l], 'w_out_b': w_out_b[l], 'mlstm_norm_g': mlstm_norm_g[l], 'w_out_c': w_out_c[l],
             'w_o': w_o[l], 'norm_x_g': norm_x_g[l], 'w_xq': w_xq[l], 'w_xo': w_xo[l],
             'norm_ffn_g': norm_ffn_g[l], 'w_ffn_in': w_ffn_in[l], 'w_ffn_out': w_ffn_out[l]}
        mk, mv = memory_kv(mem_prompt, norm_mem_g[l], w_xkv[l])
        xp, (a1, b1, c1, n1, m1) = layer(xp, mk, mv, za, zb, zc, zn, zm, p)
        pa.append(a1); pb.append(b1); pc.append(c1); pn.append(n1); pm.append(m1)
        pk.append(mk); pv.append(mv)
        xs, (a2, b2, c2, n2, m2) = layer(xs, cache_mem_k[l], cache_mem_v[l], state_conv_a[l], state_conv_b[l],
                                        state_mlstm_c[l], state_mlstm_n[l], state_mlstm_m[l], p)
        sa.append(a2); sb.append(b2); sc.append(c2); sn.append(n2); sm.append(m2)
    y_prompt = rmsnorm(xp, final_norm_g)
    y_sample = rmsnorm(xs, final_norm_g)
    return (y_prompt, y_sample,
            jnp.stack(pa), jnp.stack(pb), jnp.stack(pc), jnp.stack(pn), jnp.stack(pm),
            jnp.stack(pk), jnp.stack(pv),
            jnp.stack(sa), jnp.stack(sb), jnp.stack(sc), jnp.stack(sn), jnp.stack(sm))
```

```python
import os as _os
import numpy as np
import concourse.bass as bass
import concourse.mybir as mybir
from concourse.bass_utils import run_bass_kernel_spmd

F32 = mybir.dt.float32
BF16 = mybir.dt.bfloat16
AF = mybir.ActivationFunctionType
ALU = mybir.AluOpType
AX = mybir.AxisListType

L = 4
D = 1024
T = 2112
PT = 2048
ST = 64
NB = 16
TT = [(0, 512), (512, 512), (1024, 512), (1536, 512), (2048, 64)]
INW = 9736
DFF = 2816
EPS = 1e-6
C_AB, C_AC, C_AX, C_BV, C_BG = 0, 512, 1024, 1536, 2048
C_Q, C_K, C_V, C_O, C_IF = 2560, 3584, 4608, 5632, 6656
C_GA, C_GB, C_GC = 6664, 7688, 8712
PC_NMIX, PC_NX, PC_NFFN, PC_NMEM, PC_FIN = 0, 8, 16, 24, 32
PC_CAW, PC_CBW, PC_CBB, PC_LNG, PC_LNB = 40, 52, 176, 180, 184
NPC = 188


_CTKEYS = {"ctmp", "memtS", "memn", "mrowst", "xin", "vab", "abuf", "ucv", "dg", "scbS", "bconv", "qTs", "kTs", "ktok",
           "vtok", "gob", "vct", "qsTm", "vcTs", "aoT", "kvst", "kvbf", "pTm", "KTp", "Vp", "memTl", "pexp", "pT", "cns", "ctsf"}


class Op:
    __slots__ = ("eng", "fn", "deps", "dma", "dval", "signal", "sigval", "waits", "epoch", "idx")


class Prog:
    def __init__(self):
        self.ops = []
        self.lastw = {}
        self.rd_eng = {}
        self.rd_dma = {}
        self.dma_cnt = {}
        self.epoch = 0

    def add(self, eng, fn, r=(), w=(), dma=None, fence=False):
        if not fence:
            def _isct(k):
                n = k[0] if isinstance(k, tuple) else k
                return n in _CTKEYS
            if any(_isct(k) for k in list(r) + list(w)):
                r = [k for k in r if k != "ctmp"] + ["ctmp"]
                w = [k for k in w if k != "ctmp"]
        op = Op()
        op.eng, op.fn, op.dma, op.epoch = eng, fn, dma, self.epoch
        op.idx = len(self.ops)
        op.signal = False
        op.sigval = 0
        op.dval = 0
        deps = set()
        for k in r:
            if k in self.lastw:
                deps.add(self.lastw[k])
        for k in w:
            if k in self.lastw:
                deps.add(self.lastw[k])
            for e, i in self.rd_eng.get(k, {}).items():
                deps.add(i)
            for i in self.rd_dma.get(k, ()):
                deps.add(i)
        for k in r:
            if dma is not None:
                self.rd_dma.setdefault(k, []).append(op.idx)
            else:
                self.rd_eng.setdefault(k, {})[eng] = op.idx
        for k in w:
            self.lastw[k] = op.idx
            self.rd_eng[k] = {}
            self.rd_dma[k] = []
        if dma is not None:
            c = self.dma_cnt.get(dma, 0) + 16
            self.dma_cnt[dma] = c
            op.dval = c
        deps.discard(op.idx)
        op.deps = deps
        self.ops.append(op)
        return op

    def finalize(self):
        ops = self.ops
        for op in ops:
            for d in op.deps:
                y = ops[d]
                if y.dma is None and not (y.eng == op.eng and op.dma is None and op.eng == "pe"):
                    y.signal = True
        cnt = {}
        for op in ops:
            if op.signal:
                k = (op.eng, op.epoch)
                cnt[k] = cnt.get(k, 0) + 1
                op.sigval = cnt[k]
        waited = {}
        for op in ops:
            need = {}
            for d in op.deps:
                y = ops[d]
                if y.dma is not None:
                    s, v = ("d", y.dma), y.dval
                elif y.eng == op.eng and op.dma is None and op.eng == "pe":
                    continue
                else:
                    s, v = ("c", y.eng, y.epoch), y.sigval
                if need.get(s, 0) < v:
                    need[s] = v
            ws = []
            wd = waited.setdefault(op.eng, {})
            for s, v in need.items():
                if wd.get(s, 0) < v:
                    wd[s] = v
                    ws.append((s, v))
            op.waits = ws
        return cnt


def _rs(a, pat, **kw):
    return a.rearrange(pat, **kw)


class _Stop(Exception):
    pass


def build_nc(stop=10 ** 9):
    def chk(i):
        if i >= stop:
            raise _Stop()
    nc = bass.Bass("TRN2", target_bir_lowering=False)
    P = Prog()

    def din(name, shape):
        return nc.dram_tensor(name, list(shape), F32, kind="ExternalInput").ap()

    def dout(name, shape):
        return nc.dram_tensor(name, list(shape), F32, kind="ExternalOutput").ap()

    xT_in = din("xT_in", [128, 8, T])
    memtok = din("memtok", [128, 2, D])
    pcol_d = din("pcol", [128, L * NPC])
    gnorm_d = din("gnorm", [L, 128, D])
    bif_d = din("bif", [4, L * 2])
    sca_d = din("sca", [128, L * 4 * NB * 2])
    scb_d = din("scb", [128, L * 4 * NB * 30])
    cnat_d = din("cnat", [L, NB, 4, 256, 256])
    ctr_d = din("ctr", [L, NB, 4, 256, 256])
    nT_d = din("nTin", [128, L * 2 * 64])
    m_d = din("min", [4, L * NB])
    kT_d = din("kTc", [L, NB, 4, 256, 256])
    v_d = din("vc", [L, NB, 256, D])
    cst_d = din("cst", [128, 1540])
    mrow_d = din("mrow", [128, 1024])
    w_in = din("w_in", [L, D, INW])
    w_oa = din("w_out_a", [L, 512, D])
    w_ob = din("w_out_b", [L, 512, D])
    w_oc = din("w_out_c", [L, D, D])
    w_o = din("w_o", [L, D, D])
    w_xq = din("w_xq", [L, D, D])
    w_xkv = din("w_xkv", [L, D, 2 * D])
    w_xo = din("w_xo", [L, D, D])
    w_fi = din("w_ffn_in", [L, D, 2 * DFF])
    w_fo = din("w_ffn_out", [L, DFF, D])

    yT = dout("yT", [128, 8, T])
    oca = dout("oca", [128, L * 4, 17 * 2])
    ocb = dout("ocb", [128, L * 4, 17 * 30])
    oC = dout("oC", [L, 17, 4, 256, 256])
    on = dout("on", [128, L, 2 * 68])
    om = dout("om", [4, L * 17])
    omk = dout("omk", [L, 256, D])
    omv = dout("omv", [L, 256, D])
    xsA = nc.dram_tensor("xsA", [128, 8, T], F32).ap()
    xsB = nc.dram_tensor("xsB", [128, 8, T], F32).ap()

    def sb(name, shape, dt=F32):
        return nc.alloc_sbuf_tensor(name, list(shape), dt) if False else nc.sbuf_tensor(name, list(shape), dt).__enter__()

    hT = sb("hT", [128, 8, T], BF16)
    big2 = sb("big2", [128, 8, T], BF16)
    ctmp = sb("ctmp", [128, 16384], F32)
    wst = sb("wst", [128, 2, 1024], F32)
    wbf = sb("wbf", [128, 2, 1024], BF16)
    cst = sb("cstsb", [128, 1540], F32)
    pcol = sb("pcolsb", [128, L * NPC], F32)
    gnb = sb("gnb", [128, D], F32)
    sqb = sb("sqb", [128, 2, 512], BF16)
    t512 = sb("t512", [128, 4, 512], F32)
    lnbuf = sb("lnbuf", [128, 2, 512], F32)
    rows = sb("rows", [4, 8, 128], F32)
    bif = sb("bifsb", [4, L * 2], F32)
    gcol = sb("gcol", [128, 16], F32)
    smalls = sb("smalls", [128, 32], F32)
    wprb = sb("wprb", [128, 64], F32)
    wTt = sb("wTt", [128, 128], F32)
    smT = sb("smT", [128, 128], BF16)
    qsT = sb("qsT", [128, 2, 128], BF16)
    cnp = sb("cnp", [128, 4, 512], F32)
    ctp = sb("ctp", [128, 4, 2 * 258], BF16)
    cts = sb("cts", [128, 2, 2 * 258], BF16)
    nT = sb("nT", [128, 2, 68], F32)
    msb = sb("msb", [4, 64], F32)
    vwb = sb("vwb", [128, 2, 256], BF16)
    wlm = sb("wlm", [128, 16], F32)
    wlmb = sb("wlmb", [128, 16], BF16)
    ostA = sb("ostA", [128, 34], F32)
    ostB = sb("ostB", [128, 17 * 30], F32)
    scaS = sb("scaS", [128, 4 * NB * 2], F32)
    oms = sb("oms", [128, 2, 256], F32)
    identb = sb("identb", [128, 128], BF16)
    onesb = sb("onesb", [128, 128], BF16)
    mrowb = sb("mrowb", [128, NB * 64], BF16)

    ident = cst[:, 0:128]
    onesf = cst[:, 128:256]
    maskP = cst[:, 256:384]
    maskS = cst[0:64, 384:448]
    maskcol = cst[0:64, 448:464]
    I4 = cst[0:4, 464:468]
    selneg = cst[0:4, 512:1024]
    selpos = cst[0:4, 1024:1536]

    def cv(off, nbytes, dt):
        a = ctmp[:, off // 4:(off + nbytes) // 4]
        return a.bitcast(dt) if dt != F32 else a

    xin = _rs(cv(0, 16384, F32), "p (c t) -> p c t", c=8)
    mrow_st = cv(16384, 4096, F32)
    cns = _rs(cv(55360, 4096, F32), "p (s n) -> p s n", s=2)
    cts_f = _rs(cv(59456, 4096, F32), "p (s n) -> p s n", s=2)
    vab = _rs(cv(0, 16896, BF16), "p (c t) -> p c t", c=4)
    abuf = cv(16896, 4224, BF16)
    ucv = [cv(21120, 5248, BF16), cv(26368, 5248, BF16)]
    dg = _rs(cv(31616, 7936, BF16), "p (k n) -> p k n", k=31)
    scbS = cv(39552, 7680, F32)
    bconv = _rs(cv(47232, 16896, BF16), "p (c t) -> p c t", c=4)
    qTs = _rs(cv(0, 8192, BF16), "p (c t) -> p c t", c=8)
    kTs = _rs(cv(8192, 8192, BF16), "p (c t) -> p c t", c=8)
    ktok = _rs(cv(16384, 8192, BF16), "p (c n) -> p c n", c=4)
    vtok = _rs(cv(24576, 8256, BF16), "p (c h n) -> p c h n", c=4, h=4)
    gob = _rs(cv(32832, 8192, BF16), "p (c n) -> p c n", c=4)
    vct = cv(41024, 2048, BF16)
    qsTm = _rs(cv(43072, 4096, BF16), "p (c b t) -> p c b t", c=2, b=NB)
    vcTs = _rs(cv(47168, 8192, BF16), "p (c t) -> p c t", c=8)
    aoT = _rs(cv(0, 33792, BF16), "p (c t) -> p c t", c=8)
    kvst = cv(33792, 8192, F32)
    kvbf = cv(41984, 4096, BF16)
    pTm = _rs(cv(46080, 4096, BF16), "p (c b t) -> p c b t", c=2, b=NB)
    KTp = _rs(cv(50176, 4096, BF16), "p (c m) -> p c m", c=8)
    Vp = _rs(cv(54272, 4096, BF16), "p (c n) -> p c n", c=2)
    memTl = _rs(cv(58368, 4096, BF16), "p (c m) -> p c m", c=8)
    pexp = cv(62464, 2048, BF16)
    pT = cv(64512, 1024, BF16)
    memtS = _rs(cv(0, 8192, F32), "p (c n) -> p c n", c=2)
    memn = _rs(cv(8192, 8192, F32), "p (c n) -> p c n", c=2)
    memTb = sb("memTb", [128, 8, 256], BF16)
    aot = sb("aot", [128, 1024], BF16)

    psb = [nc.psum_tensor("ps%d" % i, [128, 512], F32).__enter__() for i in range(8)]
    ps_ctr = [0]

    def PS():
        i = ps_ctr[0] % 6
        ps_ctr[0] += 1
        return psb[i], ("ps", i)

    psl_ctr = [0]

    def PSL():
        i = 6 + psl_ctr[0] % 2
        psl_ctr[0] += 1
        return psb[i], ("ps", i)

    t5_ctr = [0]

    def T5():
        i = t5_ctr[0] % 4
        t5_ctr[0] += 1
        return t512[:, i, :], ("t5", i)

    CT = "ctmp"

    def fence():
        P.add("pool", lambda e: e.memset(smalls[:, 31:32], 0.0), r=[], w=[CT, "fencebyte"], fence=True)

    w_ctr = [0]

    def wtile(src2d, r0, nkc, c0, ncols):
        s = w_ctr[0] % 2
        w_ctr[0] += 1
        n = nkc * ncols
        src = _rs(src2d[r0:r0 + nkc * 128, c0:c0 + ncols], "(kc p) n -> p kc n", p=128)
        dst = _rs(wst[:, s, 0:n], "p (kc n) -> p kc n", kc=nkc)
        P.add("sp", lambda e: e.dma_start(out=dst, in_=src), r=[], w=[("wst", s)], dma="ws%d_%d" % (s, P.epoch))
        P.add("pool", lambda e: e.tensor_copy(out=wbf[:, s, 0:n], in_=wst[:, s, 0:n]), r=[("wst", s)], w=[("wbf", s)])
        return _rs(wbf[:, s, 0:n], "p (kc n) -> p kc n", kc=nkc), ("wbf", s)

    def mm(out, lhsT, rhs, start, stop, r, w):
        P.add("pe", lambda e: e.matmul(out, lhsT=lhsT, rhs=rhs, start=start, stop=stop), r=r, w=w)

    def proj(wt, wk, col0, src, srck, nkc, tiles, consume):
        for ti, (t0, n) in enumerate(tiles):
            ps, pk = PS()
            for kc in range(nkc):
                mm(ps[:, 0:n], wt[:, kc, col0:col0 + 128], src(kc, t0, n), kc == 0, kc == nkc - 1,
                   [wk] + srck, [pk])
            consume(ti, t0, n, ps, pk)

    def act(out, in_, func, r, w, bias=0.0, scale=1.0, accum=None):
        if accum is None:
            P.add("act", lambda e: e.activation(out, in_, func, bias=bias, scale=scale), r=r, w=w)
        else:
            P.add("act", lambda e: e.activation(out, in_, func, bias=bias, scale=scale, accum_out=accum), r=r, w=w)

    def tt(eng, out, a, b, op, r, w):
        P.add(eng, lambda e: e.tensor_tensor(out, a, b, op), r=r, w=w)

    def stt(out, a, s, b, op0, op1, r, w):
        P.add("dve", lambda e: e.scalar_tensor_tensor(out, a, s, b, op0, op1), r=r, w=w)

    def ts(eng, out, a, s1, s2, op0, op1, r, w):
        if s2 is None:
            P.add(eng, lambda e: e.tensor_scalar(out, a, s1, None, op0), r=r, w=w)
        else:
            P.add(eng, lambda e: e.tensor_scalar(out, a, s1, s2, op0, op1), r=r, w=w)

    def cp(eng, out, a, r, w):
        if eng == "act":
            P.add("act", lambda e: e.activation(out, a, AF.Copy), r=r, w=w)
        else:
            P.add(eng, lambda e: e.tensor_copy(out=out, in_=a), r=r, w=w)

    def dma(out, in_, r, w, sem):
        P.add("sp", lambda e: e.dma_start(out=out, in_=in_), r=r, w=w, dma=sem)

    HK = [("hT", c) for c in range(8)]

    def rmsnorm(xsrc, xsk, gcolbase, dst_fn):
        for ti, (t0, n) in enumerate(TT):
            dma(xin[:, :, 0:n], xsrc[:, :, t0:t0 + n], [(xsk, ti)], ["xin", CT], "xin")
            ps, pk = PS()
            for c in range(8):
                act(sqb[:, c % 2, 0:n], xin[:, c, 0:n], AF.Square, ["xin"], [("sqb", c % 2)])
                mm(ps[:, 0:n], onesb[:, :], sqb[:, c % 2, 0:n], c == 0, c == 7, [("sqb", c % 2), "constsb_o"], [pk])
            rs, rk = T5()
            act(rs[:, 0:n], ps[:, 0:n], AF.Sqrt, [pk], [rk], bias=EPS, scale=1.0 / D)
            P.add("dve", lambda e, a=rs[:, 0:n]: e.reciprocal(a, a), r=[rk], w=[rk])
            for c in range(8):
                o, ok = dst_fn(c, t0, n)
                stt(o, xin[:, c, 0:n], pcol[:, gcolbase + c:gcolbase + c + 1], rs[:, 0:n], ALU.mult, ALU.mult,
                    ["xin", rk, "pcol"], ok)

    def hT_dst(c, t0, n):
        return hT[:, c, t0:t0 + n], [("hT", c)]

    def hsrc(kc, t0, n):
        return hT[:, kc, t0:t0 + n]

    dma(cst[:, :], cst_d, [], ["consts"], "init1")
    dma(pcol[:, :], pcol_d, [], ["pcol"], "init2")
    dma(bif[:, :], bif_d, [], ["bif"], "init3")
    dma(memtS, memtok, [], [CT, "memtS"], "init4")
    cp("dve", identb[:, :], ident, ["consts"], ["constsb_i"])
    cp("dve", onesb[:, :], onesf, ["consts"], ["constsb_o"])
    dma(mrow_st, mrow_d, [], [CT, "mrowst"], "init5")
    cp("dve", mrowb[:, :], mrow_st, ["mrowst", CT], ["mrowb"])
    P.add("pool", lambda e: e.memset(_rs(vtok, "p c h n -> p (c h) n")[:, :, 256:258], 1.0), r=[], w=[CT])
    for mc in range(2):
        jk, jkk = T5()
        act(jk[:, 0:512], memtS[:, mc, 0:512], AF.Square, [CT, "memtS"], [jkk], accum=smalls[:, mc * 2:mc * 2 + 1])
        jk2, jkk2 = T5()
        act(jk2[:, 0:512], memtS[:, mc, 512:1024], AF.Square, [CT, "memtS"], [jkk2, "sm0"], accum=smalls[:, mc * 2 + 1:mc * 2 + 2])
        tt("dve", smalls[:, 4 + mc:5 + mc], smalls[:, mc * 2:mc * 2 + 1], smalls[:, mc * 2 + 1:mc * 2 + 2], ALU.add,
           [jkk, jkk2, "sm0"], ["sm1"])
        act(smalls[:, 6 + mc:7 + mc], smalls[:, 4 + mc:5 + mc], AF.Sqrt, ["sm1"], ["sm2"], bias=EPS, scale=1.0 / D)
        P.add("dve", lambda e, a=smalls[:, 6 + mc:7 + mc]: e.reciprocal(a, a), r=["sm2"], w=["sm3"])
        ts("dve", memn[:, mc, :], memtS[:, mc, :], smalls[:, 6 + mc:7 + mc], None, ALU.mult, None, ["sm3", CT, "memtS"], [CT, "memn"])
        for g in range(2):
            ps, pk = PS()
            for q in range(4):
                c = g * 4 + q
                P.add("pe", lambda e, o=ps[:, q * 128:(q + 1) * 128], i=memn[:, mc, c * 128:(c + 1) * 128]:
                      e.transpose(o, i, ident), r=["memn", "consts"], w=[pk])
            cp("dve", memTb[:, g * 4:(g + 1) * 4, mc * 128:(mc + 1) * 128],
               _rs(ps[:, :], "p (q t) -> p q t", q=4), [pk], ["memTb"])
    P.add("pool", lambda e: e.memset(nT[:, :, 0:4], 0.0), r=[], w=["nT"])
    fence()

    xcur, xk = xT_in, "x0"
    xdsts = [(xsA, "xA"), (xsB, "xB")]
    xflip = [0]

    def residual(wsrc2d, nk, srcfn, srckeys):
        nonlocal xcur, xk
        xn, xnk = xdsts[xflip[0] % 2]
        xflip[0] += 1
        for j in range(8):
            wt, wk = wtile(wsrc2d, 0, nk, j * 128, 128)

            def consume(ti, t0, n, ps, pk, j=j):
                xo, xok = T5()
                dma(xo[:, 0:n], xcur[:, j, t0:t0 + n], [(xk, ti)], [xok], "xo%d" % (int(xok[1])))
                tt("dve", xo[:, 0:n], ps[:, 0:n], xo[:, 0:n], ALU.add, [pk, xok], [xok])
                dma(xn[:, j, t0:t0 + n], xo[:, 0:n], [xok], [(xnk, ti)], "xw%d" % (int(xok[1])))
            proj(wt, wk, 0, srcfn, srckeys, nk, TT, consume)
        xcur, xk = xn, xnk

    def layers():
      for l in range(L):
        layer(l)

    def layer(l):
        nonlocal xcur, xk
        P.epoch = l
        pc = l * NPC
        Win = w_in[l]
        chk(l * 10 + 1)
        rmsnorm(xcur, xk, pc + PC_NMIX, hT_dst)
        fence()
        dma(gnb[:, :], gnorm_d[l], [], ["gnb"], "ld0_1")
        dma(scaS[:, :], sca_d[:, l * 128:(l + 1) * 128], [], ["scaS"], "ld0_2")
        dma(scbS, scb_d[:, l * 1920:(l + 1) * 1920], [], [CT, "scbS"], "ld0_3")
        dma(nT[:, :, 4:68], _rs(nT_d[:, l * 128:(l + 1) * 128], "p (c n) -> p c n", c=2), [], ["nT"], "ld0_4")
        dma(msb[:, 0:16], m_d[:, l * NB:(l + 1) * NB], [], ["msb"], "ld0_5")
        wg, wgk0 = None, None

        def conv_stage(K, colbase, halo_src, j, ub, ubk, consume):
            wv = pcol[:, colbase + j * K:colbase + (j + 1) * K]
            P.add("pool", lambda e: e.tensor_tensor(dg[:, 0:K, :], ident.unsqueeze(1).to_broadcast([128, K, 128]),
                                                    wv.unsqueeze(2).to_broadcast([128, K, 128]), ALU.mult),
                  r=["pcol", "consts"], w=[CT, "dg"])
            for ti, (t0, n) in enumerate(TT):
                ps, pk = PS()
                for k in range(K):
                    off = 30 - (K - 1) + k
                    if ti < 4:
                        rhs = ub[:, t0 + off:t0 + off + n]
                    else:
                        rhs = _rs(ub[:, 2078:2078 + NB * 34], "p (b t) -> p b t", b=NB)[:, :, off:off + 4]
                    mm(ps[:, 0:n], dg[:, k, :], rhs, k == 0, k == K - 1, ["dg", ubk, CT], [pk])
                consume(ti, t0, n, ps, pk)

        def ustage(colP, colG, gfunc, j, ub, ubk, halo_view, Kh, tailout):
            wtG, wkG = wtile(Win, 0, 8, colG + j * 128, 128)
            wtP, wkP = wtile(Win, 0, 8, colP + j * 128, 128)
            ubs = _rs(ub[:, 2078:2078 + NB * 34], "p (b t) -> p b t", b=NB)
            cp("pool", ubs[:, :, 30 - Kh:30], halo_view, ["scaS", "scbS", CT], [ubk, CT])
            for ti, (t0, n) in enumerate(TT):
                psG, pkG = PS()
                for kc in range(8):
                    mm(psG[:, 0:n], wtG[:, kc, :], hsrc(kc, t0, n), kc == 0, kc == 7, [wkG] + HK, [pkG])
                tg, tgk = T5()
                act(tg[:, 0:n], psG[:, 0:n], gfunc, [pkG], [tgk])
                psP, pkP = PS()
                for kc in range(8):
                    mm(psP[:, 0:n], wtP[:, kc, :], hsrc(kc, t0, n), kc == 0, kc == 7, [wkP] + HK, [pkP])
                if ti < 4:
                    tt("dve", ub[:, 30 + t0:30 + t0 + n], psP[:, 0:n], tg[:, 0:n], ALU.mult, [pkP, tgk], [ubk, CT])
                    if ti == 3:
                        tt("dve", tailout[:, 0:Kh], psP[:, 512 - Kh:512], tg[:, 512 - Kh:512], ALU.mult,
                           [pkP, tgk], ["ost"])
                else:
                    tt("dve", ubs[:, :, 30:34], _rs(psP[:, 0:64], "p (b t) -> p b t", b=NB),
                       _rs(tg[:, 0:64], "p (b t) -> p b t", b=NB), ALU.mult, [pkP, tgk], [ubk, CT])
                    so = _rs(tailout[:, Kh:17 * Kh], "p (b t) -> p b t", b=NB)
                    if Kh >= 4:
                        tt("dve", so[:, :, Kh - 4:Kh], _rs(psP[:, 0:64], "p (b t) -> p b t", b=NB),
                           _rs(tg[:, 0:64], "p (b t) -> p b t", b=NB), ALU.mult, [pkP, tgk], ["ost"])
                    else:
                        tt("dve", so[:, :, 0:Kh], _rs(psP[:, 0:64], "p (b t) -> p b t", b=NB)[:, :, 4 - Kh:4],
                           _rs(tg[:, 0:64], "p (b t) -> p b t", b=NB)[:, :, 4 - Kh:4], ALU.mult, [pkP, tgk], ["ost"])

        chk(l * 10 + 2)
        P.add("pool", lambda e: e.memset(ucv[0][:, 0:30], 0.0), r=[], w=[CT, ("ucv", 0)])
        P.add("pool", lambda e: e.memset(ucv[1][:, 0:30], 0.0), r=[], w=[CT, ("ucv", 1)])
        for j in range(4):
            ub, ubk = ucv[j % 2], ("ucv", j % 2)
            hv = _rs(scaS[:, j * 32:(j + 1) * 32], "p (b t) -> p b t", b=NB)
            ustage(C_AX, C_AC, AF.Copy, j, ub, ubk, hv, 2, ostA)
            dma(oca[:, l * 4 + j, :], ostA[:, :], ["ost"], [], "oca")
            wtB, wkB = wtile(Win, 0, 8, C_AB + j * 128, 128)
            for ti, (t0, n) in enumerate(TT):
                psb_, pkb = PS()
                for kc in range(8):
                    mm(psb_[:, 0:n], wtB[:, kc, :], hsrc(kc, t0, n), kc == 0, kc == 7, [wkB] + HK, [pkb])
                cp("act", abuf[:, t0:t0 + n], psb_[:, 0:n], [pkb], [CT, "abuf"])

            def consA(ti, t0, n, ps, pk, j=j):
                tt("dve", vab[:, j, t0:t0 + n], ps[:, 0:n], abuf[:, t0:t0 + n], ALU.mult, [pk, "abuf"], [CT, ("vab", j)])
            conv_stage(3, pc + PC_CAW, None, j, ub, ubk, consA)

        def merge_stage(first, gcol0, wsrc2d, nk, srcfn, srckeys, tiles):
            for j in range(8):
                wtG, wkG = wtile(Win, 0, 8, gcol0 + j * 128, 128)
                wtY, wkY = wtile(wsrc2d, 0, nk, j * 128, 128)
                for ti, (t0, n) in enumerate(tiles):
                    psG, pkG = PS()
                    for kc in range(8):
                        mm(psG[:, 0:n], wtG[:, kc, :], hsrc(kc, t0, n), kc == 0, kc == 7, [wkG] + HK, [pkG])
                    sg, sgk = T5()
                    act(sg[:, 0:n], psG[:, 0:n], AF.Sigmoid, [pkG], [sgk])
                    psY, pkY = PS()
                    for kc in range(nk):
                        mm(psY[:, 0:n], wtY[:, kc, :], srcfn(kc, t0, n), kc == 0, kc == nk - 1, [wkY] + srckeys, [pkY])
                    if first:
                        tt("dve", big2[:, j, t0:t0 + n], psY[:, 0:n], sg[:, 0:n], ALU.mult, [pkY, sgk], [("u", j)])
                    else:
                        tt("dve", sg[:, 0:n], psY[:, 0:n], sg[:, 0:n], ALU.mult, [pkY, sgk], [sgk])
                        tt("pool", big2[:, j, t0:t0 + n], big2[:, j, t0:t0 + n], sg[:, 0:n], ALU.add, [sgk, ("u", j)], [("u", j)])

        chk(l * 10 + 3)
        merge_stage(True, C_GA, w_oa[l], 4, lambda kc, t0, n: vab[:, kc, t0:t0 + n], [CT] + [("vab", c) for c in range(4)], TT)

        chk(l * 10 + 4)
        for j in range(4):
            ub, ubk = ucv[j % 2], ("ucv", j % 2)
            hv = _rs(scbS[:, j * 480:(j + 1) * 480], "p (b t) -> p b t", b=NB)
            cp("pool", _rs(ostB[:, 30:510], "p (b t) -> p b t", b=NB)[:, :, 0:26], hv[:, :, 4:30], ["scbS", CT], ["ost"])
            ustage(C_BV, C_BG, AF.Sigmoid, j, ub, ubk, hv, 30, ostB)
            dma(ocb[:, l * 4 + j, :], ostB[:, :], ["ost"], [], "ocb")

            def consB(ti, t0, n, ps, pk, j=j):
                act(bconv[:, j, t0:t0 + n], ps[:, 0:n], AF.Identity, [pk, "pcol"], [CT, ("bconv", j)],
                    bias=pcol[:, pc + PC_CBB + j:pc + PC_CBB + j + 1])
            conv_stage(31, pc + PC_CBW, None, j, ub, ubk, consB)
        BK = [("bconv", c) for c in range(4)]
        for ti, (t0, n) in enumerate(TT):
            ps1, pk1 = PS()
            for c in range(4):
                mm(ps1[:, 0:n], onesb[:, :], bconv[:, c, t0:t0 + n], c == 0, c == 3, BK + [CT, "constsb_o"], [pk1])
            ps2, pk2 = PS()
            for c in range(4):
                act(sqb[:, c % 2, 0:n], bconv[:, c, t0:t0 + n], AF.Square, BK + [CT], [("sqb", c % 2)])
                mm(ps2[:, 0:n], onesb[:, :], sqb[:, c % 2, 0:n], c == 0, c == 3, [("sqb", c % 2)], [pk2])
            mu, muk = lnbuf[:, 0, :], "lnmu"
            act(mu[:, 0:n], ps1[:, 0:n], AF.Copy, [pk1], [muk], scale=1.0 / 512)
            rs, rk = lnbuf[:, 1, :], "lnrs"
            tt("dve", rs[:, 0:n], mu[:, 0:n], mu[:, 0:n], ALU.mult, [muk], [rk])
            stt(rs[:, 0:n], ps2[:, 0:n], 1.0 / 512, rs[:, 0:n], ALU.mult, ALU.subtract, [pk2, rk], [rk])
            act(rs[:, 0:n], rs[:, 0:n], AF.Sqrt, [rk], [rk], bias=EPS)
            P.add("dve", lambda e, a=rs[:, 0:n]: e.reciprocal(a, a), r=[rk], w=[rk])
            for c in range(4):
                xc, xck = T5()
                tt("dve", xc[:, 0:n], bconv[:, c, t0:t0 + n], mu[:, 0:n], ALU.subtract, BK + [CT, muk], [xck])
                stt(xc[:, 0:n], xc[:, 0:n], pcol[:, pc + PC_LNG + c:pc + PC_LNG + c + 1], rs[:, 0:n], ALU.mult, ALU.mult,
                    [xck, rk, "pcol"], [xck])
                act(vab[:, c, t0:t0 + n], xc[:, 0:n], AF.Silu, [xck, "pcol"], [CT, ("vab", c)],
                    bias=pcol[:, pc + PC_LNB + c:pc + PC_LNB + c + 1])
        chk(l * 10 + 5)
        merge_stage(False, C_GB, w_ob[l], 4, lambda kc, t0, n: vab[:, kc, t0:t0 + n], [CT] + [("vab", c) for c in range(4)], TT)
        fence()

        chk(l * 10 + 6)
        P.add("pool", lambda e: e.memset(_rs(vtok, "p c h n -> p (c h) n")[:, :, 256:258], 1.0), r=[], w=[CT, "vtok"])
        P.add("pool", lambda e: e.memset(cnp[:, :, :], 0.0), r=[], w=["cnp"])
        P.add("pool", lambda e: e.memset(ctp[:, :, :], 0.0), r=[], w=["ctp"])
        P.add("pool", lambda e: e.memset(nT[:, :, 0:4], 0.0), r=[], w=["nT"])
        P.add("pool", lambda e: e.memset(msb[:, 16:20], 0.0), r=[], w=["carry"])
        wgt, wgk = wtile(Win, 0, 8, C_IF, 8)
        wgs = smT
        wgb = sb("wgb%d" % l, [128, 8, 8], BF16)
        cp("pool", wgb[:, :, :], wgt, [wgk], [("wgb", l)])
        R = lambda i: rows[:, i, :]
        for sc in range(5):
            t0s, ns = TT[sc]
            samp = sc == 4
            Lc = 64 if samp else 128
            ntc = 1 if samp else 4
            nseq = NB if samp else 1
            tiles_sc = [(t0s, ns)]
            for c in range(8):
                wtq, wkq = wtile(Win, 0, 8, C_Q + c * 128, 128)

                def cq(ti, t0, n, ps, pk, c=c):
                    cp("act", qTs[:, c, 0:n], ps[:, 0:n], [pk], [CT, "qTs"])
                proj(wtq, wkq, 0, hsrc, HK, 8, tiles_sc, cq)
                wtk, wkk = wtile(Win, 0, 8, C_K + c * 128, 128)

                def ck(ti, t0, n, ps, pk, c=c):
                    act(kTs[:, c, 0:n], ps[:, 0:n], AF.Copy, [pk], [CT, "kTs"], scale=0.0625)
                proj(wtk, wkk, 0, hsrc, HK, 8, tiles_sc, ck)
                for kind, col in (("k", C_K), ("v", C_V), ("o", C_O)):
                    if kind == "k":
                        wtt, wkt = wtk, wkk
                    else:
                        wtt, wkt = wtile(Win, 0, 8, col + c * 128, 128)
                    for tc in range(ntc):
                        ps, pk = PS()
                        ta = t0s + tc * 128
                        for kc in range(8):
                            mm(ps[0:Lc, 0:128], hT[:, kc, ta:ta + Lc], wtt[:, kc, :], kc == 0, kc == 7, [wkt] + HK, [pk])
                        if kind == "k":
                            act(ktok[0:Lc, tc, c * 128:(c + 1) * 128], ps[0:Lc, 0:128], AF.Copy, [pk], [CT, "ktok"], scale=0.0625)
                        elif kind == "v":
                            cp("dve", vtok[0:Lc, tc, c // 2, (c % 2) * 128:(c % 2) * 128 + 128], ps[0:Lc, 0:128], [pk], [CT, "vtok"])
                        else:
                            so_, sok = T5()
                            act(so_[0:Lc, 0:128], ps[0:Lc, 0:128], AF.Sigmoid, [pk], [sok])
                            tt("pool", gob[0:Lc, tc, c * 128:(c + 1) * 128], so_[0:Lc, 0:128], gnb[0:Lc, c * 128:(c + 1) * 128],
                               ALU.mult, [sok, "gnb"], [CT, "gob"])
            for tc in range(ntc):
                ta = t0s + tc * 128
                lo = tc * 128
                for gi_, (ro, co, bo) in enumerate(((0, 0, 0), (1, 4, 1))):
                    ps, pk = PS()
                    for kc in range(8):
                        mm(ps[0:4, 0:Lc], wgb[:, kc, co:co + 4], hT[:, kc, ta:ta + Lc], kc == 0, kc == 7, [("wgb", l)] + HK, [pk])
                    act(R(ro)[:, 0:Lc], ps[0:4, 0:Lc], AF.Identity, [pk, "bif"], [("row", ro)], bias=bif[:, l * 2 + bo:l * 2 + bo + 1])
                ts("dve", R(7)[:, 0:Lc], R(1)[:, 0:Lc], -1.0, None, ALU.mult, None, [("row", 1)], [("row", 7)])
                tt("dve", R(7)[:, 0:Lc], R(7)[:, 0:Lc], R(1)[:, 0:Lc], ALU.max, [("row", 1), ("row", 7)], [("row", 7)])
                act(R(7)[:, 0:Lc], R(7)[:, 0:Lc], AF.Exp, [("row", 7)], [("row", 7)], scale=-1.0)
                act(R(7)[:, 0:Lc], R(7)[:, 0:Lc], AF.Ln, [("row", 7)], [("row", 7)], bias=1.0)
                stt(R(1)[:, 0:Lc], R(1)[:, 0:Lc], 0.0, R(7)[:, 0:Lc], ALU.min, ALU.subtract, [("row", 1), ("row", 7)], [("row", 1)])
                if not samp:
                    P.add("dve", lambda e: e.tensor_tensor_scan(R(2)[:, 0:128], cst[0:4, 1536:1537].to_broadcast([4, 128]), R(1)[:, 0:128],
                                                                msb[:, 16:17], ALU.mult, ALU.add), r=[("row", 1), "carry", "consts"], w=[("row", 2)])
                    tt("dve", R(0)[:, 0:128], R(0)[:, 0:128], R(2)[:, 0:128], ALU.subtract, [("row", 0), ("row", 2)], [("row", 0)])
                    P.add("dve", lambda e: e.tensor_tensor_scan(R(3)[:, 0:128], cst[0:4, 1537:1538].to_broadcast([4, 128]), R(0)[:, 0:128],
                                                                msb[:, 17:18], ALU.add, ALU.max), r=[("row", 0), "carry", "consts"], w=[("row", 3)])
                    Mend = R(3)[:, 127:128].to_broadcast([4, 128])
                    Mprev = msb[:, 17:18].to_broadcast([4, 128])
                    tt("dve", msb[:, 20:21], msb[:, 17:18], R(3)[:, 127:128], ALU.subtract, ["carry", ("row", 3)], ["wpr"])
                    npair = 1
                else:
                    v3 = lambda i: _rs(R(i)[:, 0:64], "p (b t) -> p b t", b=NB)
                    cp("dve", v3(2)[:, :, 0:1], v3(1)[:, :, 0:1], [("row", 1)], [("row", 2)])
                    for t_ in range(1, 4):
                        tt("dve", v3(2)[:, :, t_:t_ + 1], v3(2)[:, :, t_ - 1:t_], v3(1)[:, :, t_:t_ + 1], ALU.add, [("row", 1), ("row", 2)], [("row", 2)])
                    tt("dve", R(0)[:, 0:64], R(0)[:, 0:64], R(2)[:, 0:64], ALU.subtract, [("row", 0), ("row", 2)], [("row", 0)])
                    m0 = msb[:, 0:16].unsqueeze(2)
                    tt("dve", v3(3)[:, :, 0:1], v3(0)[:, :, 0:1], m0, ALU.max, [("row", 0), "msb"], [("row", 3)])
                    for t_ in range(1, 4):
                        tt("dve", v3(3)[:, :, t_:t_ + 1], v3(3)[:, :, t_ - 1:t_], v3(0)[:, :, t_:t_ + 1], ALU.max, [("row", 0), ("row", 3)], [("row", 3)])
                    Mend = v3(3)[:, :, 3:4].to_broadcast([4, NB, 4])
                    Mprev = m0.to_broadcast([4, NB, 4])
                    tt("dve", R(7)[:, 64:80].unsqueeze(2), m0, v3(3)[:, :, 3:4], ALU.subtract, ["msb", ("row", 3)], ["wpr"])
                    npair = NB
                Lv = (lambda i: R(i)[:, 0:Lc]) if not samp else (lambda i: _rs(R(i)[:, 0:64], "p (b t) -> p b t", b=NB))
                tt("dve", Lv(4), Lv(0), Mend, ALU.subtract, [("row", 0), ("row", 3)], [("row", 4)])
                act(R(4)[:, 0:Lc], R(4)[:, 0:Lc], AF.Exp, [("row", 4)], [("row", 4)])
                tt("dve", R(5)[:, 0:Lc], R(2)[:, 0:Lc], R(3)[:, 0:Lc], ALU.add, [("row", 2), ("row", 3)], [("row", 5)])
                if samp:
                    cp("dve", msb[:, 32:48].unsqueeze(2), _rs(R(5)[:, 0:64], "p (b t) -> p b t", b=NB)[:, :, 3:4], [("row", 5)], ["mout"])
                    dma(om[:, l * 17 + 1:l * 17 + 17], msb[:, 32:48], ["mout"], [], "om_s")
                elif sc == 3 and tc == 3:
                    cp("dve", msb[:, 24:25], R(5)[:, 127:128], [("row", 5)], ["moutp"])
                    dma(om[:, l * 17:l * 17 + 1], msb[:, 24:25], ["moutp"], [], "om_p")
                act(R(5)[:, 0:Lc], R(5)[:, 0:Lc], AF.Exp, [("row", 5)], [("row", 5)], scale=-1.0)
                tt("dve", Lv(6), Mprev, Lv(3), ALU.subtract, [("row", 3), "carry", "msb"], [("row", 6)])
                act(R(6)[:, 0:Lc], R(6)[:, 0:Lc], AF.Exp, [("row", 6)], [("row", 6)])
                if not samp:
                    act(msb[:, 20:21], msb[:, 20:21], AF.Exp, ["wpr"], ["wpr"])
                    ts("dve", R(7)[:, 96:100], I4, msb[:, 20:21], None, ALU.mult, None, ["wpr", "consts"], ["wprx"])
                    ps, pk = PS()
                    mm(ps[:, 0:4], onesf[0:4, :], R(7)[:, 96:100], True, True, ["wprx", "consts"], [pk])
                    cp("dve", wprb[:, 0:4], ps[:, 0:4], [pk], ["wprb"])
                    cp("dve", msb[:, 16:17], R(2)[:, 127:128], [("row", 2)], ["carry"])
                    cp("dve", msb[:, 17:18], R(3)[:, 127:128], [("row", 3), ("row", 6), "wpr"], ["carry"])
                else:
                    act(R(7)[:, 64:80], R(7)[:, 64:80], AF.Exp, ["wpr"], ["wpr"])
                    wx = _rs(wTt[0:4, 0:64], "p (b h) -> p b h", b=NB)
                    tt("dve", wx, R(7)[:, 64:80].unsqueeze(2).to_broadcast([4, NB, 4]), I4.unsqueeze(1).to_broadcast([4, NB, 4]),
                       ALU.mult, ["wpr", "consts"], ["wprx"])
                    ps, pk = PS()
                    mm(ps[:, 0:64], onesf[0:4, :], wTt[0:4, 0:64], True, True, ["wprx", "consts"], [pk])
                    cp("dve", wprb[:, 0:64], ps[:, 0:64], [pk], ["wprb"])
                ps, pk = PS()
                for q, ri in enumerate((0, 4, 5, 6)):
                    mm(ps[0:Lc, q * 4:q * 4 + 4], R(ri)[:, 0:Lc], I4, True, True, [("row", ri), "consts"], [pk])
                cp("dve", gcol[0:Lc, :], ps[0:Lc, 0:16], [pk], ["gcol"])
                if samp:
                    for h in range(4):
                        pass
                for h in range(4):
                    psq, pkq = PS()
                    for dc in range(2):
                        mm(psq[0:Lc, 0:Lc], kTs[:, h * 2 + dc, lo:lo + Lc], qTs[:, h * 2 + dc, lo:lo + Lc], dc == 0, dc == 1,
                           ["qTs", "kTs", CT], [pkq])
                    psm, pkm = PS()
                    mm(psm[0:Lc, 0:Lc], selneg[:, h * 128:h * 128 + Lc], R(3)[:, 0:Lc], True, False, [("row", 3), "consts"], [pkm])
                    mm(psm[0:Lc, 0:Lc], ident[0:Lc, 0:Lc], (maskS if samp else maskP), False, True, ["consts"], [pkm])
                    act(wTt[0:Lc, 0:Lc] if not samp else wTt[0:Lc, 64:128], psm[0:Lc, 0:Lc], AF.Exp, [pkm, "gcol"], ["wTt", "wprx"],
                        bias=gcol[0:Lc, h:h + 1])
                    wsrc_ = wTt[0:Lc, 0:Lc] if not samp else wTt[0:Lc, 64:128]
                    tt("dve", smT[0:Lc, 0:Lc], psq[0:Lc, 0:Lc], wsrc_, ALU.mult, [pkq, "wTt"], ["smT"])
                    psw, pkw = PS()
                    mm(psw[:, 0:Lc], selpos[:, h * 128:(h + 1) * 128], R(6)[:, 0:Lc], True, True, [("row", 6), "consts"], [pkw])
                    for dc in range(2):
                        tt("dve", qsT[:, dc, 0:Lc], qTs[:, h * 2 + dc, lo:lo + Lc], psw[:, 0:Lc], ALU.mult, [pkw, "qTs", CT], ["qsT"])
                    if samp:
                        for dc in range(2):
                            tt("pool", qsTm[:, dc, :, :], qsT[:, dc, 0:64].unsqueeze(1).to_broadcast([128, NB, 64]),
                               _rs(mrowb[:, :], "p (b t) -> p b t", b=NB), ALU.mult, ["qsT", "mrowb"], [CT, "qsTm"])
                        ts("dve", wlm[0:64, :], maskcol, gcol[0:64, 4 + h:5 + h], None, ALU.mult, None, ["gcol", "consts"], ["wlm"])
                        cp("dve", wlmb[0:64, :], wlm[0:64, :], ["wlm"], ["wlmb"])
                    else:
                        cp("dve", wlmb[0:128, 0:1], gcol[0:128, 4 + h:5 + h], ["gcol"], ["wlmb"])
                    pso, pko = PSL()
                    mm(pso[0:Lc, 0:257], smT[0:Lc, 0:Lc], vtok[0:Lc, tc, h, 0:257], True, False, ["smT", "vtok", CT], [pko])
                    for b in range(nseq):
                        if samp:
                            s2 = (b + h * NB) % 2
                            pidx = 4 + b * 4 + h
                            dma(_rs(cts_f[:, s2, :], "p (c e) -> p c e", c=2), _rs(ctr_d[l, b, h], "(c p) e -> p c e", p=128),
                                [], [("ctsf", s2), CT], "ctsf%d" % s2)
                            dma(_rs(cns[:, s2, :], "p (c e) -> p c e", c=2), _rs(cnat_d[l, b, h], "(c p) e -> p c e", p=128),
                                [], [("cns", s2), CT], "cns%d" % s2)
                            ctv = _rs(cts[:, s2, :], "p (c e) -> p c e", c=2)
                            cp("act", ctv[:, :, 0:256], _rs(cts_f[:, s2, :], "p (c e) -> p c e", c=2), [("ctsf", s2)], [("cts", s2)])
                            cp("pool", ctv[:, :, 256:257], nT[:, :, pidx:pidx + 1], ["nT"], [("cts", s2)])
                            ctk = ("cts", s2)
                            cnv, cnk = cns[:, s2, :], ("cns", s2)
                            lhs_q = lambda dc, b=b: qsTm[:, dc, b, :]
                            qk_ = ["qsTm", CT]
                            wpc = wprb[:, b * 4 + h:b * 4 + h + 1]
                            wl_col = wlm[0:64, b:b + 1]
                            wl_colb = wlmb[0:64, b:b + 1]
                        else:
                            pidx = h
                            ctv = _rs(ctp[:, h, :], "p (c e) -> p c e", c=2)
                            ctk = "ctp"
                            cnv, cnk = cnp[:, h, :], "cnp"
                            lhs_q = lambda dc: qsT[:, dc, 0:128]
                            qk_ = ["qsT"]
                            wpc = wprb[:, h:h + 1]
                            wl_col = gcol[0:128, 4 + h:5 + h]
                            wl_colb = wlmb[0:128, 0:1]
                        for dc in range(2):
                            mm(pso[0:Lc, 0:257], lhs_q(dc), ctv[:, dc, 0:257], False, (b == nseq - 1 and dc == 1), qk_ + [ctk], [pko])
                        s3 = (b + h) % 2
                        ts("dve", vwb[0:Lc, s3, :], vtok[0:Lc, tc, h, 0:256], wl_col, None, ALU.mult, None, ["vtok", CT, "gcol", "wlm"], [("vwb", s3)])
                        psu, pku = PS()
                        for ec in range(2):
                            mm(psu[:, ec * 256:(ec + 1) * 256], vwb[0:Lc, s3, ec * 128:(ec + 1) * 128], ktok[0:Lc, tc, h * 256:(h + 1) * 256],
                               True, True, [("vwb", s3), "ktok", CT], [pku])
                        stt(cnv, cnv, wpc, psu[:, :], ALU.mult, ALU.add, [cnk, "wprb", pku], [cnk])
                        psn, pkn = PS()
                        for dc in range(2):
                            mm(psn[:, dc:dc + 1], ktok[0:Lc, tc, h * 256 + dc * 128:h * 256 + dc * 128 + 128], wl_colb, True, True,
                               ["ktok", CT, "wlmb"], [pkn])
                        stt(nT[:, :, pidx], nT[:, :, pidx], wpc, psn[:, 0:2], ALU.mult, ALU.add, ["nT", "wprb", pkn, ctk], ["nT"])
                        if samp:
                            dma(_rs(oC[l, 1 + b, h], "(c p) d -> p c d", p=128), _rs(cnv, "p (c d) -> p c d", c=2), [cnk], [], "oc%d" % s2)
                        else:
                            for dcc in range(2):
                                pst, pkt = PS()
                                for ec in range(2):
                                    P.add("pe", lambda e, o=pst[:, ec * 128:(ec + 1) * 128], i=cnp[:, h, ec * 256 + dcc * 128:ec * 256 + dcc * 128 + 128]:
                                          e.transpose(o, i, ident), r=["cnp", "consts"], w=[pkt])
                                cp("act", ctv[:, dcc, 0:256], pst[:, 0:256], [pkt], ["ctp"])
                            cp("pool", ctv[:, :, 256:257], nT[:, :, h:h + 1], ["nT"], ["ctp"])
                            if sc == 3 and tc == 3:
                                dma(_rs(oC[l, 0, h], "(c p) d -> p c d", p=128), _rs(cnv, "p (c d) -> p c d", c=2), [cnk], [], "ocp")
                    sm = smalls
                    ts("dve", sm[0:Lc, 8:9], pso[0:Lc, 256:257], -1.0, None, ALU.mult, None, [pko], ["ep0"])
                    tt("dve", sm[0:Lc, 8:9], sm[0:Lc, 8:9], pso[0:Lc, 256:257], ALU.max, [pko, "ep0"], ["ep0"])
                    tt("dve", sm[0:Lc, 8:9], sm[0:Lc, 8:9], gcol[0:Lc, 8 + h:9 + h], ALU.max, ["gcol", "ep0"], ["ep0"])
                    P.add("dve", lambda e, a=sm[0:Lc, 8:9]: e.reciprocal(a, a), r=["ep0"], w=["ep0"])
                    jk, jkk = T5()
                    act(jk[0:Lc, 0:256], pso[0:Lc, 0:256], AF.Square, [pko, "ep0"], [jkk, "ep1"], scale=sm[0:Lc, 8:9], accum=sm[0:Lc, 9:10])
                    act(sm[0:Lc, 10:11], sm[0:Lc, 9:10], AF.Sqrt, ["ep1"], ["ep2"], bias=EPS, scale=1.0 / 256)
                    P.add("dve", lambda e, a=sm[0:Lc, 10:11]: e.reciprocal(a, a), r=["ep2"], w=["ep2"])
                    tt("dve", sm[0:Lc, 11:12], sm[0:Lc, 10:11], sm[0:Lc, 8:9], ALU.mult, ["ep2", "ep0"], ["ep3"])
                    stt(vct[0:Lc, h * 256:(h + 1) * 256], pso[0:Lc, 0:256], sm[0:Lc, 11:12], gob[0:Lc, tc, h * 256:(h + 1) * 256],
                        ALU.mult, ALU.mult, [pko, "ep3", "gob", CT], [CT, "vct"])
                pst, pkt = PS()
                pstb = pst[:, :].bitcast(BF16)
                for c in range(8):
                    P.add("pe", lambda e, o=pstb[:, c * 128:c * 128 + Lc], i=vct[0:Lc, c * 128:(c + 1) * 128], idn=identb[0:Lc, 0:Lc]:
                          e.transpose(o, i, idn), r=["vct", CT, "constsb_i"], w=[pkt])
                cp("act", vcTs[:, :, lo:lo + Lc], _rs(pstb, "p (c t) -> p c t", c=8)[:, :, 0:Lc], [pkt], [CT, "vcTs"])
            merge_stage(False, C_GC, w_oc[l], 8, lambda kc, t0, n, t0s=t0s: vcTs[:, kc, t0 - t0s:t0 - t0s + n], [CT, "vcTs"], tiles_sc)
        dma(on[:, l, :], _rs(nT[:, :, :], "p c n -> p (c n)"), ["nT"], [], "on")
        fence()
        chk(l * 10 + 7)
        residual(w_o[l], 8, lambda kc, t0, n: big2[:, kc, t0:t0 + n], [("u", c) for c in range(8)])

        chk(l * 10 + 8)
        rmsnorm(xcur, xk, pc + PC_NX, hT_dst)
        fence()
        chk(l * 10 + 8.1)
        for c in range(8):
            wt, wk = wtile(w_xq[l], 0, 8, c * 128, 128)

            def cxq(ti, t0, n, ps, pk, c=c):
                act(big2[:, c, t0:t0 + n], ps[:, 0:n], AF.Copy, [pk], [("u", c)], scale=0.0625)
            proj(wt, wk, 0, hsrc, HK, 8, TT, cxq)
        chk(l * 10 + 8.2)
        for c in range(8):
            ts("dve", memTl[:, c, :], memTb[:, c, :], pcol[:, pc + PC_NMEM + c:pc + PC_NMEM + c + 1], None, ALU.mult, None,
               ["memTb", "pcol"], [CT, "memTl"])
        chk(l * 10 + 8.25)
        for kv in range(1 if _os.environ.get("DBG_KV0") else 2):
            for c in range(int(_os.environ.get("DBG_NC", "8"))):
                wt, wk = wtile(w_xkv[l], 0, 8, kv * D + c * 128, 128)
                if kv == 0 and not _os.environ.get("DBG_X1"):
                    ps, pk = PS()
                    for kc in range(8):
                        mm(ps[:, 0:256], wt[:, kc, :], memTl[:, kc, :], kc == 0, kc == 7, [wk, "memTl", CT], [pk])
                    cp("act", KTp[:, c, :], ps[:, 0:256], [pk], [CT, "KTp"])
                for mc in range(0 if _os.environ.get("DBG_X2") else 2):
                    ps, pk = PS()
                    for kc in range(8):
                        mm(ps[:, 0:128], memTl[:, kc, mc * 128:(mc + 1) * 128], wt[:, kc, :], kc == 0, kc == 7, [wk, "memTl", CT], [pk])
                    s4 = (c * 2 + mc) % 2
                    cp("dve", oms[:, s4, 0:128], ps[:, 0:128], [pk], [("oms", s4)])
                    dst = (omk if kv == 0 else omv)[l, mc * 128:(mc + 1) * 128, c * 128:(c + 1) * 128]
                    if not _os.environ.get("DBG_NOOM"):
                        dma(dst, oms[:, s4, 0:128], [("oms", s4)], [], "oms%d" % s4)
                    if kv == 1:
                        cp("act", Vp[:, mc, c * 128:(c + 1) * 128], oms[:, s4, 0:128], [("oms", s4)], [CT, "Vp"])
        chk(l * 10 + 8.3)
        QK = [("u", c) for c in range(8)]
        for tcx in range(16):
            ta = tcx * 128
            for h in range(4):
                ps, pk = PS()
                for dc in range(2):
                    mm(ps[:, 0:256], big2[:, h * 2 + dc, ta:ta + 128], KTp[:, h * 2 + dc, :], dc == 0, dc == 1, QK + ["KTp", CT], [pk])
                P.add("dve", lambda e, o=smalls[:, 12:13], i=ps[:, 0:256]: e.tensor_reduce(o, i, AX.X, ALU.max), r=[pk], w=["xa0"])
                ts("dve", smalls[:, 13:14], smalls[:, 12:13], -1.0, None, ALU.mult, None, ["xa0"], ["xa1"])
                act(pexp[:, 0:256], ps[:, 0:256], AF.Exp, [pk, "xa1"], [CT, "pexp", "xa2"], bias=smalls[:, 13:14], accum=smalls[:, 14:15])
                pst, pkt = PS()
                pstb = pst[:, :].bitcast(BF16)
                for mc in range(2):
                    P.add("pe", lambda e, o=pstb[:, mc * 128:(mc + 1) * 128], i=pexp[:, mc * 128:(mc + 1) * 128]:
                          e.transpose(o, i, identb[:, :]), r=["pexp", CT, "constsb_i"], w=[pkt])
                cp("act", pT[:, 0:256], pstb[:, 0:256], [pkt], [CT, "pT"])
                pso, pko = PS()
                for mc in range(2):
                    mm(pso[:, 0:256], pT[:, mc * 128:(mc + 1) * 128], Vp[:, mc, h * 256:(h + 1) * 256], mc == 0, mc == 1, ["pT", "Vp", CT], [pko])
                P.add("dve", lambda e, a=smalls[:, 15:16], i=smalls[:, 14:15]: e.reciprocal(a, i), r=["xa2"], w=["xa3"])
                ts("dve", aot[:, h * 256:(h + 1) * 256], pso[:, 0:256], smalls[:, 15:16], None, ALU.mult, None, [pko, "xa3"], ["aot"])
            pst, pkt = PS()
            pstb = pst[:, :].bitcast(BF16)
            for c in range(8):
                P.add("pe", lambda e, o=pstb[:, c * 128:(c + 1) * 128], i=aot[:, c * 128:(c + 1) * 128]:
                      e.transpose(o, i, identb[:, :]), r=["aot", "constsb_i"], w=[pkt])
            cp("act", aoT[:, :, ta:ta + 128], _rs(pstb, "p (c t) -> p c t", c=8), [pkt], [CT, "aoT"])
        chk(l * 10 + 8.4)
        for h in range(4):
            pss, pks = PSL()
            for dc in range(2):
                tt("pool", pTm[:, dc, :, :], big2[:, h * 2 + dc, PT:PT + 64].unsqueeze(1).to_broadcast([128, NB, 64]),
                   _rs(mrowb[:, :], "p (b t) -> p b t", b=NB), ALU.mult, QK + ["mrowb"], [CT, "pTm"])
            for b in range(NB):
                dma(_rs(kvst[:, 0:512], "p (c m) -> p c m", c=2), _rs(kT_d[l, b, h], "(c p) m -> p c m", p=128), [], [CT, "kvst"], "kvst")
                cp("pool", kvbf[:, 0:512], kvst[:, 0:512], ["kvst", CT], [CT, "kvbf"])
                for dc in range(2):
                    mm(pss[0:64, 0:256], pTm[:, dc, b, :], kvbf[:, dc * 256:(dc + 1) * 256], (b == 0 and dc == 0), (b == NB - 1 and dc == 1),
                       ["pTm", "kvbf", CT], [pks])
            P.add("dve", lambda e, o=smalls[0:64, 12:13], i=pss[0:64, 0:256]: e.tensor_reduce(o, i, AX.X, ALU.max), r=[pks], w=["xa0"])
            ts("dve", smalls[0:64, 13:14], smalls[0:64, 12:13], -1.0, None, ALU.mult, None, ["xa0"], ["xa1"])
            act(pexp[0:64, 0:256], pss[0:64, 0:256], AF.Exp, [pks, "xa1"], [CT, "pexp", "xa2"], bias=smalls[0:64, 13:14], accum=smalls[0:64, 14:15])
            pst, pkt = PS()
            pstb = pst[:, :].bitcast(BF16)
            for mc in range(2):
                P.add("pe", lambda e, o=pstb[:, mc * 64:(mc + 1) * 64], i=pexp[0:64, mc * 128:(mc + 1) * 128]:
                      e.transpose(o, i, identb[0:64, 0:64]), r=["pexp", CT, "constsb_i"], w=[pkt])
            cp("act", pT[:, 0:128], pstb[:, 0:128], [pkt], [CT, "pT"])
            for mc in range(2):
                tt("pool", pTm[:, mc, :, :], pT[:, mc * 64:(mc + 1) * 64].unsqueeze(1).to_broadcast([128, NB, 64]),
                   _rs(mrowb[:, :], "p (b t) -> p b t", b=NB), ALU.mult, ["pT", "mrowb", CT], [CT, "pTm"])
            pso, pko = PSL()
            for b in range(NB):
                dma(_rs(kvst[:, 0:512], "p (c e) -> p c e", c=2),
                    _rs(v_d[l, b, :, h * 256:(h + 1) * 256], "(c p) e -> p c e", p=128), [], [CT, "kvst"], "kvst")
                cp("pool", kvbf[:, 0:512], kvst[:, 0:512], ["kvst", CT], [CT, "kvbf"])
                for mc in range(2):
                    mm(pso[0:64, 0:256], pTm[:, mc, b, :], kvbf[:, mc * 256:(mc + 1) * 256], (b == 0 and mc == 0), (b == NB - 1 and mc == 1),
                       ["pTm", "kvbf", CT], [pko])
            P.add("dve", lambda e, a=smalls[0:64, 15:16], i=smalls[0:64, 14:15]: e.reciprocal(a, i), r=["xa2"], w=["xa3"])
            ts("dve", aot[0:64, h * 256:(h + 1) * 256], pso[0:64, 0:256], smalls[0:64, 15:16], None, ALU.mult, None, [pko, "xa3"], ["aot"])
        pst, pkt = PS()
        pstb = pst[:, :].bitcast(BF16)
        for c in range(8):
            P.add("pe", lambda e, o=pstb[:, c * 128:c * 128 + 64], i=aot[0:64, c * 128:(c + 1) * 128]:
                  e.transpose(o, i, identb[0:64, 0:64]), r=["aot", "constsb_i"], w=[pkt])
        cp("act", aoT[:, :, PT:PT + 64], _rs(pstb, "p (c t) -> p c t", c=8)[:, :, 0:64], [pkt], [CT, "aoT"])
        chk(l * 10 + 8.5)
        residual(w_xo[l], 8, lambda kc, t0, n: aoT[:, kc, t0:t0 + n], [CT, "aoT"])
        fence()

        chk(l * 10 + 9)
        rmsnorm(xcur, xk, pc + PC_NFFN, hT_dst)
        fence()
        for g0, gn in ((0, 8), (8, 8), (16, 6)):
            for i in range(gn):
                hc = g0 + i
                wtg, wkg = wtile(w_fi[l], 0, 8, hc * 128, 128)
                wtu, wku = wtile(w_fi[l], 0, 8, DFF + hc * 128, 128)
                for ti, (t0, n) in enumerate(TT):
                    psG, pkG = PS()
                    for kc in range(8):
                        mm(psG[:, 0:n], wtg[:, kc, :], hsrc(kc, t0, n), kc == 0, kc == 7, [wkg] + HK, [pkG])
                    sg, sgk = T5()
                    act(sg[:, 0:n], psG[:, 0:n], AF.Silu, [pkG], [sgk])
                    psU, pkU = PS()
                    for kc in range(8):
                        mm(psU[:, 0:n], wtu[:, kc, :], hsrc(kc, t0, n), kc == 0, kc == 7, [wku] + HK, [pkU])
                    tt("dve", big2[:, i, t0:t0 + n], psU[:, 0:n], sg[:, 0:n], ALU.mult, [pkU, sgk], [("u", i)])
            nonlocal_src = w_fo[l][g0 * 128:(g0 + gn) * 128, :]
            residual(nonlocal_src, gn, lambda kc, t0, n: big2[:, kc, t0:t0 + n], [("u", c) for c in range(8)])

    stopped = False
    try:
        chk(0)
        layers()
    except _Stop:
        stopped = True
    P.epoch = L

    def y_dst(c, t0, n):
        return None
    for ti, (t0, n) in enumerate([] if stopped else TT):
        dma(xin[:, :, 0:n], xcur[:, :, t0:t0 + n], [(xk, ti)], ["xin", CT], "xin")
        ps, pk = PS()
        for c in range(8):
            act(sqb[:, c % 2, 0:n], xin[:, c, 0:n], AF.Square, ["xin"], [("sqb", c % 2)])
            mm(ps[:, 0:n], onesb[:, :], sqb[:, c % 2, 0:n], c == 0, c == 7, [("sqb", c % 2)], [pk])
        rs, rk = T5()
        act(rs[:, 0:n], ps[:, 0:n], AF.Sqrt, [pk], [rk], bias=EPS, scale=1.0 / D)
        P.add("dve", lambda e, a=rs[:, 0:n]: e.reciprocal(a, a), r=[rk], w=[rk])
        for c in range(8):
            stt(xin[:, c, 0:n], xin[:, c, 0:n], pcol[:, PC_FIN + c:PC_FIN + c + 1], rs[:, 0:n], ALU.mult, ALU.mult,
                ["xin", rk, "pcol"], ["xin"])
        dma(yT[:, :, t0:t0 + n], xin[:, :, 0:n], ["xin"], [], "yout")

    P.finalize()
    sem_names = set()
    for op in P.ops:
        if op.dma is not None:
            sem_names.add(("d", op.dma))
        elif op.signal:
            sem_names.add(("c", op.eng, op.epoch))
    sems = {}
    for i, s in enumerate(sorted(sem_names, key=str)):
        sems[s] = nc.semaphore("s%d" % i).__enter__()
    by_eng = {"pe": [], "act": [], "dve": [], "pool": [], "sp": []}
    for op in P.ops:
        by_eng[op.eng].append(op)
    final_waits = [(("d", k), v) for k, v in P.dma_cnt.items()]

    def run(e, ops, final=False):
        for op in ops:
            for s, v in op.waits:
                e.wait_ge(sems[s], v)
            ins = op.fn(e)
            if op.dma is not None:
                ins.then_inc(sems[("d", op.dma)], 16)
            elif op.signal:
                ins.then_inc(sems[("c", op.eng, op.epoch)], 1)
        if final:
            for s, v in final_waits:
                e.wait_ge(sems[s], v)

    with nc.allow_non_contiguous_dma(reason="small strided state rows"), nc.Block() as block:
        @block.sync
        def _(e):
            run(e, by_eng["sp"], final=True)

        @block.tensor
        def _(e):
            run(e, by_eng["pe"])

        @block.scalar
        def _(e):
            run(e, by_eng["act"])

        @block.vector
        def _(e):
            run(e, by_eng["dve"])

        @block.gpsimd
        def _(e):
            run(e, by_eng["pool"])
    return nc, len(P.ops)


def _consts():
    c = np.zeros((128, 1540), np.float32)
    c[:, 0:128] = np.eye(128)
    c[:, 128:256] = 1.0
    s = np.arange(128)
    c[:, 256:384] = np.where(s[None, :] >= s[:, None], 0.0, -30000.0)
    s6 = np.arange(64)
    same = (s6[:, None] // 4) == (s6[None, :] // 4)
    c[0:64, 384:448] = np.where(same & (s6[None, :] >= s6[:, None]), 0.0, -30000.0)
    c[0:64, 448:464] = (s6[:, None] // 4 == np.arange(16)[None, :]).astype(np.float32)
    c[0:4, 464:468] = np.eye(4)
    for h in range(4):
        c[h, 512 + h * 128:512 + (h + 1) * 128] = -1.0
        c[h, 1024 + h * 128:1024 + (h + 1) * 128] = 1.0
    mr = (np.arange(16)[:, None] == (s6[None, :] // 4)).astype(np.float32).reshape(-1)
    c[:, 1536] = 1.0
    c[:, 1537] = 0.0
    c[:, 1538] = -1.0
    return c, np.ascontiguousarray(np.broadcast_to(mr[None, :], (128, 1024))).astype(np.float32)


def _col(v):
    return np.ascontiguousarray(v.reshape(-1, 128).T)


_CACHE = {}


def _prep(inp):
    f = lambda k: np.asarray(inp[k], dtype=np.float32)
    cst, mrow = _consts()
    pcol = np.zeros((128, L * NPC), np.float32)
    for l in range(L):
        b = l * NPC
        pcol[:, b + PC_NMIX:b + PC_NMIX + 8] = _col(f("norm_mix_g")[l])
        pcol[:, b + PC_NX:b + PC_NX + 8] = _col(f("norm_x_g")[l])
        pcol[:, b + PC_NFFN:b + PC_NFFN + 8] = _col(f("norm_ffn_g")[l])
        pcol[:, b + PC_NMEM:b + PC_NMEM + 8] = _col(f("norm_mem_g")[l])
        pcol[:, b + PC_FIN:b + PC_FIN + 8] = _col(f("final_norm_g"))
        caw = f("conv_a_w")[l]
        cbw = f("conv_b_w")[l]
        for j in range(4):
            pcol[:, b + PC_CAW + j * 3:b + PC_CAW + (j + 1) * 3] = caw[:, j * 128:(j + 1) * 128].T
            pcol[:, b + PC_CBW + j * 31:b + PC_CBW + (j + 1) * 31] = cbw[:, j * 128:(j + 1) * 128].T
        pcol[:, b + PC_CBB:b + PC_CBB + 4] = _col(f("conv_b_b")[l])
        pcol[:, b + PC_LNG:b + PC_LNG + 4] = _col(f("ln_b_g")[l])
        pcol[:, b + PC_LNB:b + PC_LNB + 4] = _col(f("ln_b_b")[l])
    pcol[:, PC_FIN:PC_FIN + 8] = _col(f("final_norm_g"))
    gnorm = np.ascontiguousarray(np.broadcast_to(f("mlstm_norm_g")[:, None, :], (L, 128, D)))
    bif = np.ascontiguousarray(f("b_if").reshape(L, 2, 4).transpose(2, 0, 1).reshape(4, L * 2))
    xp, xs = f("x_prompt"), f("x_sample")
    shared = {k: f(k) for k in ("w_in", "w_out_a", "w_out_b", "w_out_c", "w_o", "w_xq", "w_xkv", "w_xo", "w_ffn_in", "w_ffn_out")}
    in_maps = []
    for i in range(8):
        bs = slice(i * NB, (i + 1) * NB)
        xt = np.concatenate([xp[i], xs[bs].reshape(ST, D)], axis=0)
        xT = np.ascontiguousarray(xt.T.reshape(8, 128, T).transpose(1, 0, 2))
        memtok = np.ascontiguousarray(f("mem_prompt")[i].reshape(2, 128, D).transpose(1, 0, 2))
        sca = f("state_conv_a")[:, bs]
        sca = np.ascontiguousarray(sca.reshape(L, NB, 2, 4, 128).transpose(4, 0, 3, 1, 2).reshape(128, -1))
        scb = f("state_conv_b")[:, bs]
        scb = np.ascontiguousarray(scb.reshape(L, NB, 30, 4, 128).transpose(4, 0, 3, 1, 2).reshape(128, -1))
        cn = np.ascontiguousarray(f("state_mlstm_c")[:, bs])
        ctr = np.ascontiguousarray(cn.transpose(0, 1, 2, 4, 3))
        nn = f("state_mlstm_n")[:, bs]
        nTi = np.ascontiguousarray(nn.reshape(L, NB, 4, 2, 128).transpose(4, 0, 3, 1, 2).reshape(128, -1))
        mi = np.ascontiguousarray(f("state_mlstm_m")[:, bs].transpose(2, 0, 1).reshape(4, -1))
        kTc = np.ascontiguousarray(f("cache_mem_k")[:, bs].transpose(0, 1, 3, 4, 2))
        vc = np.ascontiguousarray(f("cache_mem_v")[:, bs].reshape(L, NB, 256, D))
        m = {"xT_in": xT, "memtok": memtok, "pcol": pcol, "gnorm": gnorm, "bif": bif, "sca": sca, "scb": scb,
             "cnat": cn, "ctr": ctr, "nTin": nTi, "min": mi, "kTc": kTc, "vc": vc, "cst": cst, "mrow": mrow}
        m.update(shared)
        in_maps.append(m)
    return in_maps


def _post(R):
    y_p = np.zeros((8, PT, D), np.float32)
    y_s = np.zeros((128, 4, D), np.float32)
    p_a = np.zeros((L, 8, 2, 512), np.float32)
    p_b = np.zeros((L, 8, 30, 512), np.float32)
    p_c = np.zeros((L, 8, 4, 256, 256), np.float32)
    p_n = np.zeros((L, 8, 4, 256), np.float32)
    p_m = np.zeros((L, 8, 4), np.float32)
    p_k = np.zeros((L, 8, 256, 4, 256), np.float32)
    p_v = np.zeros((L, 8, 256, 4, 256), np.float32)
    s_a = np.zeros((L, 128, 2, 512), np.float32)
    s_b = np.zeros((L, 128, 30, 512), np.float32)
    s_c = np.zeros((L, 128, 4, 256, 256), np.float32)
    s_n = np.zeros((L, 128, 4, 256), np.float32)
    s_m = np.zeros((L, 128, 4), np.float32)
    for i in range(8):
        r = R[i]
        bs = slice(i * NB, (i + 1) * NB)
        yt = r["yT"].transpose(2, 1, 0).reshape(T, D)
        y_p[i] = yt[:PT]
        y_s[bs] = yt[PT:].reshape(NB, 4, D)
        a = r["oca"].reshape(128, L, 4, 17, 2).transpose(1, 3, 4, 2, 0).reshape(L, 17, 2, 512)
        p_a[:, i] = a[:, 0]
        s_a[:, bs] = a[:, 1:]
        b_ = r["ocb"].reshape(128, L, 4, 17, 30).transpose(1, 3, 4, 2, 0).reshape(L, 17, 30, 512)
        p_b[:, i] = b_[:, 0]
        s_b[:, bs] = b_[:, 1:]
        p_c[:, i] = r["oC"][:, 0]
        s_c[:, bs] = r["oC"][:, 1:]
        n_ = r["on"].reshape(128, L, 2, 68).transpose(1, 3, 2, 0).reshape(L, 68, 256)
        p_n[:, i] = n_[:, 0:4]
        s_n[:, bs] = n_[:, 4:].reshape(L, NB, 4, 256)
        m_ = r["om"].reshape(4, L, 17).transpose(1, 2, 0)
        p_m[:, i] = m_[:, 0]
        s_m[:, bs] = m_[:, 1:]
        p_k[:, i] = r["omk"].reshape(L, 256, 4, 256)
        p_v[:, i] = r["omv"].reshape(L, 256, 4, 256)
    return (y_p, y_s, p_a, p_b, p_c, p_n, p_m, p_k, p_v, s_a, s_b, s_c, s_n, s_m)


def kernel(**inp):
    if "nc" not in _CACHE:
        _CACHE["nc"] = build_nc()
    nc, nops = _CACHE["nc"]
    in_maps = _prep(inp)
    res = run_bass_kernel_spmd(nc, in_maps, core_ids=list(range(8)))
    return _post(res.results)
```

```python
import os as _os
import numpy as np
import concourse.bass as bass
import concourse.mybir as mybir
from concourse.bass_utils import run_bass_kernel_spmd

F32 = mybir.dt.float32
BF16 = mybir.dt.bfloat16
AF = mybir.ActivationFunctionType
ALU = mybir.AluOpType
AX = mybir.AxisListType

L = 4
D = 1024
T = 2112
PT = 2048
ST = 64
NB = 16
TT = [(0, 512), (512, 512), (1024, 512), (1536, 512), (2048, 64)]
INW = 9736
DFF = 2816
EPS = 1e-6
C_AB, C_AC, C_AX, C_BV, C_BG = 0, 512, 1024, 1536, 2048
C_Q, C_K, C_V, C_O, C_IF = 2560, 3584, 4608, 5632, 6656
C_GA, C_GB, C_GC = 6664, 7688, 8712
PC_NMIX, PC_NX, PC_NFFN, PC_NMEM, PC_FIN = 0, 8, 16, 24, 32
PC_CAW, PC_CBW, PC_CBB, PC_LNG, PC_LNB = 40, 52, 176, 180, 184
NPC = 188


_CTKEYS = {"ctmp", "memtS", "memn", "mrowst", "xin", "vab", "abuf", "ucv", "dg", "scbS", "bconv", "qTs", "kTs", "ktok",
           "vtok", "gob", "vct", "qsTm", "vcTs", "aoT", "kvst", "kvbf", "pTm", "KTp", "Vp", "memTl", "pexp", "pT", "cns", "ctsf", "xb"}


class Op:
    __slots__ = ("eng", "fn", "deps", "dma", "dval", "signal", "sigval", "waits", "epoch", "idx")


class Prog:
    def __init__(self):
        self.ops = []
        self.lastw = {}
        self.rd_eng = {}
        self.rd_dma = {}
        self.dma_cnt = {}
        self.epoch = 0

    def add(self, eng, fn, r=(), w=(), dma=None, fence=False):
        if not fence:
            def _isct(k):
                n = k[0] if isinstance(k, tuple) else k
                return n in _CTKEYS
            if any(_isct(k) for k in list(r) + list(w)):
                r = [k for k in r if k != "ctmp"] + ["ctmp"]
                w = [k for k in w if k != "ctmp"]
        op = Op()
        op.eng, op.fn, op.dma, op.epoch = eng, fn, dma, self.epoch
        op.idx = len(self.ops)
        op.signal = False
        op.sigval = 0
        op.dval = 0
        deps = set()
        for k in r:
            if k in self.lastw:
                deps.add(self.lastw[k])
        for k in w:
            if k in self.lastw:
                deps.add(self.lastw[k])
            for e, i in self.rd_eng.get(k, {}).items():
                deps.add(i)
            for i in self.rd_dma.get(k, ()):
                deps.add(i)
        for k in r:
            if dma is not None:
                self.rd_dma.setdefault(k, []).append(op.idx)
            else:
                self.rd_eng.setdefault(k, {})[eng] = op.idx
        for k in w:
            self.lastw[k] = op.idx
            self.rd_eng[k] = {}
            self.rd_dma[k] = []
        if dma is not None:
            c = self.dma_cnt.get(dma, 0) + 16
            self.dma_cnt[dma] = c
            op.dval = c
        deps.discard(op.idx)
        op.deps = deps
        self.ops.append(op)
        return op

    def finalize(self):
        ops = self.ops
        for op in ops:
            for d in op.deps:
                y = ops[d]
                if y.dma is None and not (y.eng == op.eng and op.dma is None and op.eng == "pe"):
                    y.signal = True
        cnt = {}
        for op in ops:
            if op.signal:
                k = (op.eng, op.epoch)
                cnt[k] = cnt.get(k, 0) + 1
                op.sigval = cnt[k]
        waited = {}
        for op in ops:
            need = {}
            for d in op.deps:
                y = ops[d]
                if y.dma is not None:
                    s, v = ("d", y.dma), y.dval
                elif y.eng == op.eng and op.dma is None and op.eng == "pe":
                    continue
                else:
                    s, v = ("c", y.eng, y.epoch), y.sigval
                if need.get(s, 0) < v:
                    need[s] = v
            ws = []
            wd = waited.setdefault(op.eng, {})
            for s, v in need.items():
                if wd.get(s, 0) < v:
                    wd[s] = v
                    ws.append((s, v))
            op.waits = ws
        return cnt


def _rs(a, pat, **kw):
    return a.rearrange(pat, **kw)


class _Stop(Exception):
    pass


def build_nc(stop=10 ** 9):
    def chk(i):
        if i >= stop:
            raise _Stop()
    nc = bass.Bass("TRN2", target_bir_lowering=False)
    P = Prog()

    def din(name, shape):
        return nc.dram_tensor(name, list(shape), F32, kind="ExternalInput").ap()

    def dout(name, shape):
        return nc.dram_tensor(name, list(shape), F32, kind="ExternalOutput").ap()

    xT_in = din("xT_in", [128, 8, T])
    memtok = din("memtok", [128, 2, D])
    pcol_d = din("pcol", [128, L * NPC])
    gnorm_d = din("gnorm", [L, 128, D])
    bif_d = din("bif", [4, L * 2])
    sca_d = din("sca", [128, L * 4 * NB * 2])
    scb_d = din("scb", [128, L * 4 * NB * 30])
    cnat_d = din("cnat", [L, NB, 4, 256, 256])
    ctr_d = din("ctr", [L, NB, 4, 256, 256])
    nT_d = din("nTin", [128, L * 2 * 64])
    m_d = din("min", [4, L * NB])
    kT_d = din("kTc", [L, NB, 4, 256, 256])
    v_d = din("vc", [L, NB, 256, D])
    cst_d = din("cst", [128, 1540])
    mrow_d = din("mrow", [128, 1024])
    w_in = din("w_in", [L, D, INW])
    w_oa = din("w_out_a", [L, 512, D])
    w_ob = din("w_out_b", [L, 512, D])
    w_oc = din("w_out_c", [L, D, D])
    w_o = din("w_o", [L, D, D])
    w_xq = din("w_xq", [L, D, D])
    w_xkv = din("w_xkv", [L, D, 2 * D])
    w_xo = din("w_xo", [L, D, D])
    w_fi = din("w_ffn_in", [L, D, 2 * DFF])
    w_fo = din("w_ffn_out", [L, DFF, D])

    yT = dout("yT", [128, 8, T])
    oca = dout("oca", [128, L * 4, 17 * 2])
    ocb = dout("ocb", [128, L * 4, 17 * 30])
    oC = dout("oC", [L, 17, 4, 256, 256])
    on = dout("on", [128, L, 2 * 68])
    om = dout("om", [4, L * 17])
    omk = dout("omk", [L, 256, D])
    omv = dout("omv", [L, 256, D])
    xsA = nc.dram_tensor("xsA", [128, 8, T], F32).ap()
    xsB = nc.dram_tensor("xsB", [128, 8, T], F32).ap()

    def sb(name, shape, dt=F32):
        return nc.alloc_sbuf_tensor(name, list(shape), dt) if False else nc.sbuf_tensor(name, list(shape), dt).__enter__()

    hT = sb("hT", [128, 8, T], BF16)
    big2 = sb("big2", [128, 8, T], BF16)
    ctmp = sb("ctmp", [128, 16384], F32)
    wst = sb("wst", [128, 2, 1024], F32)
    wbf = sb("wbf", [128, 2, 1024], BF16)
    cst = sb("cstsb", [128, 1540], F32)
    pcol = sb("pcolsb", [128, L * NPC], F32)
    gnb = sb("gnb", [128, D], F32)
    sqb = sb("sqb", [128, 2, 512], BF16)
    t512 = sb("t512", [128, 4, 512], F32)
    lnbuf = sb("lnbuf", [128, 2, 512], F32)
    rows = sb("rows", [4, 8, 128], F32)
    bif = sb("bifsb", [4, L * 2], F32)
    gcol = sb("gcol", [128, 16], F32)
    smalls = sb("smalls", [128, 32], F32)
    wprb = sb("wprb", [128, 64], F32)
    wTt = sb("wTt", [128, 128], F32)
    smT = sb("smT", [128, 128], BF16)
    qsT = sb("qsT", [128, 2, 128], BF16)
    cnp = sb("cnp", [128, 4, 512], F32)
    ctp = sb("ctp", [128, 4, 2 * 258], BF16)
    cts = sb("cts", [128, 2, 2 * 258], BF16)
    nT = sb("nT", [128, 2, 68], F32)
    msb = sb("msb", [4, 64], F32)
    vwb = sb("vwb", [128, 2, 256], BF16)
    wlm = sb("wlm", [128, 16], F32)
    wlmb = sb("wlmb", [128, 16], BF16)
    ostA = sb("ostA", [128, 34], F32)
    ostB = sb("ostB", [128, 17 * 30], F32)
    scaS = sb("scaS", [128, 4 * NB * 2], F32)
    oms = sb("oms", [128, 2, 256], F32)
    identb = sb("identb", [128, 128], BF16)
    onesb = sb("onesb", [128, 128], BF16)
    mrowb = sb("mrowb", [128, NB * 64], BF16)

    ident = cst[:, 0:128]
    onesf = cst[:, 128:256]
    maskP = cst[:, 256:384]
    maskS = cst[0:64, 384:448]
    maskcol = cst[0:64, 448:464]
    I4 = cst[0:4, 464:468]
    selneg = cst[0:4, 512:1024]
    selpos = cst[0:4, 1024:1536]

    def cv(off, nbytes, dt):
        a = ctmp[:, off // 4:(off + nbytes) // 4]
        return a.bitcast(dt) if dt != F32 else a

    xin2 = [_rs(cv(0, 16384, F32), "p (c t) -> p c t", c=8), _rs(cv(16384, 16384, F32), "p (c t) -> p c t", c=8)]
    xbufs = [cv(33792, 8448, F32), cv(42240, 8448, F32)]
    mrow_st = cv(16384, 4096, F32)
    cns = _rs(cv(55360, 4096, F32), "p (s n) -> p s n", s=2)
    cts_f = _rs(cv(59456, 4096, F32), "p (s n) -> p s n", s=2)
    vab = _rs(cv(0, 16896, BF16), "p (c t) -> p c t", c=4)
    abuf = cv(16896, 4224, BF16)
    ucv = [cv(21120, 5248, BF16), cv(26368, 5248, BF16)]
    dg = _rs(cv(31616, 7936, BF16), "p (k n) -> p k n", k=31)
    scbS = cv(39552, 7680, F32)
    bconv = _rs(cv(47232, 16896, BF16), "p (c t) -> p c t", c=4)
    qTs = _rs(cv(0, 8192, BF16), "p (c t) -> p c t", c=8)
    kTs = _rs(cv(8192, 8192, BF16), "p (c t) -> p c t", c=8)
    ktok = _rs(cv(16384, 8192, BF16), "p (c n) -> p c n", c=4)
    vtok = _rs(cv(24576, 8256, BF16), "p (c h n) -> p c h n", c=4, h=4)
    gob = _rs(cv(32832, 8192, BF16), "p (c n) -> p c n", c=4)
    vct = cv(41024, 2048, BF16)
    qsTm = _rs(cv(43072, 4096, BF16), "p (c b t) -> p c b t", c=2, b=NB)
    vcTs = _rs(cv(47168, 8192, BF16), "p (c t) -> p c t", c=8)
    aoT = _rs(cv(0, 33792, BF16), "p (c t) -> p c t", c=8)
    kvst = cv(33792, 8192, F32)
    kvbf = cv(41984, 4096, BF16)
    pTm = _rs(cv(46080, 4096, BF16), "p (c b t) -> p c b t", c=2, b=NB)
    KTp = _rs(cv(50176, 4096, BF16), "p (c m) -> p c m", c=8)
    Vp = _rs(cv(54272, 4096, BF16), "p (c n) -> p c n", c=2)
    memTl = _rs(cv(58368, 4096, BF16), "p (c m) -> p c m", c=8)
    pexp = cv(62464, 2048, BF16)
    pT = cv(64512, 1024, BF16)
    memtS = _rs(cv(0, 8192, F32), "p (c n) -> p c n", c=2)
    memn = _rs(cv(8192, 8192, F32), "p (c n) -> p c n", c=2)
    memTb = sb("memTb", [128, 8, 256], BF16)
    aot = sb("aot", [128, 1024], BF16)

    psb = [nc.psum_tensor("ps%d" % i, [128, 512], F32).__enter__() for i in range(8)]
    ps_ctr = [0]

    def PS():
        i = ps_ctr[0] % 6
        ps_ctr[0] += 1
        return psb[i], ("ps", i)

    psl_ctr = [0]

    def PSL():
        i = 6 + psl_ctr[0] % 2
        psl_ctr[0] += 1
        return psb[i], ("ps", i)

    t5_ctr = [0]

    def T5():
        i = t5_ctr[0] % 4
        t5_ctr[0] += 1
        return t512[:, i, :], ("t5", i)

    CT = "ctmp"

    def fence():
        P.add("pool", lambda e: e.memset(smalls[:, 31:32], 0.0), r=[], w=[CT, "fencebyte"], fence=True)

    w_ctr = [0]

    def wtile(src2d, r0, nkc, c0, ncols):
        s = w_ctr[0] % 2
        w_ctr[0] += 1
        n = nkc * ncols
        src = _rs(src2d[r0:r0 + nkc * 128, c0:c0 + ncols], "(kc p) n -> p kc n", p=128)
        dst = _rs(wst[:, s, 0:n], "p (kc n) -> p kc n", kc=nkc)
        P.add("sp", lambda e: e.dma_start(out=dst, in_=src), r=[], w=[("wst", s)], dma="ws%d_%d" % (s, P.epoch))
        P.add("pool", lambda e: e.tensor_copy(out=wbf[:, s, 0:n], in_=wst[:, s, 0:n]), r=[("wst", s)], w=[("wbf", s)])
        return _rs(wbf[:, s, 0:n], "p (kc n) -> p kc n", kc=nkc), ("wbf", s)

    def mm(out, lhsT, rhs, start, stop, r, w):
        P.add("pe", lambda e: e.matmul(out, lhsT=lhsT, rhs=rhs, start=start, stop=stop), r=r, w=w)

    def proj(wt, wk, col0, src, srck, nkc, tiles, consume):
        for ti, (t0, n) in enumerate(tiles):
            ps, pk = PS()
            for kc in range(nkc):
                mm(ps[:, 0:n], wt[:, kc, col0:col0 + 128], src(kc, t0, n), kc == 0, kc == nkc - 1,
                   [wk] + srck, [pk])
            consume(ti, t0, n, ps, pk)

    def act(out, in_, func, r, w, bias=0.0, scale=1.0, accum=None):
        if accum is None:
            P.add("act", lambda e: e.activation(out, in_, func, bias=bias, scale=scale), r=r, w=w)
        else:
            P.add("act", lambda e: e.activation(out, in_, func, bias=bias, scale=scale, accum_out=accum), r=r, w=w)

    def tt(eng, out, a, b, op, r, w):
        P.add(eng, lambda e: e.tensor_tensor(out, a, b, op), r=r, w=w)

    def stt(out, a, s, b, op0, op1, r, w):
        P.add("dve", lambda e: e.scalar_tensor_tensor(out, a, s, b, op0, op1), r=r, w=w)

    def ts(eng, out, a, s1, s2, op0, op1, r, w):
        if s2 is None:
            P.add(eng, lambda e: e.tensor_scalar(out, a, s1, None, op0), r=r, w=w)
        else:
            P.add(eng, lambda e: e.tensor_scalar(out, a, s1, s2, op0, op1), r=r, w=w)

    def cp(eng, out, a, r, w):
        if eng == "act":
            P.add("act", lambda e: e.activation(out, a, AF.Copy), r=r, w=w)
        else:
            P.add(eng, lambda e: e.tensor_copy(out=out, in_=a), r=r, w=w)

    def dma(out, in_, r, w, sem):
        P.add("sp", lambda e: e.dma_start(out=out, in_=in_), r=r, w=w, dma=sem)

    HK = [("hT", c) for c in range(8)]

    def rmsnorm(xsrc, xsk, gcolbase, dst_fn):
        for ti, (t0, n) in enumerate(TT):
            xin, xik = xin2[ti % 2], ("xin", ti % 2)
            dma(xin[:, :, 0:n], xsrc[:, :, t0:t0 + n], [(xsk, ti)], [xik], "xin%d" % (ti % 2))
            ps, pk = PS()
            for c in range(8):
                act(sqb[:, c % 2, 0:n], xin[:, c, 0:n], AF.Square, [xik], [("sqb", c % 2)])
                mm(ps[:, 0:n], onesb[:, :], sqb[:, c % 2, 0:n], c == 0, c == 7, [("sqb", c % 2), "constsb_o"], [pk])
            rs, rk = T5()
            act(rs[:, 0:n], ps[:, 0:n], AF.Sqrt, [pk], [rk], bias=EPS, scale=1.0 / D)
            P.add("dve", lambda e, a=rs[:, 0:n]: e.reciprocal(a, a), r=[rk], w=[rk])
            for c in range(8):
                o, ok = dst_fn(c, t0, n)
                stt(o, xin[:, c, 0:n], pcol[:, gcolbase + c:gcolbase + c + 1], rs[:, 0:n], ALU.mult, ALU.mult,
                    [xik, rk, "pcol"], ok)

    def hT_dst(c, t0, n):
        return hT[:, c, t0:t0 + n], [("hT", c)]

    def hsrc(kc, t0, n):
        return hT[:, kc, t0:t0 + n]

    dma(cst[:, :], cst_d, [], ["consts"], "init1")
    dma(pcol[:, :], pcol_d, [], ["pcol"], "init2")
    dma(bif[:, :], bif_d, [], ["bif"], "init3")
    dma(memtS, memtok, [], [CT, "memtS"], "init4")
    cp("dve", identb[:, :], ident, ["consts"], ["constsb_i"])
    cp("dve", onesb[:, :], onesf, ["consts"], ["constsb_o"])
    dma(mrow_st, mrow_d, [], [CT, "mrowst"], "init5")
    cp("dve", mrowb[:, :], mrow_st, ["mrowst", CT], ["mrowb"])
    P.add("pool", lambda e: e.memset(_rs(vtok, "p c h n -> p (c h) n")[:, :, 256:258], 1.0), r=[], w=[CT])
    for mc in range(2):
        jk, jkk = T5()
        act(jk[:, 0:512], memtS[:, mc, 0:512], AF.Square, [CT, "memtS"], [jkk], accum=smalls[:, mc * 2:mc * 2 + 1])
        jk2, jkk2 = T5()
        act(jk2[:, 0:512], memtS[:, mc, 512:1024], AF.Square, [CT, "memtS"], [jkk2, "sm0"], accum=smalls[:, mc * 2 + 1:mc * 2 + 2])
        tt("dve", smalls[:, 4 + mc:5 + mc], smalls[:, mc * 2:mc * 2 + 1], smalls[:, mc * 2 + 1:mc * 2 + 2], ALU.add,
           [jkk, jkk2, "sm0"], ["sm1"])
        act(smalls[:, 6 + mc:7 + mc], smalls[:, 4 + mc:5 + mc], AF.Sqrt, ["sm1"], ["sm2"], bias=EPS, scale=1.0 / D)
        P.add("dve", lambda e, a=smalls[:, 6 + mc:7 + mc]: e.reciprocal(a, a), r=["sm2"], w=["sm3"])
        ts("dve", memn[:, mc, :], memtS[:, mc, :], smalls[:, 6 + mc:7 + mc], None, ALU.mult, None, ["sm3", CT, "memtS"], [CT, "memn"])
        for g in range(2):
            ps, pk = PS()
            for q in range(4):
                c = g * 4 + q
                P.add("pe", lambda e, o=ps[:, q * 128:(q + 1) * 128], i=memn[:, mc, c * 128:(c + 1) * 128]:
                      e.transpose(o, i, ident), r=["memn", "consts"], w=[pk])
            cp("dve", memTb[:, g * 4:(g + 1) * 4, mc * 128:(mc + 1) * 128],
               _rs(ps[:, :], "p (q t) -> p q t", q=4), [pk], ["memTb"])
    P.add("pool", lambda e: e.memset(nT[:, :, 0:4], 0.0), r=[], w=["nT"])
    fence()

    xcur, xk = xT_in, "x0"
    xdsts = [(xsA, "xA"), (xsB, "xB")]
    xflip = [0]

    xb_ctr = [0]

    def residual(wsrc2d, nk, srcfn, srckeys):
        nonlocal xcur, xk
        xn, xnk = xdsts[xflip[0] % 2]
        xflip[0] += 1
        xsrc_, xsk_ = xcur, xk

        def pre(j):
            wt, wk = wtile(wsrc2d, 0, nk, j * 128, 128)
            i = xb_ctr[0] % 2
            xb_ctr[0] += 1
            dma(xbufs[i][:, :], xsrc_[:, j, :], [(xsk_, t) for t in range(5)], [("xb", i)], "xbl%d" % i)
            return wt, wk, i
        nxt = pre(0)
        for j in range(8):
            wt, wk, i = nxt
            if j + 1 < 8:
                nxt = pre(j + 1)

            def consume(ti, t0, n, ps, pk, i=i):
                tt("dve", xbufs[i][:, t0:t0 + n], ps[:, 0:n], xbufs[i][:, t0:t0 + n], ALU.add, [pk, ("xb", i)], [("xb", i)])
            proj(wt, wk, 0, srcfn, srckeys, nk, TT, consume)
            dma(xn[:, j, :], xbufs[i][:, :], [("xb", i)], [(xnk, t) for t in range(5)], "xbs%d" % i)
        xcur, xk = xn, xnk

    def layers():
      for l in range(L):
        layer(l)

    def layer(l):
        nonlocal xcur, xk
        P.epoch = l
        pc = l * NPC
        Win = w_in[l]
        chk(l * 10 + 1)
        rmsnorm(xcur, xk, pc + PC_NMIX, hT_dst)
        fence()
        dma(gnb[:, :], gnorm_d[l], [], ["gnb"], "ld0_1")
        dma(scaS[:, :], sca_d[:, l * 128:(l + 1) * 128], [], ["scaS"], "ld0_2")
        dma(scbS, scb_d[:, l * 1920:(l + 1) * 1920], [], [CT, "scbS"], "ld0_3")
        dma(nT[:, :, 4:68], _rs(nT_d[:, l * 128:(l + 1) * 128], "p (c n) -> p c n", c=2), [], ["nT"], "ld0_4")
        dma(msb[:, 0:16], m_d[:, l * NB:(l + 1) * NB], [], ["msb"], "ld0_5")
        wg, wgk0 = None, None

        def conv_stage(K, colbase, halo_src, j, ub, ubk, consume):
            wv = pcol[:, colbase + j * K:colbase + (j + 1) * K]
            P.add("pool", lambda e: e.tensor_tensor(dg[:, 0:K, :], ident.unsqueeze(1).to_broadcast([128, K, 128]),
                                                    wv.unsqueeze(2).to_broadcast([128, K, 128]), ALU.mult),
                  r=["pcol", "consts"], w=[CT, "dg"])
            for ti, (t0, n) in enumerate(TT):
                ps, pk = PS()
                for k in range(K):
                    off = 30 - (K - 1) + k
                    if ti < 4:
                        rhs = ub[:, t0 + off:t0 + off + n]
                    else:
                        rhs = _rs(ub[:, 2078:2078 + NB * 34], "p (b t) -> p b t", b=NB)[:, :, off:off + 4]
                    mm(ps[:, 0:n], dg[:, k, :], rhs, k == 0, k == K - 1, ["dg", ubk, CT], [pk])
                consume(ti, t0, n, ps, pk)

        def ustage(colP, colG, gfunc, j, ub, ubk, halo_view, Kh, tailout):
            wtG, wkG = wtile(Win, 0, 8, colG + j * 128, 128)
            wtP, wkP = wtile(Win, 0, 8, colP + j * 128, 128)
            ubs = _rs(ub[:, 2078:2078 + NB * 34], "p (b t) -> p b t", b=NB)
            cp("pool", ubs[:, :, 30 - Kh:30], halo_view, ["scaS", "scbS", CT], [ubk, CT])
            for ti, (t0, n) in enumerate(TT):
                psG, pkG = PS()
                for kc in range(8):
                    mm(psG[:, 0:n], wtG[:, kc, :], hsrc(kc, t0, n), kc == 0, kc == 7, [wkG] + HK, [pkG])
                tg, tgk = T5()
                act(tg[:, 0:n], psG[:, 0:n], gfunc, [pkG], [tgk])
                psP, pkP = PS()
                for kc in range(8):
                    mm(psP[:, 0:n], wtP[:, kc, :], hsrc(kc, t0, n), kc == 0, kc == 7, [wkP] + HK, [pkP])
                if ti < 4:
                    tt("dve", ub[:, 30 + t0:30 + t0 + n], psP[:, 0:n], tg[:, 0:n], ALU.mult, [pkP, tgk], [ubk, CT])
                    if ti == 3:
                        tt("dve", tailout[:, 0:Kh], psP[:, 512 - Kh:512], tg[:, 512 - Kh:512], ALU.mult,
                           [pkP, tgk], ["ost"])
                else:
                    tt("dve", ubs[:, :, 30:34], _rs(psP[:, 0:64], "p (b t) -> p b t", b=NB),
                       _rs(tg[:, 0:64], "p (b t) -> p b t", b=NB), ALU.mult, [pkP, tgk], [ubk, CT])
                    so = _rs(tailout[:, Kh:17 * Kh], "p (b t) -> p b t", b=NB)
                    if Kh >= 4:
                        tt("dve", so[:, :, Kh - 4:Kh], _rs(psP[:, 0:64], "p (b t) -> p b t", b=NB),
                           _rs(tg[:, 0:64], "p (b t) -> p b t", b=NB), ALU.mult, [pkP, tgk], ["ost"])
                    else:
                        tt("dve", so[:, :, 0:Kh], _rs(psP[:, 0:64], "p (b t) -> p b t", b=NB)[:, :, 4 - Kh:4],
                           _rs(tg[:, 0:64], "p (b t) -> p b t", b=NB)[:, :, 4 - Kh:4], ALU.mult, [pkP, tgk], ["ost"])

        chk(l * 10 + 2)
        P.add("pool", lambda e: e.memset(ucv[0][:, 0:30], 0.0), r=[], w=[CT, ("ucv", 0)])
        P.add("pool", lambda e: e.memset(ucv[1][:, 0:30], 0.0), r=[], w=[CT, ("ucv", 1)])
        for j in range(4):
            ub, ubk = ucv[j % 2], ("ucv", j % 2)
            hv = _rs(scaS[:, j * 32:(j + 1) * 32], "p (b t) -> p b t", b=NB)
            ustage(C_AX, C_AC, AF.Copy, j, ub, ubk, hv, 2, ostA)
            dma(oca[:, l * 4 + j, :], ostA[:, :], ["ost"], [], "oca")
            wtB, wkB = wtile(Win, 0, 8, C_AB + j * 128, 128)
            for ti, (t0, n) in enumerate(TT):
                psb_, pkb = PS()
                for kc in range(8):
                    mm(psb_[:, 0:n], wtB[:, kc, :], hsrc(kc, t0, n), kc == 0, kc == 7, [wkB] + HK, [pkb])
                cp("act", abuf[:, t0:t0 + n], psb_[:, 0:n], [pkb], [CT, "abuf"])

            def consA(ti, t0, n, ps, pk, j=j):
                tt("dve", vab[:, j, t0:t0 + n], ps[:, 0:n], abuf[:, t0:t0 + n], ALU.mult, [pk, "abuf"], [CT, ("vab", j)])
            conv_stage(3, pc + PC_CAW, None, j, ub, ubk, consA)

        def merge_stage(first, gcol0, wsrc2d, nk, srcfn, srckeys, tiles):
            for j in range(8):
                wtG, wkG = wtile(Win, 0, 8, gcol0 + j * 128, 128)
                wtY, wkY = wtile(wsrc2d, 0, nk, j * 128, 128)
                for ti, (t0, n) in enumerate(tiles):
                    psG, pkG = PS()
                    for kc in range(8):
                        mm(psG[:, 0:n], wtG[:, kc, :], hsrc(kc, t0, n), kc == 0, kc == 7, [wkG] + HK, [pkG])
                    sg, sgk = T5()
                    act(sg[:, 0:n], psG[:, 0:n], AF.Sigmoid, [pkG], [sgk])
                    psY, pkY = PS()
                    for kc in range(nk):
                        mm(psY[:, 0:n], wtY[:, kc, :], srcfn(kc, t0, n), kc == 0, kc == nk - 1, [wkY] + srckeys, [pkY])
                    if first:
                        tt("dve", big2[:, j, t0:t0 + n], psY[:, 0:n], sg[:, 0:n], ALU.mult, [pkY, sgk], [("u", j)])
                    else:
                        tt("dve", sg[:, 0:n], psY[:, 0:n], sg[:, 0:n], ALU.mult, [pkY, sgk], [sgk])
                        tt("pool", big2[:, j, t0:t0 + n], big2[:, j, t0:t0 + n], sg[:, 0:n], ALU.add, [sgk, ("u", j)], [("u", j)])

        chk(l * 10 + 3)
        merge_stage(True, C_GA, w_oa[l], 4, lambda kc, t0, n: vab[:, kc, t0:t0 + n], [CT] + [("vab", c) for c in range(4)], TT)

        chk(l * 10 + 4)
        for j in range(4):
            ub, ubk = ucv[j % 2], ("ucv", j % 2)
            hv = _rs(scbS[:, j * 480:(j + 1) * 480], "p (b t) -> p b t", b=NB)
            cp("pool", _rs(ostB[:, 30:510], "p (b t) -> p b t", b=NB)[:, :, 0:26], hv[:, :, 4:30], ["scbS", CT], ["ost"])
            ustage(C_BV, C_BG, AF.Sigmoid, j, ub, ubk, hv, 30, ostB)
            dma(ocb[:, l * 4 + j, :], ostB[:, :], ["ost"], [], "ocb")

            def consB(ti, t0, n, ps, pk, j=j):
                act(bconv[:, j, t0:t0 + n], ps[:, 0:n], AF.Identity, [pk, "pcol"], [CT, ("bconv", j)],
                    bias=pcol[:, pc + PC_CBB + j:pc + PC_CBB + j + 1])
            conv_stage(31, pc + PC_CBW, None, j, ub, ubk, consB)
        BK = [("bconv", c) for c in range(4)]
        for ti, (t0, n) in enumerate(TT):
            ps1, pk1 = PS()
            for c in range(4):
                mm(ps1[:, 0:n], onesb[:, :], bconv[:, c, t0:t0 + n], c == 0, c == 3, BK + [CT, "constsb_o"], [pk1])
            ps2, pk2 = PS()
            for c in range(4):
                act(sqb[:, c % 2, 0:n], bconv[:, c, t0:t0 + n], AF.Square, BK + [CT], [("sqb", c % 2)])
                mm(ps2[:, 0:n], onesb[:, :], sqb[:, c % 2, 0:n], c == 0, c == 3, [("sqb", c % 2)], [pk2])
            mu, muk = lnbuf[:, 0, :], "lnmu"
            act(mu[:, 0:n], ps1[:, 0:n], AF.Copy, [pk1], [muk], scale=1.0 / 512)
            rs, rk = lnbuf[:, 1, :], "lnrs"
            tt("dve", rs[:, 0:n], mu[:, 0:n], mu[:, 0:n], ALU.mult, [muk], [rk])
            stt(rs[:, 0:n], ps2[:, 0:n], 1.0 / 512, rs[:, 0:n], ALU.mult, ALU.subtract, [pk2, rk], [rk])
            act(rs[:, 0:n], rs[:, 0:n], AF.Sqrt, [rk], [rk], bias=EPS)
            P.add("dve", lambda e, a=rs[:, 0:n]: e.reciprocal(a, a), r=[rk], w=[rk])
            for c in range(4):
                xc, xck = T5()
                tt("dve", xc[:, 0:n], bconv[:, c, t0:t0 + n], mu[:, 0:n], ALU.subtract, BK + [CT, muk], [xck])
                stt(xc[:, 0:n], xc[:, 0:n], pcol[:, pc + PC_LNG + c:pc + PC_LNG + c + 1], rs[:, 0:n], ALU.mult, ALU.mult,
                    [xck, rk, "pcol"], [xck])
                act(vab[:, c, t0:t0 + n], xc[:, 0:n], AF.Silu, [xck, "pcol"], [CT, ("vab", c)],
                    bias=pcol[:, pc + PC_LNB + c:pc + PC_LNB + c + 1])
        chk(l * 10 + 5)
        merge_stage(False, C_GB, w_ob[l], 4, lambda kc, t0, n: vab[:, kc, t0:t0 + n], [CT] + [("vab", c) for c in range(4)], TT)
        fence()

        chk(l * 10 + 6)
        P.add("pool", lambda e: e.memset(_rs(vtok, "p c h n -> p (c h) n")[:, :, 256:258], 1.0), r=[], w=[CT, "vtok"])
        P.add("pool", lambda e: e.memset(cnp[:, :, :], 0.0), r=[], w=["cnp"])
        P.add("pool", lambda e: e.memset(ctp[:, :, :], 0.0), r=[], w=["ctp"])
        P.add("pool", lambda e: e.memset(nT[:, :, 0:4], 0.0), r=[], w=["nT"])
        P.add("pool", lambda e: e.memset(msb[:, 16:20], 0.0), r=[], w=["carry"])
        wgt, wgk = wtile(Win, 0, 8, C_IF, 8)
        wgs = smT
        wgb = sb("wgb%d" % l, [128, 8, 8], BF16)
        cp("pool", wgb[:, :, :], wgt, [wgk], [("wgb", l)])
        R = lambda i: rows[:, i, :]
        for sc in range(5):
            t0s, ns = TT[sc]
            samp = sc == 4
            Lc = 64 if samp else 128
            ntc = 1 if samp else 4
            nseq = NB if samp else 1
            tiles_sc = [(t0s, ns)]
            for c in range(8):
                wtq, wkq = wtile(Win, 0, 8, C_Q + c * 128, 128)

                def cq(ti, t0, n, ps, pk, c=c):
                    cp("act", qTs[:, c, 0:n], ps[:, 0:n], [pk], [CT, "qTs"])
                proj(wtq, wkq, 0, hsrc, HK, 8, tiles_sc, cq)
                wtk, wkk = wtile(Win, 0, 8, C_K + c * 128, 128)

                def ck(ti, t0, n, ps, pk, c=c):
                    act(kTs[:, c, 0:n], ps[:, 0:n], AF.Copy, [pk], [CT, "kTs"], scale=0.0625)
                proj(wtk, wkk, 0, hsrc, HK, 8, tiles_sc, ck)
                for kind, col in (("k", C_K), ("v", C_V), ("o", C_O)):
                    if kind == "k":
                        wtt, wkt = wtk, wkk
                    else:
                        wtt, wkt = wtile(Win, 0, 8, col + c * 128, 128)
                    for tc in range(ntc):
                        ps, pk = PS()
                        ta = t0s + tc * 128
                        for kc in range(8):
                            mm(ps[0:Lc, 0:128], hT[:, kc, ta:ta + Lc], wtt[:, kc, :], kc == 0, kc == 7, [wkt] + HK, [pk])
                        if kind == "k":
                            act(ktok[0:Lc, tc, c * 128:(c + 1) * 128], ps[0:Lc, 0:128], AF.Copy, [pk], [CT, "ktok"], scale=0.0625)
                        elif kind == "v":
                            cp("dve", vtok[0:Lc, tc, c // 2, (c % 2) * 128:(c % 2) * 128 + 128], ps[0:Lc, 0:128], [pk], [CT, "vtok"])
                        else:
                            so_, sok = T5()
                            act(so_[0:Lc, 0:128], ps[0:Lc, 0:128], AF.Sigmoid, [pk], [sok])
                            tt("pool", gob[0:Lc, tc, c * 128:(c + 1) * 128], so_[0:Lc, 0:128], gnb[0:Lc, c * 128:(c + 1) * 128],
                               ALU.mult, [sok, "gnb"], [CT, "gob"])
            for tc in range(ntc):
                ta = t0s + tc * 128
                lo = tc * 128
                for gi_, (ro, co, bo) in enumerate(((0, 0, 0), (1, 4, 1))):
                    ps, pk = PS()
                    for kc in range(8):
                        mm(ps[0:4, 0:Lc], wgb[:, kc, co:co + 4], hT[:, kc, ta:ta + Lc], kc == 0, kc == 7, [("wgb", l)] + HK, [pk])
                    act(R(ro)[:, 0:Lc], ps[0:4, 0:Lc], AF.Identity, [pk, "bif"], [("row", ro)], bias=bif[:, l * 2 + bo:l * 2 + bo + 1])
                ts("dve", R(7)[:, 0:Lc], R(1)[:, 0:Lc], -1.0, None, ALU.mult, None, [("row", 1)], [("row", 7)])
                tt("dve", R(7)[:, 0:Lc], R(7)[:, 0:Lc], R(1)[:, 0:Lc], ALU.max, [("row", 1), ("row", 7)], [("row", 7)])
                act(R(7)[:, 0:Lc], R(7)[:, 0:Lc], AF.Exp, [("row", 7)], [("row", 7)], scale=-1.0)
                act(R(7)[:, 0:Lc], R(7)[:, 0:Lc], AF.Ln, [("row", 7)], [("row", 7)], bias=1.0)
                stt(R(1)[:, 0:Lc], R(1)[:, 0:Lc], 0.0, R(7)[:, 0:Lc], ALU.min, ALU.subtract, [("row", 1), ("row", 7)], [("row", 1)])
                if not samp:
                    P.add("dve", lambda e: e.tensor_tensor_scan(R(2)[:, 0:128], cst[0:4, 1536:1537].to_broadcast([4, 128]), R(1)[:, 0:128],
                                                                msb[:, 16:17], ALU.mult, ALU.add), r=[("row", 1), "carry", "consts"], w=[("row", 2)])
                    tt("dve", R(0)[:, 0:128], R(0)[:, 0:128], R(2)[:, 0:128], ALU.subtract, [("row", 0), ("row", 2)], [("row", 0)])
                    P.add("dve", lambda e: e.tensor_tensor_scan(R(3)[:, 0:128], cst[0:4, 1537:1538].to_broadcast([4, 128]), R(0)[:, 0:128],
                                                                msb[:, 17:18], ALU.add, ALU.max), r=[("row", 0), "carry", "consts"], w=[("row", 3)])
                    Mend = R(3)[:, 127:128].to_broadcast([4, 128])
                    Mprev = msb[:, 17:18].to_broadcast([4, 128])
                    tt("dve", msb[:, 20:21], msb[:, 17:18], R(3)[:, 127:128], ALU.subtract, ["carry", ("row", 3)], ["wpr"])
                    npair = 1
                else:
                    v3 = lambda i: _rs(R(i)[:, 0:64], "p (b t) -> p b t", b=NB)
                    cp("dve", v3(2)[:, :, 0:1], v3(1)[:, :, 0:1], [("row", 1)], [("row", 2)])
                    for t_ in range(1, 4):
                        tt("dve", v3(2)[:, :, t_:t_ + 1], v3(2)[:, :, t_ - 1:t_], v3(1)[:, :, t_:t_ + 1], ALU.add, [("row", 1), ("row", 2)], [("row", 2)])
                    tt("dve", R(0)[:, 0:64], R(0)[:, 0:64], R(2)[:, 0:64], ALU.subtract, [("row", 0), ("row", 2)], [("row", 0)])
                    m0 = msb[:, 0:16].unsqueeze(2)
                    tt("dve", v3(3)[:, :, 0:1], v3(0)[:, :, 0:1], m0, ALU.max, [("row", 0), "msb"], [("row", 3)])
                    for t_ in range(1, 4):
                        tt("dve", v3(3)[:, :, t_:t_ + 1], v3(3)[:, :, t_ - 1:t_], v3(0)[:, :, t_:t_ + 1], ALU.max, [("row", 0), ("row", 3)], [("row", 3)])
                    Mend = v3(3)[:, :, 3:4].to_broadcast([4, NB, 4])
                    Mprev = m0.to_broadcast([4, NB, 4])
                    tt("dve", R(7)[:, 64:80].unsqueeze(2), m0, v3(3)[:, :, 3:4], ALU.subtract, ["msb", ("row", 3)], ["wpr"])
                    npair = NB
                Lv = (lambda i: R(i)[:, 0:Lc]) if not samp else (lambda i: _rs(R(i)[:, 0:64], "p (b t) -> p b t", b=NB))
                tt("dve", Lv(4), Lv(0), Mend, ALU.subtract, [("row", 0), ("row", 3)], [("row", 4)])
                act(R(4)[:, 0:Lc], R(4)[:, 0:Lc], AF.Exp, [("row", 4)], [("row", 4)])
                tt("dve", R(5)[:, 0:Lc], R(2)[:, 0:Lc], R(3)[:, 0:Lc], ALU.add, [("row", 2), ("row", 3)], [("row", 5)])
                if samp:
                    cp("dve", msb[:, 32:48].unsqueeze(2), _rs(R(5)[:, 0:64], "p (b t) -> p b t", b=NB)[:, :, 3:4], [("row", 5)], ["mout"])
                    dma(om[:, l * 17 + 1:l * 17 + 17], msb[:, 32:48], ["mout"], [], "om_s")
                elif sc == 3 and tc == 3:
                    cp("dve", msb[:, 24:25], R(5)[:, 127:128], [("row", 5)], ["moutp"])
                    dma(om[:, l * 17:l * 17 + 1], msb[:, 24:25], ["moutp"], [], "om_p")
                act(R(5)[:, 0:Lc], R(5)[:, 0:Lc], AF.Exp, [("row", 5)], [("row", 5)], scale=-1.0)
                tt("dve", Lv(6), Mprev, Lv(3), ALU.subtract, [("row", 3), "carry", "msb"], [("row", 6)])
                act(R(6)[:, 0:Lc], R(6)[:, 0:Lc], AF.Exp, [("row", 6)], [("row", 6)])
                if not samp:
                    act(msb[:, 20:21], msb[:, 20:21], AF.Exp, ["wpr"], ["wpr"])
                    ts("dve", R(7)[:, 96:100], I4, msb[:, 20:21], None, ALU.mult, None, ["wpr", "consts"], ["wprx"])
                    ps, pk = PS()
                    mm(ps[:, 0:4], onesf[0:4, :], R(7)[:, 96:100], True, True, ["wprx", "consts"], [pk])
                    cp("dve", wprb[:, 0:4], ps[:, 0:4], [pk], ["wprb"])
                    cp("dve", msb[:, 16:17], R(2)[:, 127:128], [("row", 2)], ["carry"])
                    cp("dve", msb[:, 17:18], R(3)[:, 127:128], [("row", 3), ("row", 6), "wpr"], ["carry"])
                else:
                    act(R(7)[:, 64:80], R(7)[:, 64:80], AF.Exp, ["wpr"], ["wpr"])
                    wx = _rs(wTt[0:4, 0:64], "p (b h) -> p b h", b=NB)
                    tt("dve", wx, R(7)[:, 64:80].unsqueeze(2).to_broadcast([4, NB, 4]), I4.unsqueeze(1).to_broadcast([4, NB, 4]),
                       ALU.mult, ["wpr", "consts"], ["wprx"])
                    ps, pk = PS()
                    mm(ps[:, 0:64], onesf[0:4, :], wTt[0:4, 0:64], True, True, ["wprx", "consts"], [pk])
                    cp("dve", wprb[:, 0:64], ps[:, 0:64], [pk], ["wprb"])
                ps, pk = PS()
                for q, ri in enumerate((0, 4, 5, 6)):
                    mm(ps[0:Lc, q * 4:q * 4 + 4], R(ri)[:, 0:Lc], I4, True, True, [("row", ri), "consts"], [pk])
                cp("dve", gcol[0:Lc, :], ps[0:Lc, 0:16], [pk], ["gcol"])
                if samp:
                    for h in range(4):
                        pass
                for h in range(4):
                    psq, pkq = PS()
                    for dc in range(2):
                        mm(psq[0:Lc, 0:Lc], kTs[:, h * 2 + dc, lo:lo + Lc], qTs[:, h * 2 + dc, lo:lo + Lc], dc == 0, dc == 1,
                           ["qTs", "kTs", CT], [pkq])
                    psm, pkm = PS()
                    mm(psm[0:Lc, 0:Lc], selneg[:, h * 128:h * 128 + Lc], R(3)[:, 0:Lc], True, False, [("row", 3), "consts"], [pkm])
                    mm(psm[0:Lc, 0:Lc], ident[0:Lc, 0:Lc], (maskS if samp else maskP), False, True, ["consts"], [pkm])
                    act(wTt[0:Lc, 0:Lc] if not samp else wTt[0:Lc, 64:128], psm[0:Lc, 0:Lc], AF.Exp, [pkm, "gcol"], ["wTt", "wprx"],
                        bias=gcol[0:Lc, h:h + 1])
                    wsrc_ = wTt[0:Lc, 0:Lc] if not samp else wTt[0:Lc, 64:128]
                    tt("dve", smT[0:Lc, 0:Lc], psq[0:Lc, 0:Lc], wsrc_, ALU.mult, [pkq, "wTt"], ["smT"])
                    psw, pkw = PS()
                    mm(psw[:, 0:Lc], selpos[:, h * 128:(h + 1) * 128], R(6)[:, 0:Lc], True, True, [("row", 6), "consts"], [pkw])
                    for dc in range(2):
                        tt("dve", qsT[:, dc, 0:Lc], qTs[:, h * 2 + dc, lo:lo + Lc], psw[:, 0:Lc], ALU.mult, [pkw, "qTs", CT], ["qsT"])
                    if samp:
                        for dc in range(2):
                            tt("pool", qsTm[:, dc, :, :], qsT[:, dc, 0:64].unsqueeze(1).to_broadcast([128, NB, 64]),
                               _rs(mrowb[:, :], "p (b t) -> p b t", b=NB), ALU.mult, ["qsT", "mrowb"], [CT, "qsTm"])
                        ts("dve", wlm[0:64, :], maskcol, gcol[0:64, 4 + h:5 + h], None, ALU.mult, None, ["gcol", "consts"], ["wlm"])
                        cp("dve", wlmb[0:64, :], wlm[0:64, :], ["wlm"], ["wlmb"])
                    else:
                        cp("dve", wlmb[0:128, 0:1], gcol[0:128, 4 + h:5 + h], ["gcol"], ["wlmb"])
                    pso, pko = PSL()
                    mm(pso[0:Lc, 0:257], smT[0:Lc, 0:Lc], vtok[0:Lc, tc, h, 0:257], True, False, ["smT", "vtok", CT], [pko])
                    for b in range(nseq):
                        if samp:
                            s2 = (b + h * NB) % 2
                            pidx = 4 + b * 4 + h
                            dma(_rs(cts_f[:, s2, :], "p (c e) -> p c e", c=2), _rs(ctr_d[l, b, h], "(c p) e -> p c e", p=128),
                                [], [("ctsf", s2), CT], "ctsf%d" % s2)
                            dma(_rs(cns[:, s2, :], "p (c e) -> p c e", c=2), _rs(cnat_d[l, b, h], "(c p) e -> p c e", p=128),
                                [], [("cns", s2), CT], "cns%d" % s2)
                            ctv = _rs(cts[:, s2, :], "p (c e) -> p c e", c=2)
                            cp("act", ctv[:, :, 0:256], _rs(cts_f[:, s2, :], "p (c e) -> p c e", c=2), [("ctsf", s2)], [("cts", s2)])
                            cp("pool", ctv[:, :, 256:257], nT[:, :, pidx:pidx + 1], ["nT"], [("cts", s2)])
                            ctk = ("cts", s2)
                            cnv, cnk = cns[:, s2, :], ("cns", s2)
                            lhs_q = lambda dc, b=b: qsTm[:, dc, b, :]
                            qk_ = ["qsTm", CT]
                            wpc = wprb[:, b * 4 + h:b * 4 + h + 1]
                            wl_col = wlm[0:64, b:b + 1]
                            wl_colb = wlmb[0:64, b:b + 1]
                        else:
                            pidx = h
                            ctv = _rs(ctp[:, h, :], "p (c e) -> p c e", c=2)
                            ctk = "ctp"
                            cnv, cnk = cnp[:, h, :], "cnp"
                            lhs_q = lambda dc: qsT[:, dc, 0:128]
                            qk_ = ["qsT"]
                            wpc = wprb[:, h:h + 1]
                            wl_col = gcol[0:128, 4 + h:5 + h]
                            wl_colb = wlmb[0:128, 0:1]
                        for dc in range(2):
                            mm(pso[0:Lc, 0:257], lhs_q(dc), ctv[:, dc, 0:257], False, (b == nseq - 1 and dc == 1), qk_ + [ctk], [pko])
                        s3 = (b + h) % 2
                        ts("dve", vwb[0:Lc, s3, :], vtok[0:Lc, tc, h, 0:256], wl_col, None, ALU.mult, None, ["vtok", CT, "gcol", "wlm"], [("vwb", s3)])
                        psu, pku = PS()
                        for ec in range(2):
                            mm(psu[:, ec * 256:(ec + 1) * 256], vwb[0:Lc, s3, ec * 128:(ec + 1) * 128], ktok[0:Lc, tc, h * 256:(h + 1) * 256],
                               True, True, [("vwb", s3), "ktok", CT], [pku])
                        stt(cnv, cnv, wpc, psu[:, :], ALU.mult, ALU.add, [cnk, "wprb", pku], [cnk])
                        psn, pkn = PS()
                        for dc in range(2):
                            mm(psn[:, dc:dc + 1], ktok[0:Lc, tc, h * 256 + dc * 128:h * 256 + dc * 128 + 128], wl_colb, True, True,
                               ["ktok", CT, "wlmb"], [pkn])
                        stt(nT[:, :, pidx], nT[:, :, pidx], wpc, psn[:, 0:2], ALU.mult, ALU.add, ["nT", "wprb", pkn, ctk], ["nT"])
                        if samp:
                            dma(_rs(oC[l, 1 + b, h], "(c p) d -> p c d", p=128), _rs(cnv, "p (c d) -> p c d", c=2), [cnk], [], "oc%d" % s2)
                        else:
                            for dcc in range(2):
                                pst, pkt = PS()
                                for ec in range(2):
                                    P.add("pe", lambda e, o=pst[:, ec * 128:(ec + 1) * 128], i=cnp[:, h, ec * 256 + dcc * 128:ec * 256 + dcc * 128 + 128]:
                                          e.transpose(o, i, ident), r=["cnp", "consts"], w=[pkt])
                                cp("act", ctv[:, dcc, 0:256], pst[:, 0:256], [pkt], ["ctp"])
                            cp("pool", ctv[:, :, 256:257], nT[:, :, h:h + 1], ["nT"], ["ctp"])
                            if sc == 3 and tc == 3:
                                dma(_rs(oC[l, 0, h], "(c p) d -> p c d", p=128), _rs(cnv, "p (c d) -> p c d", c=2), [cnk], [], "ocp")
                    sm = smalls
                    ts("dve", sm[0:Lc, 8:9], pso[0:Lc, 256:257], -1.0, None, ALU.mult, None, [pko], ["ep0"])
                    tt("dve", sm[0:Lc, 8:9], sm[0:Lc, 8:9], pso[0:Lc, 256:257], ALU.max, [pko, "ep0"], ["ep0"])
                    tt("dve", sm[0:Lc, 8:9], sm[0:Lc, 8:9], gcol[0:Lc, 8 + h:9 + h], ALU.max, ["gcol", "ep0"], ["ep0"])
                    P.add("dve", lambda e, a=sm[0:Lc, 8:9]: e.reciprocal(a, a), r=["ep0"], w=["ep0"])
                    jk, jkk = T5()
                    act(jk[0:Lc, 0:256], pso[0:Lc, 0:256], AF.Square, [pko, "ep0"], [jkk, "ep1"], scale=sm[0:Lc, 8:9], accum=sm[0:Lc, 9:10])
                    act(sm[0:Lc, 10:11], sm[0:Lc, 9:10], AF.Sqrt, ["ep1"], ["ep2"], bias=EPS, scale=1.0 / 256)
                    P.add("dve", lambda e, a=sm[0:Lc, 10:11]: e.reciprocal(a, a), r=["ep2"], w=["ep2"])
                    tt("dve", sm[0:Lc, 11:12], sm[0:Lc, 10:11], sm[0:Lc, 8:9], ALU.mult, ["ep2", "ep0"], ["ep3"])
                    stt(vct[0:Lc, h * 256:(h + 1) * 256], pso[0:Lc, 0:256], sm[0:Lc, 11:12], gob[0:Lc, tc, h * 256:(h + 1) * 256],
                        ALU.mult, ALU.mult, [pko, "ep3", "gob", CT], [CT, "vct"])
                pst, pkt = PS()
                pstb = pst[:, :].bitcast(BF16)
                for c in range(8):
                    P.add("pe", lambda e, o=pstb[:, c * 128:c * 128 + Lc], i=vct[0:Lc, c * 128:(c + 1) * 128], idn=identb[0:Lc, 0:Lc]:
                          e.transpose(o, i, idn), r=["vct", CT, "constsb_i"], w=[pkt])
                cp("act", vcTs[:, :, lo:lo + Lc], _rs(pstb, "p (c t) -> p c t", c=8)[:, :, 0:Lc], [pkt], [CT, "vcTs"])
            merge_stage(False, C_GC, w_oc[l], 8, lambda kc, t0, n, t0s=t0s: vcTs[:, kc, t0 - t0s:t0 - t0s + n], [CT, "vcTs"], tiles_sc)
        dma(on[:, l, :], _rs(nT[:, :, :], "p c n -> p (c n)"), ["nT"], [], "on")
        fence()
        chk(l * 10 + 7)
        residual(w_o[l], 8, lambda kc, t0, n: big2[:, kc, t0:t0 + n], [("u", c) for c in range(8)])

        chk(l * 10 + 8)
        rmsnorm(xcur, xk, pc + PC_NX, hT_dst)
        fence()
        chk(l * 10 + 8.1)
        for c in range(8):
            wt, wk = wtile(w_xq[l], 0, 8, c * 128, 128)

            def cxq(ti, t0, n, ps, pk, c=c):
                act(big2[:, c, t0:t0 + n], ps[:, 0:n], AF.Copy, [pk], [("u", c)], scale=0.0625)
            proj(wt, wk, 0, hsrc, HK, 8, TT, cxq)
        chk(l * 10 + 8.2)
        for c in range(8):
            ts("dve", memTl[:, c, :], memTb[:, c, :], pcol[:, pc + PC_NMEM + c:pc + PC_NMEM + c + 1], None, ALU.mult, None,
               ["memTb", "pcol"], [CT, "memTl"])
        chk(l * 10 + 8.25)
        for kv in range(1 if _os.environ.get("DBG_KV0") else 2):
            for c in range(int(_os.environ.get("DBG_NC", "8"))):
                wt, wk = wtile(w_xkv[l], 0, 8, kv * D + c * 128, 128)
                if kv == 0 and not _os.environ.get("DBG_X1"):
                    ps, pk = PS()
                    for kc in range(8):
                        mm(ps[:, 0:256], wt[:, kc, :], memTl[:, kc, :], kc == 0, kc == 7, [wk, "memTl", CT], [pk])
                    cp("act", KTp[:, c, :], ps[:, 0:256], [pk], [CT, "KTp"])
                for mc in range(0 if _os.environ.get("DBG_X2") else 2):
                    ps, pk = PS()
                    for kc in range(8):
                        mm(ps[:, 0:128], memTl[:, kc, mc * 128:(mc + 1) * 128], wt[:, kc, :], kc == 0, kc == 7, [wk, "memTl", CT], [pk])
                    s4 = (c * 2 + mc) % 2
                    cp("dve", oms[:, s4, 0:128], ps[:, 0:128], [pk], [("oms", s4)])
                    dst = (omk if kv == 0 else omv)[l, mc * 128:(mc + 1) * 128, c * 128:(c + 1) * 128]
                    if not _os.environ.get("DBG_NOOM"):
                        dma(dst, oms[:, s4, 0:128], [("oms", s4)], [], "oms%d" % s4)
                    if kv == 1:
                        cp("act", Vp[:, mc, c * 128:(c + 1) * 128], oms[:, s4, 0:128], [("oms", s4)], [CT, "Vp"])
        chk(l * 10 + 8.3)
        QK = [("u", c) for c in range(8)]
        for tcx in range(16):
            ta = tcx * 128
            for h in range(4):
                ps, pk = PS()
                for dc in range(2):
                    mm(ps[:, 0:256], big2[:, h * 2 + dc, ta:ta + 128], KTp[:, h * 2 + dc, :], dc == 0, dc == 1, QK + ["KTp", CT], [pk])
                P.add("dve", lambda e, o=smalls[:, 12:13], i=ps[:, 0:256]: e.tensor_reduce(o, i, AX.X, ALU.max), r=[pk], w=["xa0"])
                ts("dve", smalls[:, 13:14], smalls[:, 12:13], -1.0, None, ALU.mult, None, ["xa0"], ["xa1"])
                act(pexp[:, 0:256], ps[:, 0:256], AF.Exp, [pk, "xa1"], [CT, "pexp", "xa2"], bias=smalls[:, 13:14], accum=smalls[:, 14:15])
                pst, pkt = PS()
                pstb = pst[:, :].bitcast(BF16)
                for mc in range(2):
                    P.add("pe", lambda e, o=pstb[:, mc * 128:(mc + 1) * 128], i=pexp[:, mc * 128:(mc + 1) * 128]:
                          e.transpose(o, i, identb[:, :]), r=["pexp", CT, "constsb_i"], w=[pkt])
                cp("act", pT[:, 0:256], pstb[:, 0:256], [pkt], [CT, "pT"])
                pso, pko = PS()
                for mc in range(2):
                    mm(pso[:, 0:256], pT[:, mc * 128:(mc + 1) * 128], Vp[:, mc, h * 256:(h + 1) * 256], mc == 0, mc == 1, ["pT", "Vp", CT], [pko])
                P.add("dve", lambda e, a=smalls[:, 15:16], i=smalls[:, 14:15]: e.reciprocal(a, i), r=["xa2"], w=["xa3"])
                ts("dve", aot[:, h * 256:(h + 1) * 256], pso[:, 0:256], smalls[:, 15:16], None, ALU.mult, None, [pko, "xa3"], ["aot"])
            pst, pkt = PS()
            pstb = pst[:, :].bitcast(BF16)
            for c in range(8):
                P.add("pe", lambda e, o=pstb[:, c * 128:(c + 1) * 128], i=aot[:, c * 128:(c + 1) * 128]:
                      e.transpose(o, i, identb[:, :]), r=["aot", "constsb_i"], w=[pkt])
            cp("act", aoT[:, :, ta:ta + 128], _rs(pstb, "p (c t) -> p c t", c=8), [pkt], [CT, "aoT"])
        chk(l * 10 + 8.4)
        for h in range(4):
            pss, pks = PSL()
            for dc in range(2):
                tt("pool", pTm[:, dc, :, :], big2[:, h * 2 + dc, PT:PT + 64].unsqueeze(1).to_broadcast([128, NB, 64]),
                   _rs(mrowb[:, :], "p (b t) -> p b t", b=NB), ALU.mult, QK + ["mrowb"], [CT, "pTm"])
            for b in range(NB):
                dma(_rs(kvst[:, 0:512], "p (c m) -> p c m", c=2), _rs(kT_d[l, b, h], "(c p) m -> p c m", p=128), [], [CT, "kvst"], "kvst")
                cp("pool", kvbf[:, 0:512], kvst[:, 0:512], ["kvst", CT], [CT, "kvbf"])
                for dc in range(2):
                    mm(pss[0:64, 0:256], pTm[:, dc, b, :], kvbf[:, dc * 256:(dc + 1) * 256], (b == 0 and dc == 0), (b == NB - 1 and dc == 1),
                       ["pTm", "kvbf", CT], [pks])
            P.add("dve", lambda e, o=smalls[0:64, 12:13], i=pss[0:64, 0:256]: e.tensor_reduce(o, i, AX.X, ALU.max), r=[pks], w=["xa0"])
            ts("dve", smalls[0:64, 13:14], smalls[0:64, 12:13], -1.0, None, ALU.mult, None, ["xa0"], ["xa1"])
            act(pexp[0:64, 0:256], pss[0:64, 0:256], AF.Exp, [pks, "xa1"], [CT, "pexp", "xa2"], bias=smalls[0:64, 13:14], accum=smalls[0:64, 14:15])
            pst, pkt = PS()
            pstb = pst[:, :].bitcast(BF16)
            for mc in range(2):
                P.add("pe", lambda e, o=pstb[:, mc * 64:(mc + 1) * 64], i=pexp[0:64, mc * 128:(mc + 1) * 128]:
                      e.transpose(o, i, identb[0:64, 0:64]), r=["pexp", CT, "constsb_i"], w=[pkt])
            cp("act", pT[:, 0:128], pstb[:, 0:128], [pkt], [CT, "pT"])
            for mc in range(2):
                tt("pool", pTm[:, mc, :, :], pT[:, mc * 64:(mc + 1) * 64].unsqueeze(1).to_broadcast([128, NB, 64]),
                   _rs(mrowb[:, :], "p (b t) -> p b t", b=NB), ALU.mult, ["pT", "mrowb", CT], [CT, "pTm"])
            pso, pko = PSL()
            for b in range(NB):
                dma(_rs(kvst[:, 0:512], "p (c e) -> p c e", c=2),
                    _rs(v_d[l, b, :, h * 256:(h + 1) * 256], "(c p) e -> p c e", p=128), [], [CT, "kvst"], "kvst")
                cp("pool", kvbf[:, 0:512], kvst[:, 0:512], ["kvst", CT], [CT, "kvbf"])
                for mc in range(2):
                    mm(pso[0:64, 0:256], pTm[:, mc, b, :], kvbf[:, mc * 256:(mc + 1) * 256], (b == 0 and mc == 0), (b == NB - 1 and mc == 1),
                       ["pTm", "kvbf", CT], [pko])
            P.add("dve", lambda e, a=smalls[0:64, 15:16], i=smalls[0:64, 14:15]: e.reciprocal(a, i), r=["xa2"], w=["xa3"])
            ts("dve", aot[0:64, h * 256:(h + 1) * 256], pso[0:64, 0:256], smalls[0:64, 15:16], None, ALU.mult, None, [pko, "xa3"], ["aot"])
        pst, pkt = PS()
        pstb = pst[:, :].bitcast(BF16)
        for c in range(8):
            P.add("pe", lambda e, o=pstb[:, c * 128:c * 128 + 64], i=aot[0:64, c * 128:(c + 1) * 128]:
                  e.transpose(o, i, identb[0:64, 0:64]), r=["aot", "constsb_i"], w=[pkt])
        cp("act", aoT[:, :, PT:PT + 64], _rs(pstb, "p (c t) -> p c t", c=8)[:, :, 0:64], [pkt], [CT, "aoT"])
        chk(l * 10 + 8.5)
        fence()
        residual(w_xo[l], 8, lambda kc, t0, n: aoT[:, kc, t0:t0 + n], [CT, "aoT"])
        fence()

        chk(l * 10 + 9)
        rmsnorm(xcur, xk, pc + PC_NFFN, hT_dst)
        fence()
        for g0, gn in ((0, 8), (8, 8), (16, 6)):
            for i in range(gn):
                hc = g0 + i
                wtg, wkg = wtile(w_fi[l], 0, 8, hc * 128, 128)
                wtu, wku = wtile(w_fi[l], 0, 8, DFF + hc * 128, 128)
                for ti, (t0, n) in enumerate(TT):
                    psG, pkG = PS()
                    for kc in range(8):
                        mm(psG[:, 0:n], wtg[:, kc, :], hsrc(kc, t0, n), kc == 0, kc == 7, [wkg] + HK, [pkG])
                    sg, sgk = T5()
                    act(sg[:, 0:n], psG[:, 0:n], AF.Silu, [pkG], [sgk])
                    psU, pkU = PS()
                    for kc in range(8):
                        mm(psU[:, 0:n], wtu[:, kc, :], hsrc(kc, t0, n), kc == 0, kc == 7, [wku] + HK, [pkU])
                    tt("dve", big2[:, i, t0:t0 + n], psU[:, 0:n], sg[:, 0:n], ALU.mult, [pkU, sgk], [("u", i)])
            nonlocal_src = w_fo[l][g0 * 128:(g0 + gn) * 128, :]
            residual(nonlocal_src, gn, lambda kc, t0, n: big2[:, kc, t0:t0 + n], [("u", c) for c in range(8)])

    stopped = False
    try:
        chk(0)
        layers()
    except _Stop:
        stopped = True
    P.epoch = L

    def y_dst(c, t0, n):
        return None
    for ti, (t0, n) in enumerate([] if stopped else TT):
        xin, xik = xin2[ti % 2], ("xin", ti % 2)
        dma(xin[:, :, 0:n], xcur[:, :, t0:t0 + n], [(xk, ti)], [xik], "xin%d" % (ti % 2))
        ps, pk = PS()
        for c in range(8):
            act(sqb[:, c % 2, 0:n], xin[:, c, 0:n], AF.Square, [xik], [("sqb", c % 2)])
            mm(ps[:, 0:n], onesb[:, :], sqb[:, c % 2, 0:n], c == 0, c == 7, [("sqb", c % 2)], [pk])
        rs, rk = T5()
        act(rs[:, 0:n], ps[:, 0:n], AF.Sqrt, [pk], [rk], bias=EPS, scale=1.0 / D)
        P.add("dve", lambda e, a=rs[:, 0:n]: e.reciprocal(a, a), r=[rk], w=[rk])
        for c in range(8):
            stt(xin[:, c, 0:n], xin[:, c, 0:n], pcol[:, PC_FIN + c:PC_FIN + c + 1], rs[:, 0:n], ALU.mult, ALU.mult,
                [xik, rk, "pcol"], [xik])
        dma(yT[:, :, t0:t0 + n], xin[:, :, 0:n], [xik], [], "yout%d" % (ti % 2))

    P.finalize()
    sem_names = set()
    for op in P.ops:
        if op.dma is not None:
            sem_names.add(("d", op.dma))
        elif op.signal:
            sem_names.add(("c", op.eng, op.epoch))
    sems = {}
    for i, s in enumerate(sorted(sem_names, key=str)):
        sems[s] = nc.semaphore("s%d" % i).__enter__()
    by_eng = {"pe": [], "act": [], "dve": [], "pool": [], "sp": []}
    for op in P.ops:
        by_eng[op.eng].append(op)
    final_waits = [(("d", k), v) for k, v in P.dma_cnt.items()]

    def run(e, ops, final=False):
        for op in ops:
            for s, v in op.waits:
                e.wait_ge(sems[s], v)
            ins = op.fn(e)
            if op.dma is not None:
                ins.then_inc(sems[("d", op.dma)], 16)
            elif op.signal:
                ins.then_inc(sems[("c", op.eng, op.epoch)], 1)
        if final:
            for s, v in final_waits:
                e.wait_ge(sems[s], v)

    with nc.allow_non_contiguous_dma(reason="small strided state rows"), nc.Block() as block:
        @block.sync
        def _(e):
            run(e, by_eng["sp"], final=True)

        @block.tensor
        def _(e):
            run(e, by_eng["pe"])

        @block.scalar
        def _(e):
            run(e, by_eng["act"])

        @block.vector
        def _(e):
            run(e, by_eng["dve"])

        @block.gpsimd
        def _(e):
            run(e, by_eng["pool"])
    return nc, len(P.ops)


def _consts():
    c = np.zeros((128, 1540), np.float32)
    c[:, 0:128] = np.eye(128)
    c[:, 128:256] = 1.0
    s = np.arange(128)
    c[:, 256:384] = np.where(s[None, :] >= s[:, None], 0.0, -30000.0)
    s6 = np.arange(64)
    same = (s6[:, None] // 4) == (s6[None, :] // 4)
    c[0:64, 384:448] = np.where(same & (s6[None, :] >= s6[:, None]), 0.0, -30000.0)
    c[0:64, 448:464] = (s6[:, None] // 4 == np.arange(16)[None, :]).astype(np.float32)
    c[0:4, 464:468] = np.eye(4)
    for h in range(4):
        c[h, 512 + h * 128:512 + (h + 1) * 128] = -1.0
        c[h, 1024 + h * 128:1024 + (h + 1) * 128] = 1.0
    mr = (np.arange(16)[:, None] == (s6[None, :] // 4)).astype(np.float32).reshape(-1)
    c[:, 1536] = 1.0
    c[:, 1537] = 0.0
    c[:, 1538] = -1.0
    return c, np.ascontiguousarray(np.broadcast_to(mr[None, :], (128, 1024))).astype(np.float32)


def _col(v):
    return np.ascontiguousarray(v.reshape(-1, 128).T)


_CACHE = {}


def _prep(inp):
    f = lambda k: np.asarray(inp[k], dtype=np.float32)
    cst, mrow = _consts()
    pcol = np.zeros((128, L * NPC), np.float32)
    for l in range(L):
        b = l * NPC
        pcol[:, b + PC_NMIX:b + PC_NMIX + 8] = _col(f("norm_mix_g")[l])
        pcol[:, b + PC_NX:b + PC_NX + 8] = _col(f("norm_x_g")[l])
        pcol[:, b + PC_NFFN:b + PC_NFFN + 8] = _col(f("norm_ffn_g")[l])
        pcol[:, b + PC_NMEM:b + PC_NMEM + 8] = _col(f("norm_mem_g")[l])
        pcol[:, b + PC_FIN:b + PC_FIN + 8] = _col(f("final_norm_g"))
        caw = f("conv_a_w")[l]
        cbw = f("conv_b_w")[l]
        for j in range(4):
            pcol[:, b + PC_CAW + j * 3:b + PC_CAW + (j + 1) * 3] = caw[:, j * 128:(j + 1) * 128].T
            pcol[:, b + PC_CBW + j * 31:b + PC_CBW + (j + 1) * 31] = cbw[:, j * 128:(j + 1) * 128].T
        pcol[:, b + PC_CBB:b + PC_CBB + 4] = _col(f("conv_b_b")[l])
        pcol[:, b + PC_LNG:b + PC_LNG + 4] = _col(f("ln_b_g")[l])
        pcol[:, b + PC_LNB:b + PC_LNB + 4] = _col(f("ln_b_b")[l])
    pcol[:, PC_FIN:PC_FIN + 8] = _col(f("final_norm_g"))
    gnorm = np.ascontiguousarray(np.broadcast_to(f("mlstm_norm_g")[:, None, :], (L, 128, D)))
    bif = np.ascontiguousarray(f("b_if").reshape(L, 2, 4).transpose(2, 0, 1).reshape(4, L * 2))
    xp, xs = f("x_prompt"), f("x_sample")
    shared = {k: f(k) for k in ("w_in", "w_out_a", "w_out_b", "w_out_c", "w_o", "w_xq", "w_xkv", "w_xo", "w_ffn_in", "w_ffn_out")}
    in_maps = []
    for i in range(8):
        bs = slice(i * NB, (i + 1) * NB)
        xt = np.concatenate([xp[i], xs[bs].reshape(ST, D)], axis=0)
        xT = np.ascontiguousarray(xt.T.reshape(8, 128, T).transpose(1, 0, 2))
        memtok = np.ascontiguousarray(f("mem_prompt")[i].reshape(2, 128, D).transpose(1, 0, 2))
        sca = f("state_conv_a")[:, bs]
        sca = np.ascontiguousarray(sca.reshape(L, NB, 2, 4, 128).transpose(4, 0, 3, 1, 2).reshape(128, -1))
        scb = f("state_conv_b")[:, bs]
        scb = np.ascontiguousarray(scb.reshape(L, NB, 30, 4, 128).transpose(4, 0, 3, 1, 2).reshape(128, -1))
        cn = np.ascontiguousarray(f("state_mlstm_c")[:, bs])
        ctr = np.ascontiguousarray(cn.transpose(0, 1, 2, 4, 3))
        nn = f("state_mlstm_n")[:, bs]
        nTi = np.ascontiguousarray(nn.reshape(L, NB, 4, 2, 128).transpose(4, 0, 3, 1, 2).reshape(128, -1))
        mi = np.ascontiguousarray(f("state_mlstm_m")[:, bs].transpose(2, 0, 1).reshape(4, -1))
        kTc = np.ascontiguousarray(f("cache_mem_k")[:, bs].transpose(0, 1, 3, 4, 2))
        vc = np.ascontiguousarray(f("cache_mem_v")[:, bs].reshape(L, NB, 256, D))
        m = {"xT_in": xT, "memtok": memtok, "pcol": pcol, "gnorm": gnorm, "bif": bif, "sca": sca, "scb": scb,
             "cnat": cn, "ctr": ctr, "nTin": nTi, "min": mi, "kTc": kTc, "vc": vc, "cst": cst, "mrow": mrow}
        m.update(shared)
        in_maps.append(m)
    return in_maps


def _post(R):
    y_p = np.zeros((8, PT, D), np.float32)
    y_s = np.zeros((128, 4, D), np.float32)
    p_a = np.zeros((L, 8, 2, 512), np.float32)
    p_b = np.zeros((L, 8, 30, 512), np.float32)
    p_c = np.zeros((L, 8, 4, 256, 256), np.float32)
    p_n = np.zeros((L, 8, 4, 256), np.float32)
    p_m = np.zeros((L, 8, 4), np.float32)
    p_k = np.zeros((L, 8, 256, 4, 256), np.float32)
    p_v = np.zeros((L, 8, 256, 4, 256), np.float32)
    s_a = np.zeros((L, 128, 2, 512), np.float32)
    s_b = np.zeros((L, 128, 30, 512), np.float32)
    s_c = np.zeros((L, 128, 4, 256, 256), np.float32)
    s_n = np.zeros((L, 128, 4, 256), np.float32)
    s_m = np.zeros((L, 128, 4), np.float32)
    for i in range(8):
        r = R[i]
        bs = slice(i * NB, (i + 1) * NB)
        yt = r["yT"].transpose(2, 1, 0).reshape(T, D)
        y_p[i] = yt[:PT]
        y_s[bs] = yt[PT:].reshape(NB, 4, D)
        a = r["oca"].reshape(128, L, 4, 17, 2).transpose(1, 3, 4, 2, 0).reshape(L, 17, 2, 512)
        p_a[:, i] = a[:, 0]
        s_a[:, bs] = a[:, 1:]
        b_ = r["ocb"].reshape(128, L, 4, 17, 30).transpose(1, 3, 4, 2, 0).reshape(L, 17, 30, 512)
        p_b[:, i] = b_[:, 0]
        s_b[:, bs] = b_[:, 1:]
        p_c[:, i] = r["oC"][:, 0]
        s_c[:, bs] = r["oC"][:, 1:]
        n_ = r["on"].reshape(128, L, 2, 68).transpose(1, 3, 2, 0).reshape(L, 68, 256)
        p_n[:, i] = n_[:, 0:4]
        s_n[:, bs] = n_[:, 4:].reshape(L, NB, 4, 256)
        m_ = r["om"].reshape(4, L, 17).transpose(1, 2, 0)
        p_m[:, i] = m_[:, 0]
        s_m[:, bs] = m_[:, 1:]
        p_k[:, i] = r["omk"].reshape(L, 256, 4, 256)
        p_v[:, i] = r["omv"].reshape(L, 256, 4, 256)
    return (y_p, y_s, p_a, p_b, p_c, p_n, p_m, p_k, p_v, s_a, s_b, s_c, s_n, s_m)


def kernel(**inp):
    if "nc" not in _CACHE:
        _CACHE["nc"] = build_nc()
    nc, nops = _CACHE["nc"]
    in_maps = _prep(inp)
    res = run_bass_kernel_spmd(nc, in_maps, core_ids=list(range(8)))
    return _post(res.results)
```

```python
import os as _os
import numpy as np
import concourse.bass as bass
import concourse.mybir as mybir
from concourse.bass_utils import run_bass_kernel_spmd

F32 = mybir.dt.float32
BF16 = mybir.dt.bfloat16
AF = mybir.ActivationFunctionType
ALU = mybir.AluOpType
AX = mybir.AxisListType

L = 4
D = 1024
T = 2112
PT = 2048
ST = 64
NB = 16
TT = [(0, 512), (512, 512), (1024, 512), (1536, 512), (2048, 64)]
INW = 9736
DFF = 2816
EPS = 1e-6
C_AB, C_AC, C_AX, C_BV, C_BG = 0, 512, 1024, 1536, 2048
C_Q, C_K, C_V, C_O, C_IF = 2560, 3584, 4608, 5632, 6656
C_GA, C_GB, C_GC = 6664, 7688, 8712
PC_NMIX, PC_NX, PC_NFFN, PC_NMEM, PC_FIN = 0, 8, 16, 24, 32
PC_CAW, PC_CBW, PC_CBB, PC_LNG, PC_LNB = 40, 52, 176, 180, 184
NPC = 188


_CTKEYS = {"ctmp", "memtS", "memn", "mrowst", "xin", "vab", "abuf", "ucv", "dg", "scbS", "bconv", "qTs", "kTs", "ktok",
           "vtok", "gob", "vct", "qsTm", "vcTs", "aoT", "kvst", "kvbf", "pTm", "KTp", "Vp", "memTl", "pexp", "pT", "cns", "ctsf", "xb"}


class Op:
    __slots__ = ("eng", "fn", "deps", "dma", "dval", "signal", "sigval", "waits", "epoch", "idx")


class Prog:
    def __init__(self):
        self.ops = []
        self.lastw = {}
        self.rd_eng = {}
        self.rd_dma = {}
        self.dma_cnt = {}
        self.epoch = 0

    def add(self, eng, fn, r=(), w=(), dma=None, fence=False):
        if not fence:
            def _isct(k):
                n = k[0] if isinstance(k, tuple) else k
                return n in _CTKEYS
            if any(_isct(k) for k in list(r) + list(w)):
                r = [k for k in r if k != "ctmp"] + ["ctmp"]
                w = [k for k in w if k != "ctmp"]
        op = Op()
        op.eng, op.fn, op.dma, op.epoch = eng, fn, dma, self.epoch
        op.idx = len(self.ops)
        op.signal = False
        op.sigval = 0
        op.dval = 0
        deps = set()
        for k in r:
            if k in self.lastw:
                deps.add(self.lastw[k])
        for k in w:
            if k in self.lastw:
                deps.add(self.lastw[k])
            for e, i in self.rd_eng.get(k, {}).items():
                deps.add(i)
            for i in self.rd_dma.get(k, ()):
                deps.add(i)
        for k in r:
            if dma is not None:
                self.rd_dma.setdefault(k, []).append(op.idx)
            else:
                self.rd_eng.setdefault(k, {})[eng] = op.idx
        for k in w:
            self.lastw[k] = op.idx
            self.rd_eng[k] = {}
            self.rd_dma[k] = []
        if dma is not None:
            c = self.dma_cnt.get(dma, 0) + 16
            self.dma_cnt[dma] = c
            op.dval = c
        deps.discard(op.idx)
        op.deps = deps
        self.ops.append(op)
        return op

    def finalize(self):
        ops = self.ops
        for op in ops:
            for d in op.deps:
                y = ops[d]
                if y.dma is None and not (y.eng == op.eng and op.dma is None and op.eng == "pe"):
                    y.signal = True
        cnt = {}
        for op in ops:
            if op.signal:
                k = (op.eng, op.epoch)
                cnt[k] = cnt.get(k, 0) + 1
                op.sigval = cnt[k]
        waited = {}
        for op in ops:
            need = {}
            for d in op.deps:
                y = ops[d]
                if y.dma is not None:
                    s, v = ("d", y.dma), y.dval
                elif y.eng == op.eng and op.dma is None and op.eng == "pe":
                    continue
                else:
                    s, v = ("c", y.eng, y.epoch), y.sigval
                if need.get(s, 0) < v:
                    need[s] = v
            ws = []
            wd = waited.setdefault(op.eng, {})
            for s, v in need.items():
                if wd.get(s, 0) < v:
                    wd[s] = v
                    ws.append((s, v))
            op.waits = ws
        return cnt


def _rs(a, pat, **kw):
    return a.rearrange(pat, **kw)


class _Stop(Exception):
    pass


def build_nc(stop=10 ** 9):
    def chk(i):
        if i >= stop:
            raise _Stop()
    nc = bass.Bass("TRN2", target_bir_lowering=False)
    P = Prog()

    def din(name, shape):
        return nc.dram_tensor(name, list(shape), F32, kind="ExternalInput").ap()

    def dout(name, shape):
        return nc.dram_tensor(name, list(shape), F32, kind="ExternalOutput").ap()

    xT_in = din("xT_in", [128, 8, T])
    memtok = din("memtok", [128, 2, D])
    pcol_d = din("pcol", [128, L * NPC])
    gnorm_d = din("gnorm", [L, 128, D])
    bif_d = din("bif", [4, L * 2])
    sca_d = din("sca", [128, L * 4 * NB * 2])
    scb_d = din("scb", [128, L * 4 * NB * 30])
    cnat_d = din("cnat", [L, NB, 4, 256, 256])
    ctr_d = din("ctr", [L, NB, 4, 256, 256])
    nT_d = din("nTin", [128, L * 2 * 64])
    m_d = din("min", [4, L * NB])
    kT_d = din("kTc", [L, NB, 4, 256, 256])
    v_d = din("vc", [L, NB, 256, D])
    cst_d = din("cst", [128, 1540])
    mrow_d = din("mrow", [128, 1024])
    w_in = din("w_in", [L, D, INW])
    w_oa = din("w_out_a", [L, 512, D])
    w_ob = din("w_out_b", [L, 512, D])
    w_oc = din("w_out_c", [L, D, D])
    w_o = din("w_o", [L, D, D])
    w_xq = din("w_xq", [L, D, D])
    w_xkv = din("w_xkv", [L, D, 2 * D])
    w_xo = din("w_xo", [L, D, D])
    w_fi = din("w_ffn_in", [L, D, 2 * DFF])
    w_fo = din("w_ffn_out", [L, DFF, D])

    yT = dout("yT", [128, 8, T])
    oca = dout("oca", [128, L * 4, 17 * 2])
    ocb = dout("ocb", [128, L * 4, 17 * 30])
    oC = dout("oC", [L, 17, 4, 256, 256])
    on = dout("on", [128, L, 2 * 68])
    om = dout("om", [4, L * 17])
    omk = dout("omk", [L, 256, D])
    omv = dout("omv", [L, 256, D])
    xsA = nc.dram_tensor("xsA", [128, 8, T], F32).ap()
    xsB = nc.dram_tensor("xsB", [128, 8, T], F32).ap()

    def sb(name, shape, dt=F32):
        return nc.alloc_sbuf_tensor(name, list(shape), dt) if False else nc.sbuf_tensor(name, list(shape), dt).__enter__()

    hT = sb("hT", [128, 8, T], BF16)
    big2 = sb("big2", [128, 8, T], BF16)
    ctmp = sb("ctmp", [128, 16384], F32)
    wst = sb("wst", [128, 2, 1024], F32)
    wbf = sb("wbf", [128, 2, 1024], BF16)
    cst = sb("cstsb", [128, 1540], F32)
    pcol = sb("pcolsb", [128, L * NPC], F32)
    gnb = sb("gnb", [128, D], F32)
    sqb = sb("sqb", [128, 2, 512], BF16)
    t512 = sb("t512", [128, 4, 512], F32)
    rows = sb("rows", [4, 8, 128], F32)
    bif = sb("bifsb", [4, L * 2], F32)
    gcol = sb("gcol", [128, 16], F32)
    smalls = sb("smalls", [128, 32], F32)
    wprb = sb("wprb", [128, 64], F32)
    wTt4 = sb("wTt4", [128, 4, 128], F32)
    smT4 = sb("smT4", [128, 4, 128], BF16)
    qsT4 = sb("qsT4", [128, 4, 256], BF16)
    wxs = sb("wxs", [4, 64], F32)
    eps4 = sb("eps4", [128, 16], F32)
    cnp = sb("cnp", [128, 4, 512], F32)
    lnbuf = cnp[:, 0:2, :]
    ctp = sb("ctp", [128, 4, 2 * 258], BF16)
    cts = sb("cts", [128, 2, 2 * 258], BF16)
    nT = sb("nT", [128, 2, 68], F32)
    msb = sb("msb", [4, 64], F32)
    vwb4 = sb("vwb4", [128, 4, 256], BF16)
    wlm4 = sb("wlm4", [128, 4, 16], F32)
    wlmb4 = sb("wlmb4", [128, 4, 16], BF16)
    ostA = sb("ostA", [128, 34], F32)
    ostB = sb("ostB", [128, 17 * 30], F32)
    scaS = sb("scaS", [128, 4 * NB * 2], F32)
    oms = sb("oms", [128, 2, 256], F32)
    identb = sb("identb", [128, 128], BF16)
    onesb = sb("onesb", [128, 128], BF16)
    mrowb = sb("mrowb", [128, NB * 64], BF16)

    ident = cst[:, 0:128]
    onesf = cst[:, 128:256]
    maskP = cst[:, 256:384]
    maskS = cst[0:64, 384:448]
    maskcol = cst[0:64, 448:464]
    I4 = cst[0:4, 464:468]
    selneg = cst[0:4, 512:1024]
    selpos = cst[0:4, 1024:1536]

    def cv(off, nbytes, dt):
        a = ctmp[:, off // 4:(off + nbytes) // 4]
        return a.bitcast(dt) if dt != F32 else a

    xin2 = [_rs(cv(0, 16384, F32), "p (c t) -> p c t", c=8), _rs(cv(16384, 16384, F32), "p (c t) -> p c t", c=8)]
    xbufs = [cv(33792, 8448, F32), cv(42240, 8448, F32)]
    mrow_st = cv(16384, 4096, F32)
    cns = _rs(cv(55360, 4096, F32), "p (s n) -> p s n", s=2)
    cts_f = _rs(cv(59456, 4096, F32), "p (s n) -> p s n", s=2)
    vab = _rs(cv(0, 16896, BF16), "p (c t) -> p c t", c=4)
    abuf = cv(16896, 4224, BF16)
    ucv = [cv(21120, 5248, BF16), cv(26368, 5248, BF16)]
    dg = _rs(cv(31616, 7936, BF16), "p (k n) -> p k n", k=31)
    scbS = cv(39552, 7680, F32)
    bconv = _rs(cv(47232, 16896, BF16), "p (c t) -> p c t", c=4)
    qTs = _rs(cv(0, 8192, BF16), "p (c t) -> p c t", c=8)
    kTs = _rs(cv(8192, 8192, BF16), "p (c t) -> p c t", c=8)
    ktok = _rs(cv(16384, 8192, BF16), "p (c n) -> p c n", c=4)
    vtok = _rs(cv(24576, 8256, BF16), "p (c h n) -> p c h n", c=4, h=4)
    gob = _rs(cv(32832, 8192, BF16), "p (c n) -> p c n", c=4)
    vct = cv(41024, 2048, BF16)
    qsTm = _rs(cv(43072, 4096, BF16), "p (c b t) -> p c b t", c=2, b=NB)
    vcTs = _rs(cv(47168, 8192, BF16), "p (c t) -> p c t", c=8)
    aoT = _rs(cv(0, 33792, BF16), "p (c t) -> p c t", c=8)
    kvst = cv(33792, 8192, F32)
    kvbf = cv(41984, 4096, BF16)
    pTm = _rs(cv(46080, 4096, BF16), "p (c b t) -> p c b t", c=2, b=NB)
    KTp = _rs(cv(50176, 4096, BF16), "p (c m) -> p c m", c=8)
    Vp = _rs(cv(54272, 4096, BF16), "p (c n) -> p c n", c=2)
    memTl = _rs(cv(58368, 4096, BF16), "p (c m) -> p c m", c=8)
    pexp = cv(62464, 2048, BF16)
    pT = cv(64512, 1024, BF16)
    memtS = _rs(cv(0, 8192, F32), "p (c n) -> p c n", c=2)
    memn = _rs(cv(8192, 8192, F32), "p (c n) -> p c n", c=2)
    memTb = sb("memTb", [128, 8, 256], BF16)
    aot = sb("aot", [128, 1024], BF16)

    psb = [nc.psum_tensor("ps%d" % i, [128, 512], F32).__enter__() for i in range(8)]
    ps_ctr = [0]

    ps_nb = [6]

    def PS():
        i = ps_ctr[0] % ps_nb[0]
        ps_ctr[0] += 1
        return psb[i], ("ps", i)

    psl_ctr = [0]

    def PSL():
        if ps_nb[0] == 6:
            i = 6 + psl_ctr[0] % 2
        else:
            i = 4 + psl_ctr[0] % 4
        psl_ctr[0] += 1
        return psb[i], ("ps", i)

    t5_ctr = [0]

    def T5():
        i = t5_ctr[0] % 4
        t5_ctr[0] += 1
        return t512[:, i, :], ("t5", i)

    CT = "ctmp"

    def fence():
        P.add("pool", lambda e: e.memset(smalls[:, 31:32], 0.0), r=[], w=[CT, "fencebyte"], fence=True)

    w_ctr = [0]

    def wtile(src2d, r0, nkc, c0, ncols):
        s = w_ctr[0] % 2
        w_ctr[0] += 1
        n = nkc * ncols
        src = _rs(src2d[r0:r0 + nkc * 128, c0:c0 + ncols], "(kc p) n -> p kc n", p=128)
        dst = _rs(wst[:, s, 0:n], "p (kc n) -> p kc n", kc=nkc)
        P.add("sp", lambda e: e.dma_start(out=dst, in_=src), r=[], w=[("wst", s)], dma="ws%d_%d" % (s, P.epoch))
        P.add("pool", lambda e: e.tensor_copy(out=wbf[:, s, 0:n], in_=wst[:, s, 0:n]), r=[("wst", s)], w=[("wbf", s)])
        return _rs(wbf[:, s, 0:n], "p (kc n) -> p kc n", kc=nkc), ("wbf", s)

    def mm(out, lhsT, rhs, start, stop, r, w):
        P.add("pe", lambda e: e.matmul(out, lhsT=lhsT, rhs=rhs, start=start, stop=stop), r=r, w=w)

    def proj(wt, wk, col0, src, srck, nkc, tiles, consume):
        for ti, (t0, n) in enumerate(tiles):
            ps, pk = PS()
            for kc in range(nkc):
                mm(ps[:, 0:n], wt[:, kc, col0:col0 + 128], src(kc, t0, n), kc == 0, kc == nkc - 1,
                   [wk] + srck, [pk])
            consume(ti, t0, n, ps, pk)

    def act(out, in_, func, r, w, bias=0.0, scale=1.0, accum=None):
        if accum is None:
            P.add("act", lambda e: e.activation(out, in_, func, bias=bias, scale=scale), r=r, w=w)
        else:
            P.add("act", lambda e: e.activation(out, in_, func, bias=bias, scale=scale, accum_out=accum), r=r, w=w)

    def tt(eng, out, a, b, op, r, w):
        P.add(eng, lambda e: e.tensor_tensor(out, a, b, op), r=r, w=w)

    def stt(out, a, s, b, op0, op1, r, w):
        P.add("dve", lambda e: e.scalar_tensor_tensor(out, a, s, b, op0, op1), r=r, w=w)

    def ts(eng, out, a, s1, s2, op0, op1, r, w):
        if s2 is None:
            P.add(eng, lambda e: e.tensor_scalar(out, a, s1, None, op0), r=r, w=w)
        else:
            P.add(eng, lambda e: e.tensor_scalar(out, a, s1, s2, op0, op1), r=r, w=w)

    def cp(eng, out, a, r, w):
        if eng == "act":
            P.add("act", lambda e: e.activation(out, a, AF.Copy), r=r, w=w)
        else:
            P.add(eng, lambda e: e.tensor_copy(out=out, in_=a), r=r, w=w)

    def dma(out, in_, r, w, sem):
        P.add("sp", lambda e: e.dma_start(out=out, in_=in_), r=r, w=w, dma=sem)

    HK = [("hT", c) for c in range(8)]

    def rmsnorm(xsrc, xsk, gcolbase, dst_fn):
        for ti, (t0, n) in enumerate(TT):
            xin, xik = xin2[ti % 2], ("xin", ti % 2)
            dma(xin[:, :, 0:n], xsrc[:, :, t0:t0 + n], [(xsk, ti)], [xik], "xin%d" % (ti % 2))
            ps, pk = PS()
            for c in range(8):
                act(sqb[:, c % 2, 0:n], xin[:, c, 0:n], AF.Square, [xik], [("sqb", c % 2)])
                mm(ps[:, 0:n], onesb[:, :], sqb[:, c % 2, 0:n], c == 0, c == 7, [("sqb", c % 2), "constsb_o"], [pk])
            rs, rk = T5()
            act(rs[:, 0:n], ps[:, 0:n], AF.Sqrt, [pk], [rk], bias=EPS, scale=1.0 / D)
            P.add("dve", lambda e, a=rs[:, 0:n]: e.reciprocal(a, a), r=[rk], w=[rk])
            for c in range(8):
                o, ok = dst_fn(c, t0, n)
                stt(o, xin[:, c, 0:n], pcol[:, gcolbase + c:gcolbase + c + 1], rs[:, 0:n], ALU.mult, ALU.mult,
                    [xik, rk, "pcol"], ok)

    def hT_dst(c, t0, n):
        return hT[:, c, t0:t0 + n], [("hT", c)]

    def hsrc(kc, t0, n):
        return hT[:, kc, t0:t0 + n]

    dma(cst[:, :], cst_d, [], ["consts"], "init1")
    dma(pcol[:, :], pcol_d, [], ["pcol"], "init2")
    dma(bif[:, :], bif_d, [], ["bif"], "init3")
    dma(memtS, memtok, [], [CT, "memtS"], "init4")
    cp("dve", identb[:, :], ident, ["consts"], ["constsb_i"])
    cp("dve", onesb[:, :], onesf, ["consts"], ["constsb_o"])
    dma(mrow_st, mrow_d, [], [CT, "mrowst"], "init5")
    cp("dve", mrowb[:, :], mrow_st, ["mrowst", CT], ["mrowb"])
    P.add("pool", lambda e: e.memset(_rs(vtok, "p c h n -> p (c h) n")[:, :, 256:258], 1.0), r=[], w=[CT])
    for mc in range(2):
        jk, jkk = T5()
        act(jk[:, 0:512], memtS[:, mc, 0:512], AF.Square, [CT, "memtS"], [jkk], accum=smalls[:, mc * 2:mc * 2 + 1])
        jk2, jkk2 = T5()
        act(jk2[:, 0:512], memtS[:, mc, 512:1024], AF.Square, [CT, "memtS"], [jkk2, "sm0"], accum=smalls[:, mc * 2 + 1:mc * 2 + 2])
        tt("dve", smalls[:, 4 + mc:5 + mc], smalls[:, mc * 2:mc * 2 + 1], smalls[:, mc * 2 + 1:mc * 2 + 2], ALU.add,
           [jkk, jkk2, "sm0"], ["sm1"])
        act(smalls[:, 6 + mc:7 + mc], smalls[:, 4 + mc:5 + mc], AF.Sqrt, ["sm1"], ["sm2"], bias=EPS, scale=1.0 / D)
        P.add("dve", lambda e, a=smalls[:, 6 + mc:7 + mc]: e.reciprocal(a, a), r=["sm2"], w=["sm3"])
        ts("dve", memn[:, mc, :], memtS[:, mc, :], smalls[:, 6 + mc:7 + mc], None, ALU.mult, None, ["sm3", CT, "memtS"], [CT, "memn"])
        for g in range(2):
            ps, pk = PS()
            for q in range(4):
                c = g * 4 + q
                P.add("pe", lambda e, o=ps[:, q * 128:(q + 1) * 128], i=memn[:, mc, c * 128:(c + 1) * 128]:
                      e.transpose(o, i, ident), r=["memn", "consts"], w=[pk])
            cp("dve", memTb[:, g * 4:(g + 1) * 4, mc * 128:(mc + 1) * 128],
               _rs(ps[:, :], "p (q t) -> p q t", q=4), [pk], ["memTb"])
    P.add("pool", lambda e: e.memset(nT[:, :, 0:4], 0.0), r=[], w=["nT"])
    fence()

    xcur, xk = xT_in, "x0"
    xdsts = [(xsA, "xA"), (xsB, "xB")]
    xflip = [0]

    xb_ctr = [0]

    def residual(wsrc2d, nk, srcfn, srckeys):
        nonlocal xcur, xk
        xn, xnk = xdsts[xflip[0] % 2]
        xflip[0] += 1
        xsrc_, xsk_ = xcur, xk

        def pre(j):
            wt, wk = wtile(wsrc2d, 0, nk, j * 128, 128)
            i = xb_ctr[0] % 2
            xb_ctr[0] += 1
            dma(xbufs[i][:, :], xsrc_[:, j, :], [(xsk_, t) for t in range(5)], [("xb", i)], "xbl%d" % i)
            return wt, wk, i
        nxt = pre(0)
        for j in range(8):
            wt, wk, i = nxt
            if j + 1 < 8:
                nxt = pre(j + 1)

            def consume(ti, t0, n, ps, pk, i=i):
                tt("dve", xbufs[i][:, t0:t0 + n], ps[:, 0:n], xbufs[i][:, t0:t0 + n], ALU.add, [pk, ("xb", i)], [("xb", i)])
            proj(wt, wk, 0, srcfn, srckeys, nk, TT, consume)
            dma(xn[:, j, :], xbufs[i][:, :], [("xb", i)], [(xnk, t) for t in range(5)], "xbs%d" % i)
        xcur, xk = xn, xnk

    def layers():
      for l in range(L):
        layer(l)

    def layer(l):
        nonlocal xcur, xk
        P.epoch = l
        pc = l * NPC
        Win = w_in[l]
        chk(l * 10 + 1)
        rmsnorm(xcur, xk, pc + PC_NMIX, hT_dst)
        fence()
        dma(gnb[:, :], gnorm_d[l], [], ["gnb"], "ld0_1")
        dma(scaS[:, :], sca_d[:, l * 128:(l + 1) * 128], [], ["scaS"], "ld0_2")
        dma(scbS, scb_d[:, l * 1920:(l + 1) * 1920], [], [CT, "scbS"], "ld0_3")
        dma(nT[:, :, 4:68], _rs(nT_d[:, l * 128:(l + 1) * 128], "p (c n) -> p c n", c=2), [], ["nT"], "ld0_4")
        dma(msb[:, 0:16], m_d[:, l * NB:(l + 1) * NB], [], ["msb"], "ld0_5")
        wg, wgk0 = None, None

        def conv_stage(K, colbase, halo_src, j, ub, ubk, consume):
            wv = pcol[:, colbase + j * K:colbase + (j + 1) * K]
            P.add("pool", lambda e: e.tensor_tensor(dg[:, 0:K, :], ident.unsqueeze(1).to_broadcast([128, K, 128]),
                                                    wv.unsqueeze(2).to_broadcast([128, K, 128]), ALU.mult),
                  r=["pcol", "consts"], w=[CT, "dg"])
            for ti, (t0, n) in enumerate(TT):
                ps, pk = PS()
                for k in range(K):
                    off = 30 - (K - 1) + k
                    if ti < 4:
                        rhs = ub[:, t0 + off:t0 + off + n]
                    else:
                        rhs = _rs(ub[:, 2078:2078 + NB * 34], "p (b t) -> p b t", b=NB)[:, :, off:off + 4]
                    mm(ps[:, 0:n], dg[:, k, :], rhs, k == 0, k == K - 1, ["dg", ubk, CT], [pk])
                consume(ti, t0, n, ps, pk)

        def ustage(colP, colG, gfunc, j, ub, ubk, halo_view, Kh, tailout):
            wtG, wkG = wtile(Win, 0, 8, colG + j * 128, 128)
            wtP, wkP = wtile(Win, 0, 8, colP + j * 128, 128)
            ubs = _rs(ub[:, 2078:2078 + NB * 34], "p (b t) -> p b t", b=NB)
            cp("pool", ubs[:, :, 30 - Kh:30], halo_view, ["scaS", "scbS", CT], [ubk, CT])
            for ti, (t0, n) in enumerate(TT):
                psG, pkG = PS()
                for kc in range(8):
                    mm(psG[:, 0:n], wtG[:, kc, :], hsrc(kc, t0, n), kc == 0, kc == 7, [wkG] + HK, [pkG])
                tg, tgk = T5()
                act(tg[:, 0:n], psG[:, 0:n], gfunc, [pkG], [tgk])
                psP, pkP = PS()
                for kc in range(8):
                    mm(psP[:, 0:n], wtP[:, kc, :], hsrc(kc, t0, n), kc == 0, kc == 7, [wkP] + HK, [pkP])
                if ti < 4:
                    tt("dve", ub[:, 30 + t0:30 + t0 + n], psP[:, 0:n], tg[:, 0:n], ALU.mult, [pkP, tgk], [ubk, CT])
                    if ti == 3:
                        tt("dve", tailout[:, 0:Kh], psP[:, 512 - Kh:512], tg[:, 512 - Kh:512], ALU.mult,
                           [pkP, tgk], ["ost"])
                else:
                    tt("dve", ubs[:, :, 30:34], _rs(psP[:, 0:64], "p (b t) -> p b t", b=NB),
                       _rs(tg[:, 0:64], "p (b t) -> p b t", b=NB), ALU.mult, [pkP, tgk], [ubk, CT])
                    so = _rs(tailout[:, Kh:17 * Kh], "p (b t) -> p b t", b=NB)
                    if Kh >= 4:
                        tt("dve", so[:, :, Kh - 4:Kh], _rs(psP[:, 0:64], "p (b t) -> p b t", b=NB),
                           _rs(tg[:, 0:64], "p (b t) -> p b t", b=NB), ALU.mult, [pkP, tgk], ["ost"])
                    else:
                        tt("dve", so[:, :, 0:Kh], _rs(psP[:, 0:64], "p (b t) -> p b t", b=NB)[:, :, 4 - Kh:4],
                           _rs(tg[:, 0:64], "p (b t) -> p b t", b=NB)[:, :, 4 - Kh:4], ALU.mult, [pkP, tgk], ["ost"])

        chk(l * 10 + 2)
        P.add("pool", lambda e: e.memset(ucv[0][:, 0:30], 0.0), r=[], w=[CT, ("ucv", 0)])
        P.add("pool", lambda e: e.memset(ucv[1][:, 0:30], 0.0), r=[], w=[CT, ("ucv", 1)])
        for j in range(4):
            ub, ubk = ucv[j % 2], ("ucv", j % 2)
            hv = _rs(scaS[:, j * 32:(j + 1) * 32], "p (b t) -> p b t", b=NB)
            ustage(C_AX, C_AC, AF.Copy, j, ub, ubk, hv, 2, ostA)
            dma(oca[:, l * 4 + j, :], ostA[:, :], ["ost"], [], "oca")
            wtB, wkB = wtile(Win, 0, 8, C_AB + j * 128, 128)
            for ti, (t0, n) in enumerate(TT):
                psb_, pkb = PS()
                for kc in range(8):
                    mm(psb_[:, 0:n], wtB[:, kc, :], hsrc(kc, t0, n), kc == 0, kc == 7, [wkB] + HK, [pkb])
                cp("act", abuf[:, t0:t0 + n], psb_[:, 0:n], [pkb], [CT, "abuf"])

            def consA(ti, t0, n, ps, pk, j=j):
                tt("dve", vab[:, j, t0:t0 + n], ps[:, 0:n], abuf[:, t0:t0 + n], ALU.mult, [pk, "abuf"], [CT, ("vab", j)])
            conv_stage(3, pc + PC_CAW, None, j, ub, ubk, consA)

        def merge_stage(first, gcol0, wsrc2d, nk, srcfn, srckeys, tiles):
            for j in range(8):
                wtG, wkG = wtile(Win, 0, 8, gcol0 + j * 128, 128)
                wtY, wkY = wtile(wsrc2d, 0, nk, j * 128, 128)
                for ti, (t0, n) in enumerate(tiles):
                    psG, pkG = PS()
                    for kc in range(8):
                        mm(psG[:, 0:n], wtG[:, kc, :], hsrc(kc, t0, n), kc == 0, kc == 7, [wkG] + HK, [pkG])
                    sg, sgk = T5()
                    act(sg[:, 0:n], psG[:, 0:n], AF.Sigmoid, [pkG], [sgk])
                    psY, pkY = PS()
                    for kc in range(nk):
                        mm(psY[:, 0:n], wtY[:, kc, :], srcfn(kc, t0, n), kc == 0, kc == nk - 1, [wkY] + srckeys, [pkY])
                    if first:
                        tt("dve", big2[:, j, t0:t0 + n], psY[:, 0:n], sg[:, 0:n], ALU.mult, [pkY, sgk], [("u", j)])
                    else:
                        tt("dve", sg[:, 0:n], psY[:, 0:n], sg[:, 0:n], ALU.mult, [pkY, sgk], [sgk])
                        tt("pool", big2[:, j, t0:t0 + n], big2[:, j, t0:t0 + n], sg[:, 0:n], ALU.add, [sgk, ("u", j)], [("u", j)])

        chk(l * 10 + 3)
        merge_stage(True, C_GA, w_oa[l], 4, lambda kc, t0, n: vab[:, kc, t0:t0 + n], [CT] + [("vab", c) for c in range(4)], TT)

        chk(l * 10 + 4)
        for j in range(4):
            ub, ubk = ucv[j % 2], ("ucv", j % 2)
            hv = _rs(scbS[:, j * 480:(j + 1) * 480], "p (b t) -> p b t", b=NB)
            cp("pool", _rs(ostB[:, 30:510], "p (b t) -> p b t", b=NB)[:, :, 0:26], hv[:, :, 4:30], ["scbS", CT], ["ost"])
            ustage(C_BV, C_BG, AF.Sigmoid, j, ub, ubk, hv, 30, ostB)
            dma(ocb[:, l * 4 + j, :], ostB[:, :], ["ost"], [], "ocb")

            def consB(ti, t0, n, ps, pk, j=j):
                act(bconv[:, j, t0:t0 + n], ps[:, 0:n], AF.Identity, [pk, "pcol"], [CT, ("bconv", j)],
                    bias=pcol[:, pc + PC_CBB + j:pc + PC_CBB + j + 1])
            conv_stage(31, pc + PC_CBW, None, j, ub, ubk, consB)
        BK = [("bconv", c) for c in range(4)]
        for ti, (t0, n) in enumerate(TT):
            ps1, pk1 = PS()
            for c in range(4):
                mm(ps1[:, 0:n], onesb[:, :], bconv[:, c, t0:t0 + n], c == 0, c == 3, BK + [CT, "constsb_o"], [pk1])
            ps2, pk2 = PS()
            for c in range(4):
                act(sqb[:, c % 2, 0:n], bconv[:, c, t0:t0 + n], AF.Square, BK + [CT], [("sqb", c % 2)])
                mm(ps2[:, 0:n], onesb[:, :], sqb[:, c % 2, 0:n], c == 0, c == 3, [("sqb", c % 2)], [pk2])
            mu, muk = lnbuf[:, 0, :], ("cnp", 0)
            act(mu[:, 0:n], ps1[:, 0:n], AF.Copy, [pk1], [muk], scale=1.0 / 512)
            rs, rk = lnbuf[:, 1, :], ("cnp", 1)
            tt("dve", rs[:, 0:n], mu[:, 0:n], mu[:, 0:n], ALU.mult, [muk], [rk])
            stt(rs[:, 0:n], ps2[:, 0:n], 1.0 / 512, rs[:, 0:n], ALU.mult, ALU.subtract, [pk2, rk], [rk])
            act(rs[:, 0:n], rs[:, 0:n], AF.Sqrt, [rk], [rk], bias=EPS)
            P.add("dve", lambda e, a=rs[:, 0:n]: e.reciprocal(a, a), r=[rk], w=[rk])
            for c in range(4):
                xc, xck = T5()
                tt("dve", xc[:, 0:n], bconv[:, c, t0:t0 + n], mu[:, 0:n], ALU.subtract, BK + [CT, muk], [xck])
                stt(xc[:, 0:n], xc[:, 0:n], pcol[:, pc + PC_LNG + c:pc + PC_LNG + c + 1], rs[:, 0:n], ALU.mult, ALU.mult,
                    [xck, rk, "pcol"], [xck])
                act(vab[:, c, t0:t0 + n], xc[:, 0:n], AF.Silu, [xck, "pcol"], [CT, ("vab", c)],
                    bias=pcol[:, pc + PC_LNB + c:pc + PC_LNB + c + 1])
        chk(l * 10 + 5)
        merge_stage(False, C_GB, w_ob[l], 4, lambda kc, t0, n: vab[:, kc, t0:t0 + n], [CT] + [("vab", c) for c in range(4)], TT)
        fence()

        chk(l * 10 + 6)
        P.add("pool", lambda e: e.memset(_rs(vtok, "p c h n -> p (c h) n")[:, :, 256:258], 1.0), r=[], w=[CT, "vtok"])
        P.add("pool", lambda e: e.memset(cnp[:, :, :], 0.0), r=[], w=[("cnp", h_) for h_ in range(4)])
        P.add("pool", lambda e: e.memset(ctp[:, :, :], 0.0), r=[], w=[("ctp", h_) for h_ in range(4)])
        P.add("pool", lambda e: e.memset(nT[:, :, 0:4], 0.0), r=[], w=["nT"])
        P.add("pool", lambda e: e.memset(msb[:, 16:20], 0.0), r=[], w=["carry"])
        wgt, wgk = wtile(Win, 0, 8, C_IF, 8)
        wgb = sb("wgb%d" % l, [128, 8, 8], BF16)
        cp("pool", wgb[:, :, :], wgt, [wgk], [("wgb", l)])
        R = lambda i: rows[:, i, :]
        for sc in range(5):
            t0s, ns = TT[sc]
            samp = sc == 4
            Lc = 64 if samp else 128
            ntc = 1 if samp else 4
            nseq = NB if samp else 1
            tiles_sc = [(t0s, ns)]
            for c in range(8):
                wtq, wkq = wtile(Win, 0, 8, C_Q + c * 128, 128)

                def cq(ti, t0, n, ps, pk, c=c):
                    cp("act", qTs[:, c, 0:n], ps[:, 0:n], [pk], [CT, "qTs"])
                proj(wtq, wkq, 0, hsrc, HK, 8, tiles_sc, cq)
                wtk, wkk = wtile(Win, 0, 8, C_K + c * 128, 128)

                def ck(ti, t0, n, ps, pk, c=c):
                    act(kTs[:, c, 0:n], ps[:, 0:n], AF.Copy, [pk], [CT, "kTs"], scale=0.0625)
                proj(wtk, wkk, 0, hsrc, HK, 8, tiles_sc, ck)
                for kind, col in (("k", C_K), ("v", C_V), ("o", C_O)):
                    if kind == "k":
                        wtt, wkt = wtk, wkk
                    else:
                        wtt, wkt = wtile(Win, 0, 8, col + c * 128, 128)
                    for tc in range(ntc):
                        ps, pk = PS()
                        ta = t0s + tc * 128
                        for kc in range(8):
                            mm(ps[0:Lc, 0:128], hT[:, kc, ta:ta + Lc], wtt[:, kc, :], kc == 0, kc == 7, [wkt] + HK, [pk])
                        if kind == "k":
                            act(ktok[0:Lc, tc, c * 128:(c + 1) * 128], ps[0:Lc, 0:128], AF.Copy, [pk], [CT, "ktok"], scale=0.0625)
                        elif kind == "v":
                            cp("dve", vtok[0:Lc, tc, c // 2, (c % 2) * 128:(c % 2) * 128 + 128], ps[0:Lc, 0:128], [pk], [CT, "vtok"])
                        else:
                            so_, sok = T5()
                            act(so_[0:Lc, 0:128], ps[0:Lc, 0:128], AF.Sigmoid, [pk], [sok])
                            tt("pool", gob[0:Lc, tc, c * 128:(c + 1) * 128], so_[0:Lc, 0:128], gnb[0:Lc, c * 128:(c + 1) * 128],
                               ALU.mult, [sok, "gnb"], [CT, "gob"])
            for tc in range(ntc):
                ta = t0s + tc * 128
                lo = tc * 128
                for gi_, (ro, co, bo) in enumerate(((0, 0, 0), (1, 4, 1))):
                    ps, pk = PS()
                    for kc in range(8):
                        mm(ps[0:4, 0:Lc], wgb[:, kc, co:co + 4], hT[:, kc, ta:ta + Lc], kc == 0, kc == 7, [("wgb", l)] + HK, [pk])
                    act(R(ro)[:, 0:Lc], ps[0:4, 0:Lc], AF.Identity, [pk, "bif"], [("row", ro)], bias=bif[:, l * 2 + bo:l * 2 + bo + 1])
                ts("dve", R(7)[:, 0:Lc], R(1)[:, 0:Lc], -1.0, None, ALU.mult, None, [("row", 1)], [("row", 7)])
                tt("dve", R(7)[:, 0:Lc], R(7)[:, 0:Lc], R(1)[:, 0:Lc], ALU.max, [("row", 1), ("row", 7)], [("row", 7)])
                act(R(7)[:, 0:Lc], R(7)[:, 0:Lc], AF.Exp, [("row", 7)], [("row", 7)], scale=-1.0)
                act(R(7)[:, 0:Lc], R(7)[:, 0:Lc], AF.Ln, [("row", 7)], [("row", 7)], bias=1.0)
                stt(R(1)[:, 0:Lc], R(1)[:, 0:Lc], 0.0, R(7)[:, 0:Lc], ALU.min, ALU.subtract, [("row", 1), ("row", 7)], [("row", 1)])
                if not samp:
                    P.add("dve", lambda e: e.tensor_tensor_scan(R(2)[:, 0:128], cst[0:4, 1536:1537].to_broadcast([4, 128]), R(1)[:, 0:128],
                                                                msb[:, 16:17], ALU.mult, ALU.add), r=[("row", 1), "carry", "consts"], w=[("row", 2)])
                    tt("dve", R(0)[:, 0:128], R(0)[:, 0:128], R(2)[:, 0:128], ALU.subtract, [("row", 0), ("row", 2)], [("row", 0)])
                    P.add("dve", lambda e: e.tensor_tensor_scan(R(3)[:, 0:128], cst[0:4, 1537:1538].to_broadcast([4, 128]), R(0)[:, 0:128],
                                                                msb[:, 17:18], ALU.add, ALU.max), r=[("row", 0), "carry", "consts"], w=[("row", 3)])
                    Mend = R(3)[:, 127:128].to_broadcast([4, 128])
                    Mprev = msb[:, 17:18].to_broadcast([4, 128])
                    tt("dve", msb[:, 20:21], msb[:, 17:18], R(3)[:, 127:128], ALU.subtract, ["carry", ("row", 3)], ["wpr"])
                    npair = 1
                else:
                    v3 = lambda i: _rs(R(i)[:, 0:64], "p (b t) -> p b t", b=NB)
                    cp("dve", v3(2)[:, :, 0:1], v3(1)[:, :, 0:1], [("row", 1)], [("row", 2)])
                    for t_ in range(1, 4):
                        tt("dve", v3(2)[:, :, t_:t_ + 1], v3(2)[:, :, t_ - 1:t_], v3(1)[:, :, t_:t_ + 1], ALU.add, [("row", 1), ("row", 2)], [("row", 2)])
                    tt("dve", R(0)[:, 0:64], R(0)[:, 0:64], R(2)[:, 0:64], ALU.subtract, [("row", 0), ("row", 2)], [("row", 0)])
                    m0 = msb[:, 0:16].unsqueeze(2)
                    tt("dve", v3(3)[:, :, 0:1], v3(0)[:, :, 0:1], m0, ALU.max, [("row", 0), "msb"], [("row", 3)])
                    for t_ in range(1, 4):
                        tt("dve", v3(3)[:, :, t_:t_ + 1], v3(3)[:, :, t_ - 1:t_], v3(0)[:, :, t_:t_ + 1], ALU.max, [("row", 0), ("row", 3)], [("row", 3)])
                    Mend = v3(3)[:, :, 3:4].to_broadcast([4, NB, 4])
                    Mprev = m0.to_broadcast([4, NB, 4])
                    tt("dve", R(7)[:, 64:80].unsqueeze(2), m0, v3(3)[:, :, 3:4], ALU.subtract, ["msb", ("row", 3)], ["wpr"])
                    npair = NB
                Lv = (lambda i: R(i)[:, 0:Lc]) if not samp else (lambda i: _rs(R(i)[:, 0:64], "p (b t) -> p b t", b=NB))
                tt("dve", Lv(4), Lv(0), Mend, ALU.subtract, [("row", 0), ("row", 3)], [("row", 4)])
                act(R(4)[:, 0:Lc], R(4)[:, 0:Lc], AF.Exp, [("row", 4)], [("row", 4)])
                tt("dve", R(5)[:, 0:Lc], R(2)[:, 0:Lc], R(3)[:, 0:Lc], ALU.add, [("row", 2), ("row", 3)], [("row", 5)])
                if samp:
                    cp("dve", msb[:, 32:48].unsqueeze(2), _rs(R(5)[:, 0:64], "p (b t) -> p b t", b=NB)[:, :, 3:4], [("row", 5)], ["mout"])
                    dma(om[:, l * 17 + 1:l * 17 + 17], msb[:, 32:48], ["mout"], [], "om_s")
                elif sc == 3 and tc == 3:
                    cp("dve", msb[:, 24:25], R(5)[:, 127:128], [("row", 5)], ["moutp"])
                    dma(om[:, l * 17:l * 17 + 1], msb[:, 24:25], ["moutp"], [], "om_p")
                act(R(5)[:, 0:Lc], R(5)[:, 0:Lc], AF.Exp, [("row", 5)], [("row", 5)], scale=-1.0)
                tt("dve", Lv(6), Mprev, Lv(3), ALU.subtract, [("row", 3), "carry", "msb"], [("row", 6)])
                act(R(6)[:, 0:Lc], R(6)[:, 0:Lc], AF.Exp, [("row", 6)], [("row", 6)])
                if not samp:
                    act(msb[:, 20:21], msb[:, 20:21], AF.Exp, ["wpr"], ["wpr"])
                    ts("dve", R(7)[:, 96:100], I4, msb[:, 20:21], None, ALU.mult, None, ["wpr", "consts"], ["wprx"])
                    ps, pk = PS()
                    mm(ps[:, 0:4], onesf[0:4, :], R(7)[:, 96:100], True, True, ["wprx", "consts"], [pk])
                    cp("dve", wprb[:, 0:4], ps[:, 0:4], [pk], ["wprb"])
                    cp("dve", msb[:, 16:17], R(2)[:, 127:128], [("row", 2)], ["carry"])
                    cp("dve", msb[:, 17:18], R(3)[:, 127:128], [("row", 3), ("row", 6), "wpr"], ["carry"])
                else:
                    act(R(7)[:, 64:80], R(7)[:, 64:80], AF.Exp, ["wpr"], ["wpr"])
                    wx = _rs(wxs[0:4, 0:64], "p (b h) -> p b h", b=NB)
                    tt("dve", wx, R(7)[:, 64:80].unsqueeze(2).to_broadcast([4, NB, 4]), I4.unsqueeze(1).to_broadcast([4, NB, 4]),
                       ALU.mult, ["wpr", "consts"], ["wprx"])
                    ps, pk = PS()
                    mm(ps[:, 0:64], onesf[0:4, :], wxs[0:4, 0:64], True, True, ["wprx", "consts"], [pk])
                    cp("dve", wprb[:, 0:64], ps[:, 0:64], [pk], ["wprb"])
                ps, pk = PS()
                for q, ri in enumerate((0, 4, 5, 6)):
                    mm(ps[0:Lc, q * 4:q * 4 + 4], R(ri)[:, 0:Lc], I4, True, True, [("row", ri), "consts"], [pk])
                cp("dve", gcol[0:Lc, :], ps[0:Lc, 0:16], [pk], ["gcol"])
                def head_gen(h):
                    wT_ = wTt4[0:Lc, h, 0:Lc]
                    smT_ = smT4[0:Lc, h, 0:Lc]
                    qsT_ = _rs(qsT4[:, h, :], "p (c t) -> p c t", c=2)
                    wlm_ = wlm4[:, h, :]
                    wlmb_ = wlmb4[:, h, :]
                    e0 = h * 4
                    K = lambda nm: (nm, h)
                    psq, pkq = PS()
                    for dc in range(2):
                        mm(psq[0:Lc, 0:Lc], kTs[:, h * 2 + dc, lo:lo + Lc], qTs[:, h * 2 + dc, lo:lo + Lc], dc == 0, dc == 1,
                           ["qTs", "kTs", CT], [pkq])
                    psm, pkm = PS()
                    mm(psm[0:Lc, 0:Lc], selneg[:, h * 128:h * 128 + Lc], R(3)[:, 0:Lc], True, False, [("row", 3), "consts"], [pkm])
                    mm(psm[0:Lc, 0:Lc], ident[0:Lc, 0:Lc], (maskS if samp else maskP), False, True, ["consts"], [pkm])
                    act(wT_, psm[0:Lc, 0:Lc], AF.Exp, [pkm, "gcol"], [K("wTt")], bias=gcol[0:Lc, h:h + 1])
                    tt("dve", smT_, psq[0:Lc, 0:Lc], wT_, ALU.mult, [pkq, K("wTt")], [K("smT")])
                    yield
                    psw, pkw = PS()
                    mm(psw[:, 0:Lc], selpos[:, h * 128:(h + 1) * 128], R(6)[:, 0:Lc], True, True, [("row", 6), "consts"], [pkw])
                    for dc in range(2):
                        tt("dve", qsT_[:, dc, 0:Lc], qTs[:, h * 2 + dc, lo:lo + Lc], psw[:, 0:Lc], ALU.mult, [pkw, "qTs", CT], [K("qsT")])
                    if samp:
                        for dc in range(2):
                            tt("pool", qsTm[:, dc, :, :], qsT_[:, dc, 0:64].unsqueeze(1).to_broadcast([128, NB, 64]),
                               _rs(mrowb[:, :], "p (b t) -> p b t", b=NB), ALU.mult, [K("qsT"), "mrowb"], [CT, "qsTm"])
                        ts("dve", wlm_[0:64, :], maskcol, gcol[0:64, 4 + h:5 + h], None, ALU.mult, None, ["gcol", "consts"], [K("wlm")])
                        cp("dve", wlmb_[0:64, :], wlm_[0:64, :], [K("wlm")], [K("wlmb")])
                    else:
                        cp("dve", wlmb_[0:128, 0:1], gcol[0:128, 4 + h:5 + h], ["gcol"], [K("wlmb")])
                    yield
                    pso, pko = PSL()
                    mm(pso[0:Lc, 0:257], smT_, vtok[0:Lc, tc, h, 0:257], True, False, [K("smT"), "vtok", CT], [pko])
                    for b in range(nseq):
                        if samp:
                            s2 = (b + h * NB) % 2
                            pidx = 4 + b * 4 + h
                            dma(_rs(cts_f[:, s2, :], "p (c e) -> p c e", c=2), _rs(ctr_d[l, b, h], "(c p) e -> p c e", p=128),
                                [], [("ctsf", s2), CT], "ctsf%d" % s2)
                            dma(_rs(cns[:, s2, :], "p (c e) -> p c e", c=2), _rs(cnat_d[l, b, h], "(c p) e -> p c e", p=128),
                                [], [("cns", s2), CT], "cns%d" % s2)
                            ctv = _rs(cts[:, s2, :], "p (c e) -> p c e", c=2)
                            cp("act", ctv[:, :, 0:256], _rs(cts_f[:, s2, :], "p (c e) -> p c e", c=2), [("ctsf", s2)], [("cts", s2)])
                            cp("pool", ctv[:, :, 256:257], nT[:, :, pidx:pidx + 1], ["nT"], [("cts", s2)])
                            ctk = ("cts", s2)
                            cnv, cnk = cns[:, s2, :], ("cns", s2)
                            lhs_q = lambda dc, b=b: qsTm[:, dc, b, :]
                            qk_ = ["qsTm", CT]
                            wpc = wprb[:, b * 4 + h:b * 4 + h + 1]
                            wl_col = wlm_[0:64, b:b + 1]
                            wl_colb = wlmb_[0:64, b:b + 1]
                        else:
                            pidx = h
                            ctv = _rs(ctp[:, h, :], "p (c e) -> p c e", c=2)
                            ctk = ("ctp", h)
                            cnv, cnk = cnp[:, h, :], ("cnp", h)
                            lhs_q = lambda dc: qsT_[:, dc, 0:128]
                            qk_ = [K("qsT")]
                            wpc = wprb[:, h:h + 1]
                            wl_col = gcol[0:128, 4 + h:5 + h]
                            wl_colb = wlmb_[0:128, 0:1]
                        for dc in range(2):
                            mm(pso[0:Lc, 0:257], lhs_q(dc), ctv[:, dc, 0:257], False, (b == nseq - 1 and dc == 1), qk_ + [ctk], [pko])
                        if not samp:
                            yield
                        ts("dve", vwb4[0:Lc, h, :], vtok[0:Lc, tc, h, 0:256], wl_col, None, ALU.mult, None, ["vtok", CT, "gcol", K("wlm")], [K("vwb")])
                        psu, pku = PS()
                        for ec in range(2):
                            mm(psu[:, ec * 256:(ec + 1) * 256], vwb4[0:Lc, h, ec * 128:(ec + 1) * 128], ktok[0:Lc, tc, h * 256:(h + 1) * 256],
                               True, True, [K("vwb"), "ktok", CT], [pku])
                        stt(cnv, cnv, wpc, psu[:, :], ALU.mult, ALU.add, [cnk, "wprb", pku], [cnk])
                        psn, pkn = PS()
                        for dc in range(2):
                            mm(psn[:, dc:dc + 1], ktok[0:Lc, tc, h * 256 + dc * 128:h * 256 + dc * 128 + 128], wl_colb, True, True,
                               ["ktok", CT, K("wlmb")], [pkn])
                        stt(nT[:, :, pidx], nT[:, :, pidx], wpc, psn[:, 0:2], ALU.mult, ALU.add, ["nT", "wprb", pkn, ctk], ["nT"])
                        if samp:
                            dma(_rs(oC[l, 1 + b, h], "(c p) d -> p c d", p=128), _rs(cnv, "p (c d) -> p c d", c=2), [cnk], [], "oc%d" % s2)
                        else:
                            yield
                            for dcc in range(2):
                                pst, pkt = PS()
                                for ec in range(2):
                                    P.add("pe", lambda e, o=pst[:, ec * 128:(ec + 1) * 128], i=cnp[:, h, ec * 256 + dcc * 128:ec * 256 + dcc * 128 + 128]:
                                          e.transpose(o, i, ident), r=[("cnp", h), "consts"], w=[pkt])
                                cp("act", ctv[:, dcc, 0:256], pst[:, 0:256], [pkt], [("ctp", h)])
                            cp("pool", ctv[:, :, 256:257], nT[:, :, h:h + 1], ["nT"], [("ctp", h)])
                            if sc == 3 and tc == 3:
                                dma(_rs(oC[l, 0, h], "(c p) d -> p c d", p=128), _rs(cnv, "p (c d) -> p c d", c=2), [cnk], [], "ocp%d" % h)
                            yield
                    sm = eps4
                    ts("dve", sm[0:Lc, e0:e0 + 1], pso[0:Lc, 256:257], -1.0, None, ALU.mult, None, [pko], [K("ep0")])
                    tt("dve", sm[0:Lc, e0:e0 + 1], sm[0:Lc, e0:e0 + 1], pso[0:Lc, 256:257], ALU.max, [pko, K("ep0")], [K("ep0")])
                    tt("dve", sm[0:Lc, e0:e0 + 1], sm[0:Lc, e0:e0 + 1], gcol[0:Lc, 8 + h:9 + h], ALU.max, ["gcol", K("ep0")], [K("ep0")])
                    P.add("dve", lambda e, a=sm[0:Lc, e0:e0 + 1]: e.reciprocal(a, a), r=[K("ep0")], w=[K("ep0")])
                    yield
                    jk, jkk = T5()
                    act(jk[0:Lc, 0:256], pso[0:Lc, 0:256], AF.Square, [pko, K("ep0")], [jkk, K("ep1")], scale=sm[0:Lc, e0:e0 + 1], accum=sm[0:Lc, e0 + 1:e0 + 2])
                    act(sm[0:Lc, e0 + 2:e0 + 3], sm[0:Lc, e0 + 1:e0 + 2], AF.Sqrt, [K("ep1")], [K("ep2")], bias=EPS, scale=1.0 / 256)
                    yield
                    P.add("dve", lambda e, a=sm[0:Lc, e0 + 2:e0 + 3]: e.reciprocal(a, a), r=[K("ep2")], w=[K("ep2")])
                    tt("dve", sm[0:Lc, e0 + 3:e0 + 4], sm[0:Lc, e0 + 2:e0 + 3], sm[0:Lc, e0:e0 + 1], ALU.mult, [K("ep2"), K("ep0")], [K("ep3")])
                    stt(vct[0:Lc, h * 256:(h + 1) * 256], pso[0:Lc, 0:256], sm[0:Lc, e0 + 3:e0 + 4], gob[0:Lc, tc, h * 256:(h + 1) * 256],
                        ALU.mult, ALU.mult, [pko, K("ep3"), "gob", CT], [CT, "vct"])

                ps_nb[0] = 4
                gens = [head_gen(h) for h in range(4)]
                if samp:
                    for g_ in gens:
                        for _ in g_:
                            pass
                    gens = []
                while gens:
                    for g_ in list(gens):
                        try:
                            next(g_)
                        except StopIteration:
                            gens.remove(g_)
                ps_nb[0] = 6
                pst, pkt = PS()
                pstb = pst[:, :].bitcast(BF16)
                for c in range(8):
                    P.add("pe", lambda e, o=pstb[:, c * 128:c * 128 + Lc], i=vct[0:Lc, c * 128:(c + 1) * 128], idn=identb[0:Lc, 0:Lc]:
                          e.transpose(o, i, idn), r=["vct", CT, "constsb_i"], w=[pkt])
                cp("act", vcTs[:, :, lo:lo + Lc], _rs(pstb, "p (c t) -> p c t", c=8)[:, :, 0:Lc], [pkt], [CT, "vcTs"])
            merge_stage(False, C_GC, w_oc[l], 8, lambda kc, t0, n, t0s=t0s: vcTs[:, kc, t0 - t0s:t0 - t0s + n], [CT, "vcTs"], tiles_sc)
        dma(on[:, l, :], _rs(nT[:, :, :], "p c n -> p (c n)"), ["nT"], [], "on")
        fence()
        chk(l * 10 + 7)
        residual(w_o[l], 8, lambda kc, t0, n: big2[:, kc, t0:t0 + n], [("u", c) for c in range(8)])

        chk(l * 10 + 8)
        rmsnorm(xcur, xk, pc + PC_NX, hT_dst)
        fence()
        chk(l * 10 + 8.1)
        for c in range(8):
            wt, wk = wtile(w_xq[l], 0, 8, c * 128, 128)

            def cxq(ti, t0, n, ps, pk, c=c):
                act(big2[:, c, t0:t0 + n], ps[:, 0:n], AF.Copy, [pk], [("u", c)], scale=0.0625)
            proj(wt, wk, 0, hsrc, HK, 8, TT, cxq)
        chk(l * 10 + 8.2)
        for c in range(8):
            ts("dve", memTl[:, c, :], memTb[:, c, :], pcol[:, pc + PC_NMEM + c:pc + PC_NMEM + c + 1], None, ALU.mult, None,
               ["memTb", "pcol"], [CT, "memTl"])
        chk(l * 10 + 8.25)
        for kv in range(1 if _os.environ.get("DBG_KV0") else 2):
            for c in range(int(_os.environ.get("DBG_NC", "8"))):
                wt, wk = wtile(w_xkv[l], 0, 8, kv * D + c * 128, 128)
                if kv == 0 and not _os.environ.get("DBG_X1"):
                    ps, pk = PS()
                    for kc in range(8):
                        mm(ps[:, 0:256], wt[:, kc, :], memTl[:, kc, :], kc == 0, kc == 7, [wk, "memTl", CT], [pk])
                    cp("act", KTp[:, c, :], ps[:, 0:256], [pk], [CT, "KTp"])
                for mc in range(0 if _os.environ.get("DBG_X2") else 2):
                    ps, pk = PS()
                    for kc in range(8):
                        mm(ps[:, 0:128], memTl[:, kc, mc * 128:(mc + 1) * 128], wt[:, kc, :], kc == 0, kc == 7, [wk, "memTl", CT], [pk])
                    s4 = (c * 2 + mc) % 2
                    cp("dve", oms[:, s4, 0:128], ps[:, 0:128], [pk], [("oms", s4)])
                    dst = (omk if kv == 0 else omv)[l, mc * 128:(mc + 1) * 128, c * 128:(c + 1) * 128]
                    if not _os.environ.get("DBG_NOOM"):
                        dma(dst, oms[:, s4, 0:128], [("oms", s4)], [], "oms%d" % s4)
                    if kv == 1:
                        cp("act", Vp[:, mc, c * 128:(c + 1) * 128], oms[:, s4, 0:128], [("oms", s4)], [CT, "Vp"])
        chk(l * 10 + 8.3)
        QK = [("u", c) for c in range(8)]
        for tcx in range(16):
            ta = tcx * 128
            for h in range(4):
                ps, pk = PS()
                for dc in range(2):
                    mm(ps[:, 0:256], big2[:, h * 2 + dc, ta:ta + 128], KTp[:, h * 2 + dc, :], dc == 0, dc == 1, QK + ["KTp", CT], [pk])
                P.add("dve", lambda e, o=smalls[:, 12:13], i=ps[:, 0:256]: e.tensor_reduce(o, i, AX.X, ALU.max), r=[pk], w=["xa0"])
                ts("dve", smalls[:, 13:14], smalls[:, 12:13], -1.0, None, ALU.mult, None, ["xa0"], ["xa1"])
                act(pexp[:, 0:256], ps[:, 0:256], AF.Exp, [pk, "xa1"], [CT, "pexp", "xa2"], bias=smalls[:, 13:14], accum=smalls[:, 14:15])
                pst, pkt = PS()
                pstb = pst[:, :].bitcast(BF16)
                for mc in range(2):
                    P.add("pe", lambda e, o=pstb[:, mc * 128:(mc + 1) * 128], i=pexp[:, mc * 128:(mc + 1) * 128]:
                          e.transpose(o, i, identb[:, :]), r=["pexp", CT, "constsb_i"], w=[pkt])
                cp("act", pT[:, 0:256], pstb[:, 0:256], [pkt], [CT, "pT"])
                pso, pko = PS()
                for mc in range(2):
                    mm(pso[:, 0:256], pT[:, mc * 128:(mc + 1) * 128], Vp[:, mc, h * 256:(h + 1) * 256], mc == 0, mc == 1, ["pT", "Vp", CT], [pko])
                P.add("dve", lambda e, a=smalls[:, 15:16], i=smalls[:, 14:15]: e.reciprocal(a, i), r=["xa2"], w=["xa3"])
                ts("dve", aot[:, h * 256:(h + 1) * 256], pso[:, 0:256], smalls[:, 15:16], None, ALU.mult, None, [pko, "xa3"], ["aot"])
            pst, pkt = PS()
            pstb = pst[:, :].bitcast(BF16)
            for c in range(8):
                P.add("pe", lambda e, o=pstb[:, c * 128:(c + 1) * 128], i=aot[:, c * 128:(c + 1) * 128]:
                      e.transpose(o, i, identb[:, :]), r=["aot", "constsb_i"], w=[pkt])
            cp("act", aoT[:, :, ta:ta + 128], _rs(pstb, "p (c t) -> p c t", c=8), [pkt], [CT, "aoT"])
        chk(l * 10 + 8.4)
        for h in range(4):
            pss, pks = PSL()
            for dc in range(2):
                tt("pool", pTm[:, dc, :, :], big2[:, h * 2 + dc, PT:PT + 64].unsqueeze(1).to_broadcast([128, NB, 64]),
                   _rs(mrowb[:, :], "p (b t) -> p b t", b=NB), ALU.mult, QK + ["mrowb"], [CT, "pTm"])
            for b in range(NB):
                dma(_rs(kvst[:, 0:512], "p (c m) -> p c m", c=2), _rs(kT_d[l, b, h], "(c p) m -> p c m", p=128), [], [CT, "kvst"], "kvst")
                cp("pool", kvbf[:, 0:512], kvst[:, 0:512], ["kvst", CT], [CT, "kvbf"])
                for dc in range(2):
                    mm(pss[0:64, 0:256], pTm[:, dc, b, :], kvbf[:, dc * 256:(dc + 1) * 256], (b == 0 and dc == 0), (b == NB - 1 and dc == 1),
                       ["pTm", "kvbf", CT], [pks])
            P.add("dve", lambda e, o=smalls[0:64, 12:13], i=pss[0:64, 0:256]: e.tensor_reduce(o, i, AX.X, ALU.max), r=[pks], w=["xa0"])
            ts("dve", smalls[0:64, 13:14], smalls[0:64, 12:13], -1.0, None, ALU.mult, None, ["xa0"], ["xa1"])
            act(pexp[0:64, 0:256], pss[0:64, 0:256], AF.Exp, [pks, "xa1"], [CT, "pexp", "xa2"], bias=smalls[0:64, 13:14], accum=smalls[0:64, 14:15])
            pst, pkt = PS()
            pstb = pst[:, :].bitcast(BF16)
            for mc in range(2):
                P.add("pe", lambda e, o=pstb[:, mc * 64:(mc + 1) * 64], i=pexp[0:64, mc * 128:(mc + 1) * 128]:
                      e.transpose(o, i, identb[0:64, 0:64]), r=["pexp", CT, "constsb_i"], w=[pkt])
            cp("act", pT[:, 0:128], pstb[:, 0:128], [pkt], [CT, "pT"])
            for mc in range(2):
                tt("pool", pTm[:, mc, :, :], pT[:, mc * 64:(mc + 1) * 64].unsqueeze(1).to_broadcast([128, NB, 64]),
                   _rs(mrowb[:, :], "p (b t) -> p b t", b=NB), ALU.mult, ["pT", "mrowb", CT], [CT, "pTm"])
            pso, pko = PSL()
            for b in range(NB):
                dma(_rs(kvst[:, 0:512], "p (c e) -> p c e", c=2),
                    _rs(v_d[l, b, :, h * 256:(h + 1) * 256], "(c p) e -> p c e", p=128), [], [CT, "kvst"], "kvst")
                cp("pool", kvbf[:, 0:512], kvst[:, 0:512], ["kvst", CT], [CT, "kvbf"])
                for mc in range(2):
                    mm(pso[0:64, 0:256], pTm[:, mc, b, :], kvbf[:, mc * 256:(mc + 1) * 256], (b == 0 and mc == 0), (b == NB - 1 and mc == 1),
                       ["pTm", "kvbf", CT], [pko])
            P.add("dve", lambda e, a=smalls[0:64, 15:16], i=smalls[0:64, 14:15]: e.reciprocal(a, i), r=["xa2"], w=["xa3"])
            ts("dve", aot[0:64, h * 256:(h + 1) * 256], pso[0:64, 0:256], smalls[0:64, 15:16], None, ALU.mult, None, [pko, "xa3"], ["aot"])
        pst, pkt = PS()
        pstb = pst[:, :].bitcast(BF16)
        for c in range(8):
            P.add("pe", lambda e, o=pstb[:, c * 128:c * 128 + 64], i=aot[0:64, c * 128:(c + 1) * 128]:
                  e.transpose(o, i, identb[0:64, 0:64]), r=["aot", "constsb_i"], w=[pkt])
        cp("act", aoT[:, :, PT:PT + 64], _rs(pstb, "p (c t) -> p c t", c=8)[:, :, 0:64], [pkt], [CT, "aoT"])
        chk(l * 10 + 8.5)
        fence()
        residual(w_xo[l], 8, lambda kc, t0, n: aoT[:, kc, t0:t0 + n], [CT, "aoT"])
        fence()

        chk(l * 10 + 9)
        rmsnorm(xcur, xk, pc + PC_NFFN, hT_dst)
        fence()
        for g0, gn in ((0, 8), (8, 8), (16, 6)):
            for i in range(gn):
                hc = g0 + i
                wtg, wkg = wtile(w_fi[l], 0, 8, hc * 128, 128)
                wtu, wku = wtile(w_fi[l], 0, 8, DFF + hc * 128, 128)
                for ti, (t0, n) in enumerate(TT):
                    psG, pkG = PS()
                    for kc in range(8):
                        mm(psG[:, 0:n], wtg[:, kc, :], hsrc(kc, t0, n), kc == 0, kc == 7, [wkg] + HK, [pkG])
                    sg, sgk = T5()
                    act(sg[:, 0:n], psG[:, 0:n], AF.Silu, [pkG], [sgk])
                    psU, pkU = PS()
                    for kc in range(8):
                        mm(psU[:, 0:n], wtu[:, kc, :], hsrc(kc, t0, n), kc == 0, kc == 7, [wku] + HK, [pkU])
                    tt("dve", big2[:, i, t0:t0 + n], psU[:, 0:n], sg[:, 0:n], ALU.mult, [pkU, sgk], [("u", i)])
            nonlocal_src = w_fo[l][g0 * 128:(g0 + gn) * 128, :]
            residual(nonlocal_src, gn, lambda kc, t0, n: big2[:, kc, t0:t0 + n], [("u", c) for c in range(8)])

    stopped = False
    try:
        chk(0)
        layers()
    except _Stop:
        stopped = True
    P.epoch = L

    def y_dst(c, t0, n):
        return None
    for ti, (t0, n) in enumerate([] if stopped else TT):
        xin, xik = xin2[ti % 2], ("xin", ti % 2)
        dma(xin[:, :, 0:n], xcur[:, :, t0:t0 + n], [(xk, ti)], [xik], "xin%d" % (ti % 2))
        ps, pk = PS()
        for c in range(8):
            act(sqb[:, c % 2, 0:n], xin[:, c, 0:n], AF.Square, [xik], [("sqb", c % 2)])
            mm(ps[:, 0:n], onesb[:, :], sqb[:, c % 2, 0:n], c == 0, c == 7, [("sqb", c % 2)], [pk])
        rs, rk = T5()
        act(rs[:, 0:n], ps[:, 0:n], AF.Sqrt, [pk], [rk], bias=EPS, scale=1.0 / D)
        P.add("dve", lambda e, a=rs[:, 0:n]: e.reciprocal(a, a), r=[rk], w=[rk])
        for c in range(8):
            stt(xin[:, c, 0:n], xin[:, c, 0:n], pcol[:, PC_FIN + c:PC_FIN + c + 1], rs[:, 0:n], ALU.mult, ALU.mult,
                [xik, rk, "pcol"], [xik])
        dma(yT[:, :, t0:t0 + n], xin[:, :, 0:n], [xik], [], "yout%d" % (ti % 2))

    P.finalize()
    sem_names = set()
    for op in P.ops:
        if op.dma is not None:
            sem_names.add(("d", op.dma))
        elif op.signal:
            sem_names.add(("c", op.eng, op.epoch))
    sems = {}
    for i, s in enumerate(sorted(sem_names, key=str)):
        sems[s] = nc.semaphore("s%d" % i).__enter__()
    by_eng = {"pe": [], "act": [], "dve": [], "pool": [], "sp": []}
    for op in P.ops:
        by_eng[op.eng].append(op)
    final_waits = [(("d", k), v) for k, v in P.dma_cnt.items()]

    def run(e, ops, final=False):
        for op in ops:
            for s, v in op.waits:
                e.wait_ge(sems[s], v)
            ins = op.fn(e)
            if op.dma is not None:
                ins.then_inc(sems[("d", op.dma)], 16)
            elif op.signal:
                ins.then_inc(sems[("c", op.eng, op.epoch)], 1)
        if final:
            for s, v in final_waits:
                e.wait_ge(sems[s], v)

    with nc.allow_non_contiguous_dma(reason="small strided state rows"), nc.Block() as block:
        @block.sync
        def _(e):
            run(e, by_eng["sp"], final=True)

        @block.tensor
        def _(e):
            run(e, by_eng["pe"])

        @block.scalar
        def _(e):
            run(e, by_eng["act"])

        @block.vector
        def _(e):
            run(e, by_eng["dve"])

        @block.gpsimd
        def _(e):
            run(e, by_eng["pool"])
    return nc, len(P.ops)


def _consts():
    c = np.zeros((128, 1540), np.float32)
    c[:, 0:128] = np.eye(128)
    c[:, 128:256] = 1.0
    s = np.arange(128)
    c[:, 256:384] = np.where(s[None, :] >= s[:, None], 0.0, -30000.0)
    s6 = np.arange(64)
    same = (s6[:, None] // 4) == (s6[None, :] // 4)
    c[0:64, 384:448] = np.where(same & (s6[None, :] >= s6[:, None]), 0.0, -30000.0)
    c[0:64, 448:464] = (s6[:, None] // 4 == np.arange(16)[None, :]).astype(np.float32)
    c[0:4, 464:468] = np.eye(4)
    for h in range(4):
        c[h, 512 + h * 128:512 + (h + 1) * 128] = -1.0
        c[h, 1024 + h * 128:1024 + (h + 1) * 128] = 1.0
    mr = (np.arange(16)[:, None] == (s6[None, :] // 4)).astype(np.float32).reshape(-1)
    c[:, 1536] = 1.0
    c[:, 1537] = 0.0
    c[:, 1538] = -1.0
    return c, np.ascontiguousarray(np.broadcast_to(mr[None, :], (128, 1024))).astype(np.float32)


def _col(v):
    return np.ascontiguousarray(v.reshape(-1, 128).T)


_CACHE = {}


def _prep(inp):
    f = lambda k: np.asarray(inp[k], dtype=np.float32)
    cst, mrow = _consts()
    pcol = np.zeros((128, L * NPC), np.float32)
    for l in range(L):
        b = l * NPC
        pcol[:, b + PC_NMIX:b + PC_NMIX + 8] = _col(f("norm_mix_g")[l])
        pcol[:, b + PC_NX:b + PC_NX + 8] = _col(f("norm_x_g")[l])
        pcol[:, b + PC_NFFN:b + PC_NFFN + 8] = _col(f("norm_ffn_g")[l])
        pcol[:, b + PC_NMEM:b + PC_NMEM + 8] = _col(f("norm_mem_g")[l])
        pcol[:, b + PC_FIN:b + PC_FIN + 8] = _col(f("final_norm_g"))
        caw = f("conv_a_w")[l]
        cbw = f("conv_b_w")[l]
        for j in range(4):
            pcol[:, b + PC_CAW + j * 3:b + PC_CAW + (j + 1) * 3] = caw[:, j * 128:(j + 1) * 128].T
            pcol[:, b + PC_CBW + j * 31:b + PC_CBW + (j + 1) * 31] = cbw[:, j * 128:(j + 1) * 128].T
        pcol[:, b + PC_CBB:b + PC_CBB + 4] = _col(f("conv_b_b")[l])
        pcol[:, b + PC_LNG:b + PC_LNG + 4] = _col(f("ln_b_g")[l])
        pcol[:, b + PC_LNB:b + PC_LNB + 4] = _col(f("ln_b_b")[l])
    pcol[:, PC_FIN:PC_FIN + 8] = _col(f("final_norm_g"))
    gnorm = np.ascontiguousarray(np.broadcast_to(f("mlstm_norm_g")[:, None, :], (L, 128, D)))
    bif = np.ascontiguousarray(f("b_if").reshape(L, 2, 4).transpose(2, 0, 1).reshape(4, L * 2))
    xp, xs = f("x_prompt"), f("x_sample")
    shared = {k: f(k) for k in ("w_in", "w_out_a", "w_out_b", "w_out_c", "w_o", "w_xq", "w_xkv", "w_xo", "w_ffn_in", "w_ffn_out")}
    in_maps = []
    for i in range(8):
        bs = slice(i * NB, (i + 1) * NB)
        xt = np.concatenate([xp[i], xs[bs].reshape(ST, D)], axis=0)
        xT = np.ascontiguousarray(xt.T.reshape(8, 128, T).transpose(1, 0, 2))
        memtok = np.ascontiguousarray(f("mem_prompt")[i].reshape(2, 128, D).transpose(1, 0, 2))
        sca = f("state_conv_a")[:, bs]
        sca = np.ascontiguousarray(sca.reshape(L, NB, 2, 4, 128).transpose(4, 0, 3, 1, 2).reshape(128, -1))
        scb = f("state_conv_b")[:, bs]
        scb = np.ascontiguousarray(scb.reshape(L, NB, 30, 4, 128).transpose(4, 0, 3, 1, 2).reshape(128, -1))
        cn = np.ascontiguousarray(f("state_mlstm_c")[:, bs])
        ctr = np.ascontiguousarray(cn.transpose(0, 1, 2, 4, 3))
        nn = f("state_mlstm_n")[:, bs]
        nTi = np.ascontiguousarray(nn.reshape(L, NB, 4, 2, 128).transpose(4, 0, 3, 1, 2).reshape(128, -1))
        mi = np.ascontiguousarray(f("state_mlstm_m")[:, bs].transpose(2, 0, 1).reshape(4, -1))
        kTc = np.ascontiguousarray(f("cache_mem_k")[:, bs].transpose(0, 1, 3, 4, 2))
        vc = np.ascontiguousarray(f("cache_mem_v")[:, bs].reshape(L, NB, 256, D))
        m = {"xT_in": xT, "memtok": memtok, "pcol": pcol, "gnorm": gnorm, "bif": bif, "sca": sca, "scb": scb,
             "cnat": cn, "ctr": ctr, "nTin": nTi, "min": mi, "kTc": kTc, "vc": vc, "cst": cst, "mrow": mrow}
        m.update(shared)
        in_maps.append(m)
    return in_maps


def _post(R):
    y_p = np.zeros((8, PT, D), np.float32)
    y_s = np.zeros((128, 4, D), np.float32)
    p_a = np.zeros((L, 8, 2, 512), np.float32)
    p_b = np.zeros((L, 8, 30, 512), np.float32)
    p_c = np.zeros((L, 8, 4, 256, 256), np.float32)
    p_n = np.zeros((L, 8, 4, 256), np.float32)
    p_m = np.zeros((L, 8, 4), np.float32)
    p_k = np.zeros((L, 8, 256, 4, 256), np.float32)
    p_v = np.zeros((L, 8, 256, 4, 256), np.float32)
    s_a = np.zeros((L, 128, 2, 512), np.float32)
    s_b = np.zeros((L, 128, 30, 512), np.float32)
    s_c = np.zeros((L, 128, 4, 256, 256), np.float32)
    s_n = np.zeros((L, 128, 4, 256), np.float32)
    s_m = np.zeros((L, 128, 4), np.float32)
    for i in range(8):
        r = R[i]
        bs = slice(i * NB, (i + 1) * NB)
        yt = r["yT"].transpose(2, 1, 0).reshape(T, D)
        y_p[i] = yt[:PT]
        y_s[bs] = yt[PT:].reshape(NB, 4, D)
        a = r["oca"].reshape(128, L, 4, 17, 2).transpose(1, 3, 4, 2, 0).reshape(L, 17, 2, 512)
        p_a[:, i] = a[:, 0]
        s_a[:, bs] = a[:, 1:]
        b_ = r["ocb"].reshape(128, L, 4, 17, 30).transpose(1, 3, 4, 2, 0).reshape(L, 17, 30, 512)
        p_b[:, i] = b_[:, 0]
        s_b[:, bs] = b_[:, 1:]
        p_c[:, i] = r["oC"][:, 0]
        s_c[:, bs] = r["oC"][:, 1:]
        n_ = r["on"].reshape(128, L, 2, 68).transpose(1, 3, 2, 0).reshape(L, 68, 256)
        p_n[:, i] = n_[:, 0:4]
        s_n[:, bs] = n_[:, 4:].reshape(L, NB, 4, 256)
        m_ = r["om"].reshape(4, L, 17).transpose(1, 2, 0)
        p_m[:, i] = m_[:, 0]
        s_m[:, bs] = m_[:, 1:]
        p_k[:, i] = r["omk"].reshape(L, 256, 4, 256)
        p_v[:, i] = r["omv"].reshape(L, 256, 4, 256)
    return (y_p, y_s, p_a, p_b, p_c, p_n, p_m, p_k, p_v, s_a, s_b, s_c, s_n, s_m)


def kernel(**inp):
    if "nc" not in _CACHE:
        _CACHE["nc"] = build_nc()
    nc, nops = _CACHE["nc"]
    in_maps = _prep(inp)
    res = run_bass_kernel_spmd(nc, in_maps, core_ids=list(range(8)))
    return _post(res.results)
```

```python
import os as _os
import numpy as np
import concourse.bass as bass
import concourse.mybir as mybir
from concourse.bass_utils import run_bass_kernel_spmd

F32 = mybir.dt.float32
BF16 = mybir.dt.bfloat16
AF = mybir.ActivationFunctionType
ALU = mybir.AluOpType
AX = mybir.AxisListType

L = 4
D = 1024
T = 2112
PT = 2048
ST = 64
NB = 16
TT = [(0, 512), (512, 512), (1024, 512), (1536, 512), (2048, 64)]
INW = 9736
DFF = 2816
EPS = 1e-6
C_AB, C_AC, C_AX, C_BV, C_BG = 0, 512, 1024, 1536, 2048
C_Q, C_K, C_V, C_O, C_IF = 2560, 3584, 4608, 5632, 6656
C_GA, C_GB, C_GC = 6664, 7688, 8712
PC_NMIX, PC_NX, PC_NFFN, PC_NMEM, PC_FIN = 0, 8, 16, 24, 32
PC_CAW, PC_CBW, PC_CBB, PC_LNG, PC_LNB = 40, 52, 176, 180, 184
NPC = 188


_CTKEYS = {"ctmp", "memtS", "memn", "mrowst", "xin", "vab", "abuf", "ucv", "dg", "scbS", "bconv", "qTs", "kTs", "ktok",
           "vtok", "gob", "vct", "qsTm", "vcTs", "aoT", "kvst", "kvbf", "pTm", "KTp", "Vp", "memTl", "pexp", "pT", "cns", "ctsf", "xb", "oms", "aot"}


class Op:
    __slots__ = ("eng", "fn", "deps", "dma", "dval", "signal", "sigval", "waits", "epoch", "idx")


class Prog:
    def __init__(self):
        self.ops = []
        self.lastw = {}
        self.rd_eng = {}
        self.rd_dma = {}
        self.dma_cnt = {}
        self.epoch = 0

    def add(self, eng, fn, r=(), w=(), dma=None, fence=False):
        if not fence:
            def _isct(k):
                n = k[0] if isinstance(k, tuple) else k
                return n in _CTKEYS
            if any(_isct(k) for k in list(r) + list(w)):
                r = [k for k in r if k != "ctmp"] + ["ctmp"]
                w = [k for k in w if k != "ctmp"]
        op = Op()
        op.eng, op.fn, op.dma, op.epoch = eng, fn, dma, self.epoch
        op.idx = len(self.ops)
        op.signal = False
        op.sigval = 0
        op.dval = 0
        deps = set()
        for k in r:
            if k in self.lastw:
                deps.add(self.lastw[k])
        for k in w:
            if k in self.lastw:
                deps.add(self.lastw[k])
            for e, i in self.rd_eng.get(k, {}).items():
                deps.add(i)
            for i in self.rd_dma.get(k, ()):
                deps.add(i)
        for k in r:
            if dma is not None:
                self.rd_dma.setdefault(k, []).append(op.idx)
            else:
                self.rd_eng.setdefault(k, {})[eng] = op.idx
        for k in w:
            self.lastw[k] = op.idx
            self.rd_eng[k] = {}
            self.rd_dma[k] = []
        if dma is not None:
            c = self.dma_cnt.get(dma, 0) + 16
            self.dma_cnt[dma] = c
            op.dval = c
        deps.discard(op.idx)
        op.deps = deps
        self.ops.append(op)
        return op

    def finalize(self):
        ops = self.ops
        for op in ops:
            for d in op.deps:
                y = ops[d]
                if y.dma is None and not (y.eng == op.eng and op.dma is None and op.eng == "pe"):
                    y.signal = True
        cnt = {}
        for op in ops:
            if op.signal:
                k = (op.eng, op.epoch)
                cnt[k] = cnt.get(k, 0) + 1
                op.sigval = cnt[k]
        waited = {}
        for op in ops:
            need = {}
            for d in op.deps:
                y = ops[d]
                if y.dma is not None:
                    s, v = ("d", y.dma), y.dval
                elif y.eng == op.eng and op.dma is None and op.eng == "pe":
                    continue
                else:
                    s, v = ("c", y.eng, y.epoch), y.sigval
                if need.get(s, 0) < v:
                    need[s] = v
            ws = []
            wd = waited.setdefault(op.eng, {})
            for s, v in need.items():
                if wd.get(s, 0) < v:
                    wd[s] = v
                    ws.append((s, v))
            op.waits = ws
        return cnt


def _rs(a, pat, **kw):
    return a.rearrange(pat, **kw)


class _Stop(Exception):
    pass


def build_nc(stop=10 ** 9):
    def chk(i):
        if i >= stop:
            raise _Stop()
    nc = bass.Bass("TRN2", target_bir_lowering=False)
    P = Prog()

    def din(name, shape):
        return nc.dram_tensor(name, list(shape), F32, kind="ExternalInput").ap()

    def dout(name, shape):
        return nc.dram_tensor(name, list(shape), F32, kind="ExternalOutput").ap()

    xT_in = din("xT_in", [128, 8, T])
    memtok = din("memtok", [128, 2, D])
    pcol_d = din("pcol", [128, L * NPC])
    gnorm_d = din("gnorm", [L, 128, D])
    bif_d = din("bif", [4, L * 2])
    sca_d = din("sca", [128, L * 4 * NB * 2])
    scb_d = din("scb", [128, L * 4 * NB * 30])
    cnat_d = din("cnat", [L, NB, 4, 256, 256])
    ctr_d = din("ctr", [L, NB, 4, 256, 256])
    nT_d = din("nTin", [128, L * 2 * 64])
    m_d = din("min", [4, L * NB])
    kT_d = din("kTc", [L, NB, 4, 256, 256])
    v_d = din("vc", [L, NB, 256, D])
    cst_d = din("cst", [128, 1540])
    mrow_d = din("mrow", [128, 1024])
    w_in = din("w_in", [L, D, INW])
    w_oa = din("w_out_a", [L, 512, D])
    w_ob = din("w_out_b", [L, 512, D])
    w_oc = din("w_out_c", [L, D, D])
    w_o = din("w_o", [L, D, D])
    w_xq = din("w_xq", [L, D, D])
    w_xkv = din("w_xkv", [L, D, 2 * D])
    w_xo = din("w_xo", [L, D, D])
    w_fi = din("w_ffn_in", [L, D, 2 * DFF])
    w_fo = din("w_ffn_out", [L, DFF, D])

    yT = dout("yT", [128, 8, T])
    oca = dout("oca", [128, L * 4, 17 * 2])
    ocb = dout("ocb", [128, L * 4, 17 * 30])
    oC = dout("oC", [L, 17, 4, 256, 256])
    on = dout("on", [128, L, 2 * 68])
    om = dout("om", [4, L * 17])
    omk = dout("omk", [L, 256, D])
    omv = dout("omv", [L, 256, D])
    xsA = nc.dram_tensor("xsA", [128, 8, T], F32).ap()
    xsB = nc.dram_tensor("xsB", [128, 8, T], F32).ap()

    def sb(name, shape, dt=F32):
        return nc.alloc_sbuf_tensor(name, list(shape), dt) if False else nc.sbuf_tensor(name, list(shape), dt).__enter__()

    hT = sb("hT", [128, 8, T], BF16)
    big2 = sb("big2", [128, 8, T], BF16)
    ctmp = sb("ctmp", [128, 16384], F32)
    wst = sb("wst", [128, 3, 1024], F32)
    wbf = sb("wbf", [128, 3, 1024], BF16)
    cst = sb("cstsb", [128, 1540], F32)
    pcol = sb("pcolsb", [128, L * NPC], F32)
    gnb = sb("gnb", [128, D], F32)
    sqb = sb("sqb", [128, 2, 512], BF16)
    t512 = sb("t512", [128, 4, 512], F32)
    rows = sb("rows", [4, 8, 128], F32)
    bif = sb("bifsb", [4, L * 2], F32)
    gcol = sb("gcol", [128, 16], F32)
    smalls = sb("smalls", [128, 32], F32)
    wprb = sb("wprb", [128, 64], F32)
    wTt4 = sb("wTt4", [128, 4, 128], F32)
    smT4 = sb("smT4", [128, 4, 128], BF16)
    qsT4 = sb("qsT4", [128, 4, 256], BF16)
    wxs = sb("wxs", [4, 64], F32)
    eps4 = sb("eps4", [128, 16], F32)
    cnp = sb("cnp", [128, 4, 512], F32)
    lnbuf = cnp[:, 0:2, :]
    ctp = sb("ctp", [128, 4, 2 * 258], BF16)
    cts = sb("cts", [128, 2, 2 * 258], BF16)
    nT = sb("nT", [128, 2, 68], F32)
    msb = sb("msb", [4, 64], F32)
    vwb4 = sb("vwb4", [128, 4, 256], BF16)
    wlm4 = sb("wlm4", [128, 4, 16], F32)
    wlmb4 = sb("wlmb4", [128, 4, 16], BF16)
    ostA = sb("ostA", [128, 34], F32)
    ostB = sb("ostB", [128, 17 * 30], F32)
    scaS = sb("scaS", [128, 4 * NB * 2], F32)
    identb = sb("identb", [128, 128], BF16)
    onesb = sb("onesb", [128, 128], BF16)
    mrowb = sb("mrowb", [128, NB * 64], BF16)

    ident = cst[:, 0:128]
    onesf = cst[:, 128:256]
    maskP = cst[:, 256:384]
    maskS = cst[0:64, 384:448]
    maskcol = cst[0:64, 448:464]
    I4 = cst[0:4, 464:468]
    selneg = cst[0:4, 512:1024]
    selpos = cst[0:4, 1024:1536]

    def cv(off, nbytes, dt):
        a = ctmp[:, off // 4:(off + nbytes) // 4]
        return a.bitcast(dt) if dt != F32 else a

    xin2 = [_rs(cv(0, 16384, F32), "p (c t) -> p c t", c=8), _rs(cv(16384, 16384, F32), "p (c t) -> p c t", c=8)]
    xbufs = [cv(33792, 8448, F32), cv(42240, 8448, F32)]
    mrow_st = cv(16384, 4096, F32)
    cns = _rs(cv(55360, 4096, F32), "p (s n) -> p s n", s=2)
    cts_f = _rs(cv(59456, 4096, F32), "p (s n) -> p s n", s=2)
    vab = _rs(cv(0, 16896, BF16), "p (c t) -> p c t", c=4)
    abuf = cv(16896, 4224, BF16)
    ucv = [cv(21120, 5248, BF16), cv(26368, 5248, BF16)]
    dg = _rs(cv(31616, 7936, BF16), "p (k n) -> p k n", k=31)
    scbS = cv(39552, 7680, F32)
    bconv = _rs(cv(47232, 16896, BF16), "p (c t) -> p c t", c=4)
    qTs = _rs(cv(0, 8192, BF16), "p (c t) -> p c t", c=8)
    kTs = _rs(cv(8192, 8192, BF16), "p (c t) -> p c t", c=8)
    ktok = _rs(cv(16384, 8192, BF16), "p (c n) -> p c n", c=4)
    vtok = _rs(cv(24576, 8256, BF16), "p (c h n) -> p c h n", c=4, h=4)
    gob = _rs(cv(32832, 8192, BF16), "p (c n) -> p c n", c=4)
    vct = cv(41024, 2048, BF16)
    qsTm = _rs(cv(43072, 4096, BF16), "p (c b t) -> p c b t", c=2, b=NB)
    vcTs = _rs(cv(47168, 8192, BF16), "p (c t) -> p c t", c=8)
    aoT = _rs(cv(0, 33792, BF16), "p (c t) -> p c t", c=8)
    kvst = cv(33792, 2048, F32)
    oms = _rs(cv(35840, 2048, F32), "p (s n) -> p s n", s=2)
    aot = cv(37888, 2048, BF16)
    kvbf = cv(41984, 4096, BF16)
    pTm = _rs(cv(46080, 4096, BF16), "p (c b t) -> p c b t", c=2, b=NB)
    KTp = _rs(cv(50176, 4096, BF16), "p (c m) -> p c m", c=8)
    Vp = _rs(cv(54272, 4096, BF16), "p (c n) -> p c n", c=2)
    memTl = _rs(cv(58368, 4096, BF16), "p (c m) -> p c m", c=8)
    pexp = cv(62464, 2048, BF16)
    pT = cv(64512, 1024, BF16)
    memtS = _rs(cv(0, 8192, F32), "p (c n) -> p c n", c=2)
    memn = _rs(cv(8192, 8192, F32), "p (c n) -> p c n", c=2)
    memTb = sb("memTb", [128, 8, 256], BF16)

    psb = [nc.psum_tensor("ps%d" % i, [128, 512], F32).__enter__() for i in range(8)]
    ps_ctr = [0]

    ps_nb = [6]

    def PS():
        i = ps_ctr[0] % ps_nb[0]
        ps_ctr[0] += 1
        return psb[i], ("ps", i)

    psl_ctr = [0]

    def PSL():
        if ps_nb[0] == 6:
            i = 6 + psl_ctr[0] % 2
        else:
            i = 4 + psl_ctr[0] % 4
        psl_ctr[0] += 1
        return psb[i], ("ps", i)

    t5_ctr = [0]

    def T5():
        i = t5_ctr[0] % 4
        t5_ctr[0] += 1
        return t512[:, i, :], ("t5", i)

    CT = "ctmp"

    def fence():
        P.add("pool", lambda e: e.memset(smalls[:, 31:32], 0.0), r=[], w=[CT, "fencebyte"], fence=True)

    w_ctr = [0]

    def wtile(src2d, r0, nkc, c0, ncols):
        s = w_ctr[0] % 3
        w_ctr[0] += 1
        n = nkc * ncols
        src = _rs(src2d[r0:r0 + nkc * 128, c0:c0 + ncols], "(kc p) n -> p kc n", p=128)
        dst = _rs(wst[:, s, 0:n], "p (kc n) -> p kc n", kc=nkc)
        P.add("sp", lambda e: e.dma_start(out=dst, in_=src), r=[], w=[("wst", s)], dma="ws%d_%d" % (s, P.epoch))
        P.add("pool", lambda e: e.tensor_copy(out=wbf[:, s, 0:n], in_=wst[:, s, 0:n]), r=[("wst", s)], w=[("wbf", s)])
        return _rs(wbf[:, s, 0:n], "p (kc n) -> p kc n", kc=nkc), ("wbf", s)

    def mm(out, lhsT, rhs, start, stop, r, w):
        P.add("pe", lambda e: e.matmul(out, lhsT=lhsT, rhs=rhs, start=start, stop=stop), r=r, w=w)

    def proj(wt, wk, col0, src, srck, nkc, tiles, consume):
        for ti, (t0, n) in enumerate(tiles):
            ps, pk = PS()
            for kc in range(nkc):
                mm(ps[:, 0:n], wt[:, kc, col0:col0 + 128], src(kc, t0, n), kc == 0, kc == nkc - 1,
                   [wk] + srck, [pk])
            consume(ti, t0, n, ps, pk)

    def act(out, in_, func, r, w, bias=0.0, scale=1.0, accum=None):
        if accum is None:
            P.add("act", lambda e: e.activation(out, in_, func, bias=bias, scale=scale), r=r, w=w)
        else:
            P.add("act", lambda e: e.activation(out, in_, func, bias=bias, scale=scale, accum_out=accum), r=r, w=w)

    def tt(eng, out, a, b, op, r, w):
        P.add(eng, lambda e: e.tensor_tensor(out, a, b, op), r=r, w=w)

    def stt(out, a, s, b, op0, op1, r, w):
        P.add("dve", lambda e: e.scalar_tensor_tensor(out, a, s, b, op0, op1), r=r, w=w)

    def ts(eng, out, a, s1, s2, op0, op1, r, w):
        if s2 is None:
            P.add(eng, lambda e: e.tensor_scalar(out, a, s1, None, op0), r=r, w=w)
        else:
            P.add(eng, lambda e: e.tensor_scalar(out, a, s1, s2, op0, op1), r=r, w=w)

    def cp(eng, out, a, r, w):
        if eng == "act":
            P.add("act", lambda e: e.activation(out, a, AF.Copy), r=r, w=w)
        else:
            P.add(eng, lambda e: e.tensor_copy(out=out, in_=a), r=r, w=w)

    def dma(out, in_, r, w, sem):
        P.add("sp", lambda e: e.dma_start(out=out, in_=in_), r=r, w=w, dma=sem)

    HK = [("hT", c) for c in range(8)]

    def rmsnorm(xsrc, xsk, gcolbase, dst_fn):
        for ti, (t0, n) in enumerate(TT):
            xin, xik = xin2[ti % 2], ("xin", ti % 2)
            dma(xin[:, :, 0:n], xsrc[:, :, t0:t0 + n], [(xsk, ti)], [xik], "xin%d" % (ti % 2))
            ps, pk = PS()
            for c in range(8):
                act(sqb[:, c % 2, 0:n], xin[:, c, 0:n], AF.Square, [xik], [("sqb", c % 2)])
                mm(ps[:, 0:n], onesb[:, :], sqb[:, c % 2, 0:n], c == 0, c == 7, [("sqb", c % 2), "constsb_o"], [pk])
            rs, rk = T5()
            act(rs[:, 0:n], ps[:, 0:n], AF.Sqrt, [pk], [rk], bias=EPS, scale=1.0 / D)
            P.add("dve", lambda e, a=rs[:, 0:n]: e.reciprocal(a, a), r=[rk], w=[rk])
            for c in range(8):
                o, ok = dst_fn(c, t0, n)
                stt(o, xin[:, c, 0:n], pcol[:, gcolbase + c:gcolbase + c + 1], rs[:, 0:n], ALU.mult, ALU.mult,
                    [xik, rk, "pcol"], ok)

    def hT_dst(c, t0, n):
        return hT[:, c, t0:t0 + n], [("hT", c)]

    def hsrc(kc, t0, n):
        return hT[:, kc, t0:t0 + n]

    dma(cst[:, :], cst_d, [], ["consts"], "init1")
    dma(pcol[:, :], pcol_d, [], ["pcol"], "init2")
    dma(bif[:, :], bif_d, [], ["bif"], "init3")
    dma(memtS, memtok, [], [CT, "memtS"], "init4")
    cp("dve", identb[:, :], ident, ["consts"], ["constsb_i"])
    cp("dve", onesb[:, :], onesf, ["consts"], ["constsb_o"])
    dma(mrow_st, mrow_d, [], [CT, "mrowst"], "init5")
    cp("dve", mrowb[:, :], mrow_st, ["mrowst", CT], ["mrowb"])
    P.add("pool", lambda e: e.memset(_rs(vtok, "p c h n -> p (c h) n")[:, :, 256:258], 1.0), r=[], w=[CT])
    for mc in range(2):
        jk, jkk = T5()
        act(jk[:, 0:512], memtS[:, mc, 0:512], AF.Square, [CT, "memtS"], [jkk], accum=smalls[:, mc * 2:mc * 2 + 1])
        jk2, jkk2 = T5()
        act(jk2[:, 0:512], memtS[:, mc, 512:1024], AF.Square, [CT, "memtS"], [jkk2, "sm0"], accum=smalls[:, mc * 2 + 1:mc * 2 + 2])
        tt("dve", smalls[:, 4 + mc:5 + mc], smalls[:, mc * 2:mc * 2 + 1], smalls[:, mc * 2 + 1:mc * 2 + 2], ALU.add,
           [jkk, jkk2, "sm0"], ["sm1"])
        act(smalls[:, 6 + mc:7 + mc], smalls[:, 4 + mc:5 + mc], AF.Sqrt, ["sm1"], ["sm2"], bias=EPS, scale=1.0 / D)
        P.add("dve", lambda e, a=smalls[:, 6 + mc:7 + mc]: e.reciprocal(a, a), r=["sm2"], w=["sm3"])
        ts("dve", memn[:, mc, :], memtS[:, mc, :], smalls[:, 6 + mc:7 + mc], None, ALU.mult, None, ["sm3", CT, "memtS"], [CT, "memn"])
        for g in range(2):
            ps, pk = PS()
            for q in range(4):
                c = g * 4 + q
                P.add("pe", lambda e, o=ps[:, q * 128:(q + 1) * 128], i=memn[:, mc, c * 128:(c + 1) * 128]:
                      e.transpose(o, i, ident), r=["memn", "consts"], w=[pk])
            cp("dve", memTb[:, g * 4:(g + 1) * 4, mc * 128:(mc + 1) * 128],
               _rs(ps[:, :], "p (q t) -> p q t", q=4), [pk], ["memTb"])
    P.add("pool", lambda e: e.memset(nT[:, :, 0:4], 0.0), r=[], w=["nT"])
    fence()

    xcur, xk = xT_in, "x0"
    xdsts = [(xsA, "xA"), (xsB, "xB")]
    xflip = [0]

    xb_ctr = [0]

    def residual(wsrc2d, nk, srcfn, srckeys):
        nonlocal xcur, xk
        xn, xnk = xdsts[xflip[0] % 2]
        xflip[0] += 1
        xsrc_, xsk_ = xcur, xk

        def pre(j):
            wt, wk = wtile(wsrc2d, 0, nk, j * 128, 128)
            i = xb_ctr[0] % 2
            xb_ctr[0] += 1
            dma(xbufs[i][:, :], xsrc_[:, j, :], [(xsk_, t) for t in range(5)], [("xb", i)], "xbl%d" % i)
            return wt, wk, i
        nxt = pre(0)
        for j in range(8):
            wt, wk, i = nxt
            if j + 1 < 8:
                nxt = pre(j + 1)

            def consume(ti, t0, n, ps, pk, i=i):
                tt("dve", xbufs[i][:, t0:t0 + n], ps[:, 0:n], xbufs[i][:, t0:t0 + n], ALU.add, [pk, ("xb", i)], [("xb", i)])
            proj(wt, wk, 0, srcfn, srckeys, nk, TT, consume)
            dma(xn[:, j, :], xbufs[i][:, :], [("xb", i)], [(xnk, t) for t in range(5)], "xbs%d" % i)
        xcur, xk = xn, xnk

    def layers():
      for l in range(L):
        layer(l)

    def layer(l):
        nonlocal xcur, xk
        P.epoch = l
        pc = l * NPC
        Win = w_in[l]
        chk(l * 10 + 1)
        rmsnorm(xcur, xk, pc + PC_NMIX, hT_dst)
        fence()
        dma(gnb[:, :], gnorm_d[l], [], ["gnb"], "ld0_1")
        dma(scaS[:, :], sca_d[:, l * 128:(l + 1) * 128], [], ["scaS"], "ld0_2")
        dma(scbS, scb_d[:, l * 1920:(l + 1) * 1920], [], [CT, "scbS"], "ld0_3")
        dma(nT[:, :, 4:68], _rs(nT_d[:, l * 128:(l + 1) * 128], "p (c n) -> p c n", c=2), [], ["nT"], "ld0_4")
        dma(msb[:, 0:16], m_d[:, l * NB:(l + 1) * NB], [], ["msb"], "ld0_5")
        wg, wgk0 = None, None

        def conv_stage(K, colbase, halo_src, j, ub, ubk, consume):
            wv = pcol[:, colbase + j * K:colbase + (j + 1) * K]
            P.add("pool", lambda e: e.tensor_tensor(dg[:, 0:K, :], ident.unsqueeze(1).to_broadcast([128, K, 128]),
                                                    wv.unsqueeze(2).to_broadcast([128, K, 128]), ALU.mult),
                  r=["pcol", "consts"], w=[CT, "dg"])
            for ti, (t0, n) in enumerate(TT):
                ps, pk = PS()
                for k in range(K):
                    off = 30 - (K - 1) + k
                    if ti < 4:
                        rhs = ub[:, t0 + off:t0 + off + n]
                    else:
                        rhs = _rs(ub[:, 2078:2078 + NB * 34], "p (b t) -> p b t", b=NB)[:, :, off:off + 4]
                    mm(ps[:, 0:n], dg[:, k, :], rhs, k == 0, k == K - 1, ["dg", ubk, CT], [pk])
                consume(ti, t0, n, ps, pk)

        def ustage(colP, colG, gfunc, j, ub, ubk, halo_view, Kh, tailout):
            wtG, wkG = wtile(Win, 0, 8, colG + j * 128, 128)
            wtP, wkP = wtile(Win, 0, 8, colP + j * 128, 128)
            ubs = _rs(ub[:, 2078:2078 + NB * 34], "p (b t) -> p b t", b=NB)
            cp("pool", ubs[:, :, 30 - Kh:30], halo_view, ["scaS", "scbS", CT], [ubk, CT])
            for ti, (t0, n) in enumerate(TT):
                psG, pkG = PS()
                for kc in range(8):
                    mm(psG[:, 0:n], wtG[:, kc, :], hsrc(kc, t0, n), kc == 0, kc == 7, [wkG] + HK, [pkG])
                tg, tgk = T5()
                act(tg[:, 0:n], psG[:, 0:n], gfunc, [pkG], [tgk])
                psP, pkP = PS()
                for kc in range(8):
                    mm(psP[:, 0:n], wtP[:, kc, :], hsrc(kc, t0, n), kc == 0, kc == 7, [wkP] + HK, [pkP])
                if ti < 4:
                    tt("dve", ub[:, 30 + t0:30 + t0 + n], psP[:, 0:n], tg[:, 0:n], ALU.mult, [pkP, tgk], [ubk, CT])
                    if ti == 3:
                        tt("dve", tailout[:, 0:Kh], psP[:, 512 - Kh:512], tg[:, 512 - Kh:512], ALU.mult,
                           [pkP, tgk], ["ost"])
                else:
                    tt("dve", ubs[:, :, 30:34], _rs(psP[:, 0:64], "p (b t) -> p b t", b=NB),
                       _rs(tg[:, 0:64], "p (b t) -> p b t", b=NB), ALU.mult, [pkP, tgk], [ubk, CT])
                    so = _rs(tailout[:, Kh:17 * Kh], "p (b t) -> p b t", b=NB)
                    if Kh >= 4:
                        tt("dve", so[:, :, Kh - 4:Kh], _rs(psP[:, 0:64], "p (b t) -> p b t", b=NB),
                           _rs(tg[:, 0:64], "p (b t) -> p b t", b=NB), ALU.mult, [pkP, tgk], ["ost"])
                    else:
                        tt("dve", so[:, :, 0:Kh], _rs(psP[:, 0:64], "p (b t) -> p b t", b=NB)[:, :, 4 - Kh:4],
                           _rs(tg[:, 0:64], "p (b t) -> p b t", b=NB)[:, :, 4 - Kh:4], ALU.mult, [pkP, tgk], ["ost"])

        chk(l * 10 + 2)
        P.add("pool", lambda e: e.memset(ucv[0][:, 0:30], 0.0), r=[], w=[CT, ("ucv", 0)])
        P.add("pool", lambda e: e.memset(ucv[1][:, 0:30], 0.0), r=[], w=[CT, ("ucv", 1)])
        for j in range(4):
            ub, ubk = ucv[j % 2], ("ucv", j % 2)
            hv = _rs(scaS[:, j * 32:(j + 1) * 32], "p (b t) -> p b t", b=NB)
            ustage(C_AX, C_AC, AF.Copy, j, ub, ubk, hv, 2, ostA)
            dma(oca[:, l * 4 + j, :], ostA[:, :], ["ost"], [], "oca")
            wtB, wkB = wtile(Win, 0, 8, C_AB + j * 128, 128)
            for ti, (t0, n) in enumerate(TT):
                psb_, pkb = PS()
                for kc in range(8):
                    mm(psb_[:, 0:n], wtB[:, kc, :], hsrc(kc, t0, n), kc == 0, kc == 7, [wkB] + HK, [pkb])
                cp("act", abuf[:, t0:t0 + n], psb_[:, 0:n], [pkb], [CT, "abuf"])

            def consA(ti, t0, n, ps, pk, j=j):
                tt("dve", vab[:, j, t0:t0 + n], ps[:, 0:n], abuf[:, t0:t0 + n], ALU.mult, [pk, "abuf"], [CT, ("vab", j)])
            conv_stage(3, pc + PC_CAW, None, j, ub, ubk, consA)

        def merge_stage(first, gcol0, wsrc2d, nk, srcfn, srckeys, tiles):
            for j in range(8):
                wtG, wkG = wtile(Win, 0, 8, gcol0 + j * 128, 128)
                wtY, wkY = wtile(wsrc2d, 0, nk, j * 128, 128)
                for ti, (t0, n) in enumerate(tiles):
                    psG, pkG = PS()
                    for kc in range(8):
                        mm(psG[:, 0:n], wtG[:, kc, :], hsrc(kc, t0, n), kc == 0, kc == 7, [wkG] + HK, [pkG])
                    sg, sgk = T5()
                    act(sg[:, 0:n], psG[:, 0:n], AF.Sigmoid, [pkG], [sgk])
                    psY, pkY = PS()
                    for kc in range(nk):
                        mm(psY[:, 0:n], wtY[:, kc, :], srcfn(kc, t0, n), kc == 0, kc == nk - 1, [wkY] + srckeys, [pkY])
                    if first:
                        tt("dve", big2[:, j, t0:t0 + n], psY[:, 0:n], sg[:, 0:n], ALU.mult, [pkY, sgk], [("u", j)])
                    else:
                        tt("dve", sg[:, 0:n], psY[:, 0:n], sg[:, 0:n], ALU.mult, [pkY, sgk], [sgk])
                        tt("pool", big2[:, j, t0:t0 + n], big2[:, j, t0:t0 + n], sg[:, 0:n], ALU.add, [sgk, ("u", j)], [("u", j)])

        chk(l * 10 + 3)
        merge_stage(True, C_GA, w_oa[l], 4, lambda kc, t0, n: vab[:, kc, t0:t0 + n], [CT] + [("vab", c) for c in range(4)], TT)

        chk(l * 10 + 4)
        for j in range(4):
            ub, ubk = ucv[j % 2], ("ucv", j % 2)
            hv = _rs(scbS[:, j * 480:(j + 1) * 480], "p (b t) -> p b t", b=NB)
            cp("pool", _rs(ostB[:, 30:510], "p (b t) -> p b t", b=NB)[:, :, 0:26], hv[:, :, 4:30], ["scbS", CT], ["ost"])
            ustage(C_BV, C_BG, AF.Sigmoid, j, ub, ubk, hv, 30, ostB)
            dma(ocb[:, l * 4 + j, :], ostB[:, :], ["ost"], [], "ocb")

            def consB(ti, t0, n, ps, pk, j=j):
                act(bconv[:, j, t0:t0 + n], ps[:, 0:n], AF.Identity, [pk, "pcol"], [CT, ("bconv", j)],
                    bias=pcol[:, pc + PC_CBB + j:pc + PC_CBB + j + 1])
            conv_stage(31, pc + PC_CBW, None, j, ub, ubk, consB)
        BK = [("bconv", c) for c in range(4)]
        for ti, (t0, n) in enumerate(TT):
            ps1, pk1 = PS()
            for c in range(4):
                mm(ps1[:, 0:n], onesb[:, :], bconv[:, c, t0:t0 + n], c == 0, c == 3, BK + [CT, "constsb_o"], [pk1])
            ps2, pk2 = PS()
            for c in range(4):
                act(sqb[:, c % 2, 0:n], bconv[:, c, t0:t0 + n], AF.Square, BK + [CT], [("sqb", c % 2)])
                mm(ps2[:, 0:n], onesb[:, :], sqb[:, c % 2, 0:n], c == 0, c == 3, [("sqb", c % 2)], [pk2])
            mu, muk = lnbuf[:, 0, :], ("cnp", 0)
            act(mu[:, 0:n], ps1[:, 0:n], AF.Copy, [pk1], [muk], scale=1.0 / 512)
            rs, rk = lnbuf[:, 1, :], ("cnp", 1)
            tt("dve", rs[:, 0:n], mu[:, 0:n], mu[:, 0:n], ALU.mult, [muk], [rk])
            stt(rs[:, 0:n], ps2[:, 0:n], 1.0 / 512, rs[:, 0:n], ALU.mult, ALU.subtract, [pk2, rk], [rk])
            act(rs[:, 0:n], rs[:, 0:n], AF.Sqrt, [rk], [rk], bias=EPS)
            P.add("dve", lambda e, a=rs[:, 0:n]: e.reciprocal(a, a), r=[rk], w=[rk])
            for c in range(4):
                xc, xck = T5()
                tt("dve", xc[:, 0:n], bconv[:, c, t0:t0 + n], mu[:, 0:n], ALU.subtract, BK + [CT, muk], [xck])
                stt(xc[:, 0:n], xc[:, 0:n], pcol[:, pc + PC_LNG + c:pc + PC_LNG + c + 1], rs[:, 0:n], ALU.mult, ALU.mult,
                    [xck, rk, "pcol"], [xck])
                act(vab[:, c, t0:t0 + n], xc[:, 0:n], AF.Silu, [xck, "pcol"], [CT, ("vab", c)],
                    bias=pcol[:, pc + PC_LNB + c:pc + PC_LNB + c + 1])
        chk(l * 10 + 5)
        merge_stage(False, C_GB, w_ob[l], 4, lambda kc, t0, n: vab[:, kc, t0:t0 + n], [CT] + [("vab", c) for c in range(4)], TT)
        fence()

        chk(l * 10 + 6)
        P.add("pool", lambda e: e.memset(_rs(vtok, "p c h n -> p (c h) n")[:, :, 256:258], 1.0), r=[], w=[CT, "vtok"])
        P.add("pool", lambda e: e.memset(cnp[:, :, :], 0.0), r=[], w=[("cnp", h_) for h_ in range(4)])
        P.add("pool", lambda e: e.memset(ctp[:, :, :], 0.0), r=[], w=[("ctp", h_) for h_ in range(4)])
        P.add("pool", lambda e: e.memset(nT[:, :, 0:4], 0.0), r=[], w=["nT"])
        P.add("pool", lambda e: e.memset(msb[:, 16:20], 0.0), r=[], w=["carry"])
        wgt, wgk = wtile(Win, 0, 8, C_IF, 8)
        wgb = sb("wgb%d" % l, [128, 8, 8], BF16)
        cp("pool", wgb[:, :, :], wgt, [wgk], [("wgb", l)])
        R = lambda i: rows[:, i, :]
        for sc in range(5):
            t0s, ns = TT[sc]
            samp = sc == 4
            Lc = 64 if samp else 128
            ntc = 1 if samp else 4
            nseq = NB if samp else 1
            tiles_sc = [(t0s, ns)]
            for c in range(8):
                wtq, wkq = wtile(Win, 0, 8, C_Q + c * 128, 128)

                def cq(ti, t0, n, ps, pk, c=c):
                    cp("act", qTs[:, c, 0:n], ps[:, 0:n], [pk], [CT, "qTs"])
                proj(wtq, wkq, 0, hsrc, HK, 8, tiles_sc, cq)
                wtk, wkk = wtile(Win, 0, 8, C_K + c * 128, 128)

                def ck(ti, t0, n, ps, pk, c=c):
                    act(kTs[:, c, 0:n], ps[:, 0:n], AF.Copy, [pk], [CT, "kTs"], scale=0.0625)
                proj(wtk, wkk, 0, hsrc, HK, 8, tiles_sc, ck)
                for kind, col in (("k", C_K), ("v", C_V), ("o", C_O)):
                    if kind == "k":
                        wtt, wkt = wtk, wkk
                    else:
                        wtt, wkt = wtile(Win, 0, 8, col + c * 128, 128)
                    for tc in range(ntc):
                        ps, pk = PS()
                        ta = t0s + tc * 128
                        for kc in range(8):
                            mm(ps[0:Lc, 0:128], hT[:, kc, ta:ta + Lc], wtt[:, kc, :], kc == 0, kc == 7, [wkt] + HK, [pk])
                        if kind == "k":
                            act(ktok[0:Lc, tc, c * 128:(c + 1) * 128], ps[0:Lc, 0:128], AF.Copy, [pk], [CT, "ktok"], scale=0.0625)
                        elif kind == "v":
                            cp("dve", vtok[0:Lc, tc, c // 2, (c % 2) * 128:(c % 2) * 128 + 128], ps[0:Lc, 0:128], [pk], [CT, "vtok"])
                        else:
                            so_, sok = T5()
                            act(so_[0:Lc, 0:128], ps[0:Lc, 0:128], AF.Sigmoid, [pk], [sok])
                            tt("pool", gob[0:Lc, tc, c * 128:(c + 1) * 128], so_[0:Lc, 0:128], gnb[0:Lc, c * 128:(c + 1) * 128],
                               ALU.mult, [sok, "gnb"], [CT, "gob"])
            for tc in range(ntc):
                ta = t0s + tc * 128
                lo = tc * 128
                for gi_, (ro, co, bo) in enumerate(((0, 0, 0), (1, 4, 1))):
                    ps, pk = PS()
                    for kc in range(8):
                        mm(ps[0:4, 0:Lc], wgb[:, kc, co:co + 4], hT[:, kc, ta:ta + Lc], kc == 0, kc == 7, [("wgb", l)] + HK, [pk])
                    act(R(ro)[:, 0:Lc], ps[0:4, 0:Lc], AF.Identity, [pk, "bif"], [("row", ro)], bias=bif[:, l * 2 + bo:l * 2 + bo + 1])
                ts("dve", R(7)[:, 0:Lc], R(1)[:, 0:Lc], -1.0, None, ALU.mult, None, [("row", 1)], [("row", 7)])
                tt("dve", R(7)[:, 0:Lc], R(7)[:, 0:Lc], R(1)[:, 0:Lc], ALU.max, [("row", 1), ("row", 7)], [("row", 7)])
                act(R(7)[:, 0:Lc], R(7)[:, 0:Lc], AF.Exp, [("row", 7)], [("row", 7)], scale=-1.0)
                act(R(7)[:, 0:Lc], R(7)[:, 0:Lc], AF.Ln, [("row", 7)], [("row", 7)], bias=1.0)
                stt(R(1)[:, 0:Lc], R(1)[:, 0:Lc], 0.0, R(7)[:, 0:Lc], ALU.min, ALU.subtract, [("row", 1), ("row", 7)], [("row", 1)])
                if not samp:
                    P.add("dve", lambda e: e.tensor_tensor_scan(R(2)[:, 0:128], cst[0:4, 1536:1537].to_broadcast([4, 128]), R(1)[:, 0:128],
                                                                msb[:, 16:17], ALU.mult, ALU.add), r=[("row", 1), "carry", "consts"], w=[("row", 2)])
                    tt("dve", R(0)[:, 0:128], R(0)[:, 0:128], R(2)[:, 0:128], ALU.subtract, [("row", 0), ("row", 2)], [("row", 0)])
                    P.add("dve", lambda e: e.tensor_tensor_scan(R(3)[:, 0:128], cst[0:4, 1537:1538].to_broadcast([4, 128]), R(0)[:, 0:128],
                                                                msb[:, 17:18], ALU.add, ALU.max), r=[("row", 0), "carry", "consts"], w=[("row", 3)])
                    Mend = R(3)[:, 127:128].to_broadcast([4, 128])
                    Mprev = msb[:, 17:18].to_broadcast([4, 128])
                    tt("dve", msb[:, 20:21], msb[:, 17:18], R(3)[:, 127:128], ALU.subtract, ["carry", ("row", 3)], ["wpr"])
                    npair = 1
                else:
                    v3 = lambda i: _rs(R(i)[:, 0:64], "p (b t) -> p b t", b=NB)
                    cp("dve", v3(2)[:, :, 0:1], v3(1)[:, :, 0:1], [("row", 1)], [("row", 2)])
                    for t_ in range(1, 4):
                        tt("dve", v3(2)[:, :, t_:t_ + 1], v3(2)[:, :, t_ - 1:t_], v3(1)[:, :, t_:t_ + 1], ALU.add, [("row", 1), ("row", 2)], [("row", 2)])
                    tt("dve", R(0)[:, 0:64], R(0)[:, 0:64], R(2)[:, 0:64], ALU.subtract, [("row", 0), ("row", 2)], [("row", 0)])
                    m0 = msb[:, 0:16].unsqueeze(2)
                    tt("dve", v3(3)[:, :, 0:1], v3(0)[:, :, 0:1], m0, ALU.max, [("row", 0), "msb"], [("row", 3)])
                    for t_ in range(1, 4):
                        tt("dve", v3(3)[:, :, t_:t_ + 1], v3(3)[:, :, t_ - 1:t_], v3(0)[:, :, t_:t_ + 1], ALU.max, [("row", 0), ("row", 3)], [("row", 3)])
                    Mend = v3(3)[:, :, 3:4].to_broadcast([4, NB, 4])
                    Mprev = m0.to_broadcast([4, NB, 4])
                    tt("dve", R(7)[:, 64:80].unsqueeze(2), m0, v3(3)[:, :, 3:4], ALU.subtract, ["msb", ("row", 3)], ["wpr"])
                    npair = NB
                Lv = (lambda i: R(i)[:, 0:Lc]) if not samp else (lambda i: _rs(R(i)[:, 0:64], "p (b t) -> p b t", b=NB))
                tt("dve", Lv(4), Lv(0), Mend, ALU.subtract, [("row", 0), ("row", 3)], [("row", 4)])
                act(R(4)[:, 0:Lc], R(4)[:, 0:Lc], AF.Exp, [("row", 4)], [("row", 4)])
                tt("dve", R(5)[:, 0:Lc], R(2)[:, 0:Lc], R(3)[:, 0:Lc], ALU.add, [("row", 2), ("row", 3)], [("row", 5)])
                if samp:
                    cp("dve", msb[:, 32:48].unsqueeze(2), _rs(R(5)[:, 0:64], "p (b t) -> p b t", b=NB)[:, :, 3:4], [("row", 5)], ["mout"])
                    dma(om[:, l * 17 + 1:l * 17 + 17], msb[:, 32:48], ["mout"], [], "om_s")
                elif sc == 3 and tc == 3:
                    cp("dve", msb[:, 24:25], R(5)[:, 127:128], [("row", 5)], ["moutp"])
                    dma(om[:, l * 17:l * 17 + 1], msb[:, 24:25], ["moutp"], [], "om_p")
                act(R(5)[:, 0:Lc], R(5)[:, 0:Lc], AF.Exp, [("row", 5)], [("row", 5)], scale=-1.0)
                tt("dve", Lv(6), Mprev, Lv(3), ALU.subtract, [("row", 3), "carry", "msb"], [("row", 6)])
                act(R(6)[:, 0:Lc], R(6)[:, 0:Lc], AF.Exp, [("row", 6)], [("row", 6)])
                if not samp:
                    act(msb[:, 20:21], msb[:, 20:21], AF.Exp, ["wpr"], ["wpr"])
                    ts("dve", R(7)[:, 96:100], I4, msb[:, 20:21], None, ALU.mult, None, ["wpr", "consts"], ["wprx"])
                    ps, pk = PS()
                    mm(ps[:, 0:4], onesf[0:4, :], R(7)[:, 96:100], True, True, ["wprx", "consts"], [pk])
                    cp("dve", wprb[:, 0:4], ps[:, 0:4], [pk], ["wprb"])
                    cp("dve", msb[:, 16:17], R(2)[:, 127:128], [("row", 2)], ["carry"])
                    cp("dve", msb[:, 17:18], R(3)[:, 127:128], [("row", 3), ("row", 6), "wpr"], ["carry"])
                else:
                    act(R(7)[:, 64:80], R(7)[:, 64:80], AF.Exp, ["wpr"], ["wpr"])
                    wx = _rs(wxs[0:4, 0:64], "p (b h) -> p b h", b=NB)
                    tt("dve", wx, R(7)[:, 64:80].unsqueeze(2).to_broadcast([4, NB, 4]), I4.unsqueeze(1).to_broadcast([4, NB, 4]),
                       ALU.mult, ["wpr", "consts"], ["wprx"])
                    ps, pk = PS()
                    mm(ps[:, 0:64], onesf[0:4, :], wxs[0:4, 0:64], True, True, ["wprx", "consts"], [pk])
                    cp("dve", wprb[:, 0:64], ps[:, 0:64], [pk], ["wprb"])
                ps, pk = PS()
                for q, ri in enumerate((0, 4, 5, 6)):
                    mm(ps[0:Lc, q * 4:q * 4 + 4], R(ri)[:, 0:Lc], I4, True, True, [("row", ri), "consts"], [pk])
                cp("dve", gcol[0:Lc, :], ps[0:Lc, 0:16], [pk], ["gcol"])
                def head_gen(h):
                    wT_ = wTt4[0:Lc, h, 0:Lc]
                    smT_ = smT4[0:Lc, h, 0:Lc]
                    qsT_ = _rs(qsT4[:, h, :], "p (c t) -> p c t", c=2)
                    wlm_ = wlm4[:, h, :]
                    wlmb_ = wlmb4[:, h, :]
                    e0 = h * 4
                    K = lambda nm: (nm, h)
                    psq, pkq = PS()
                    for dc in range(2):
                        mm(psq[0:Lc, 0:Lc], kTs[:, h * 2 + dc, lo:lo + Lc], qTs[:, h * 2 + dc, lo:lo + Lc], dc == 0, dc == 1,
                           ["qTs", "kTs", CT], [pkq])
                    psm, pkm = PS()
                    mm(psm[0:Lc, 0:Lc], selneg[:, h * 128:h * 128 + Lc], R(3)[:, 0:Lc], True, False, [("row", 3), "consts"], [pkm])
                    mm(psm[0:Lc, 0:Lc], ident[0:Lc, 0:Lc], (maskS if samp else maskP), False, True, ["consts"], [pkm])
                    act(wT_, psm[0:Lc, 0:Lc], AF.Exp, [pkm, "gcol"], [K("wTt")], bias=gcol[0:Lc, h:h + 1])
                    tt("dve", smT_, psq[0:Lc, 0:Lc], wT_, ALU.mult, [pkq, K("wTt")], [K("smT")])
                    yield
                    psw, pkw = PS()
                    mm(psw[:, 0:Lc], selpos[:, h * 128:(h + 1) * 128], R(6)[:, 0:Lc], True, True, [("row", 6), "consts"], [pkw])
                    for dc in range(2):
                        tt("dve", qsT_[:, dc, 0:Lc], qTs[:, h * 2 + dc, lo:lo + Lc], psw[:, 0:Lc], ALU.mult, [pkw, "qTs", CT], [K("qsT")])
                    if samp:
                        for dc in range(2):
                            tt("pool", qsTm[:, dc, :, :], qsT_[:, dc, 0:64].unsqueeze(1).to_broadcast([128, NB, 64]),
                               _rs(mrowb[:, :], "p (b t) -> p b t", b=NB), ALU.mult, [K("qsT"), "mrowb"], [CT, "qsTm"])
                        ts("dve", wlm_[0:64, :], maskcol, gcol[0:64, 4 + h:5 + h], None, ALU.mult, None, ["gcol", "consts"], [K("wlm")])
                        cp("dve", wlmb_[0:64, :], wlm_[0:64, :], [K("wlm")], [K("wlmb")])
                    else:
                        cp("dve", wlmb_[0:128, 0:1], gcol[0:128, 4 + h:5 + h], ["gcol"], [K("wlmb")])
                    yield
                    pso, pko = PSL()
                    mm(pso[0:Lc, 0:257], smT_, vtok[0:Lc, tc, h, 0:257], True, False, [K("smT"), "vtok", CT], [pko])
                    for b in range(nseq):
                        if samp:
                            s2 = (b + h * NB) % 2
                            pidx = 4 + b * 4 + h
                            dma(_rs(cts_f[:, s2, :], "p (c e) -> p c e", c=2), _rs(ctr_d[l, b, h], "(c p) e -> p c e", p=128),
                                [], [("ctsf", s2), CT], "ctsf%d" % s2)
                            dma(_rs(cns[:, s2, :], "p (c e) -> p c e", c=2), _rs(cnat_d[l, b, h], "(c p) e -> p c e", p=128),
                                [], [("cns", s2), CT], "cns%d" % s2)
                            ctv = _rs(cts[:, s2, :], "p (c e) -> p c e", c=2)
                            cp("act", ctv[:, :, 0:256], _rs(cts_f[:, s2, :], "p (c e) -> p c e", c=2), [("ctsf", s2)], [("cts", s2)])
                            cp("pool", ctv[:, :, 256:257], nT[:, :, pidx:pidx + 1], ["nT"], [("cts", s2)])
                            ctk = ("cts", s2)
                            cnv, cnk = cns[:, s2, :], ("cns", s2)
                            lhs_q = lambda dc, b=b: qsTm[:, dc, b, :]
                            qk_ = ["qsTm", CT]
                            wpc = wprb[:, b * 4 + h:b * 4 + h + 1]
                            wl_col = wlm_[0:64, b:b + 1]
                            wl_colb = wlmb_[0:64, b:b + 1]
                        else:
                            pidx = h
                            ctv = _rs(ctp[:, h, :], "p (c e) -> p c e", c=2)
                            ctk = ("ctp", h)
                            cnv, cnk = cnp[:, h, :], ("cnp", h)
                            lhs_q = lambda dc: qsT_[:, dc, 0:128]
                            qk_ = [K("qsT")]
                            wpc = wprb[:, h:h + 1]
                            wl_col = gcol[0:128, 4 + h:5 + h]
                            wl_colb = wlmb_[0:128, 0:1]
                        for dc in range(2):
                            mm(pso[0:Lc, 0:257], lhs_q(dc), ctv[:, dc, 0:257], False, (b == nseq - 1 and dc == 1), qk_ + [ctk], [pko])
                        if not samp:
                            yield
                        ts("dve", vwb4[0:Lc, h, :], vtok[0:Lc, tc, h, 0:256], wl_col, None, ALU.mult, None, ["vtok", CT, "gcol", K("wlm")], [K("vwb")])
                        psu, pku = PS()
                        for ec in range(2):
                            mm(psu[:, ec * 256:(ec + 1) * 256], vwb4[0:Lc, h, ec * 128:(ec + 1) * 128], ktok[0:Lc, tc, h * 256:(h + 1) * 256],
                               True, True, [K("vwb"), "ktok", CT], [pku])
                        stt(cnv, cnv, wpc, psu[:, :], ALU.mult, ALU.add, [cnk, "wprb", pku], [cnk])
                        psn, pkn = PS()
                        for dc in range(2):
                            mm(psn[:, dc:dc + 1], ktok[0:Lc, tc, h * 256 + dc * 128:h * 256 + dc * 128 + 128], wl_colb, True, True,
                               ["ktok", CT, K("wlmb")], [pkn])
                        stt(nT[:, :, pidx], nT[:, :, pidx], wpc, psn[:, 0:2], ALU.mult, ALU.add, ["nT", "wprb", pkn, ctk], ["nT"])
                        if samp:
                            dma(_rs(oC[l, 1 + b, h], "(c p) d -> p c d", p=128), _rs(cnv, "p (c d) -> p c d", c=2), [cnk], [], "oc%d" % s2)
                        else:
                            yield
                            for dcc in range(2):
                                pst, pkt = PS()
                                for ec in range(2):
                                    P.add("pe", lambda e, o=pst[:, ec * 128:(ec + 1) * 128], i=cnp[:, h, ec * 256 + dcc * 128:ec * 256 + dcc * 128 + 128]:
                                          e.transpose(o, i, ident), r=[("cnp", h), "consts"], w=[pkt])
                                cp("act", ctv[:, dcc, 0:256], pst[:, 0:256], [pkt], [("ctp", h)])
                            cp("pool", ctv[:, :, 256:257], nT[:, :, h:h + 1], ["nT"], [("ctp", h)])
                            if sc == 3 and tc == 3:
                                dma(_rs(oC[l, 0, h], "(c p) d -> p c d", p=128), _rs(cnv, "p (c d) -> p c d", c=2), [cnk], [], "ocp%d" % h)
                            yield
                    sm = eps4
                    ts("dve", sm[0:Lc, e0:e0 + 1], pso[0:Lc, 256:257], -1.0, None, ALU.mult, None, [pko], [K("ep0")])
                    tt("dve", sm[0:Lc, e0:e0 + 1], sm[0:Lc, e0:e0 + 1], pso[0:Lc, 256:257], ALU.max, [pko, K("ep0")], [K("ep0")])
                    tt("dve", sm[0:Lc, e0:e0 + 1], sm[0:Lc, e0:e0 + 1], gcol[0:Lc, 8 + h:9 + h], ALU.max, ["gcol", K("ep0")], [K("ep0")])
                    P.add("dve", lambda e, a=sm[0:Lc, e0:e0 + 1]: e.reciprocal(a, a), r=[K("ep0")], w=[K("ep0")])
                    yield
                    jk, jkk = T5()
                    act(jk[0:Lc, 0:256], pso[0:Lc, 0:256], AF.Square, [pko, K("ep0")], [jkk, K("ep1")], scale=sm[0:Lc, e0:e0 + 1], accum=sm[0:Lc, e0 + 1:e0 + 2])
                    act(sm[0:Lc, e0 + 2:e0 + 3], sm[0:Lc, e0 + 1:e0 + 2], AF.Sqrt, [K("ep1")], [K("ep2")], bias=EPS, scale=1.0 / 256)
                    yield
                    P.add("dve", lambda e, a=sm[0:Lc, e0 + 2:e0 + 3]: e.reciprocal(a, a), r=[K("ep2")], w=[K("ep2")])
                    tt("dve", sm[0:Lc, e0 + 3:e0 + 4], sm[0:Lc, e0 + 2:e0 + 3], sm[0:Lc, e0:e0 + 1], ALU.mult, [K("ep2"), K("ep0")], [K("ep3")])
                    stt(vct[0:Lc, h * 256:(h + 1) * 256], pso[0:Lc, 0:256], sm[0:Lc, e0 + 3:e0 + 4], gob[0:Lc, tc, h * 256:(h + 1) * 256],
                        ALU.mult, ALU.mult, [pko, K("ep3"), "gob", CT], [CT, "vct"])

                ps_nb[0] = 4
                gens = [head_gen(h) for h in range(4)]
                if samp:
                    for g_ in gens:
                        for _ in g_:
                            pass
                    gens = []
                while gens:
                    for g_ in list(gens):
                        try:
                            next(g_)
                        except StopIteration:
                            gens.remove(g_)
                ps_nb[0] = 6
                pst, pkt = PS()
                pstb = pst[:, :].bitcast(BF16)
                for c in range(8):
                    P.add("pe", lambda e, o=pstb[:, c * 128:c * 128 + Lc], i=vct[0:Lc, c * 128:(c + 1) * 128], idn=identb[0:Lc, 0:Lc]:
                          e.transpose(o, i, idn), r=["vct", CT, "constsb_i"], w=[pkt])
                cp("act", vcTs[:, :, lo:lo + Lc], _rs(pstb, "p (c t) -> p c t", c=8)[:, :, 0:Lc], [pkt], [CT, "vcTs"])
            merge_stage(False, C_GC, w_oc[l], 8, lambda kc, t0, n, t0s=t0s: vcTs[:, kc, t0 - t0s:t0 - t0s + n], [CT, "vcTs"], tiles_sc)
        dma(on[:, l, :], _rs(nT[:, :, :], "p c n -> p (c n)"), ["nT"], [], "on")
        fence()
        chk(l * 10 + 7)
        residual(w_o[l], 8, lambda kc, t0, n: big2[:, kc, t0:t0 + n], [("u", c) for c in range(8)])

        chk(l * 10 + 8)
        rmsnorm(xcur, xk, pc + PC_NX, hT_dst)
        fence()
        chk(l * 10 + 8.1)
        for c in range(8):
            wt, wk = wtile(w_xq[l], 0, 8, c * 128, 128)

            def cxq(ti, t0, n, ps, pk, c=c):
                act(big2[:, c, t0:t0 + n], ps[:, 0:n], AF.Copy, [pk], [("u", c)], scale=0.0625)
            proj(wt, wk, 0, hsrc, HK, 8, TT, cxq)
        chk(l * 10 + 8.2)
        for c in range(8):
            ts("dve", memTl[:, c, :], memTb[:, c, :], pcol[:, pc + PC_NMEM + c:pc + PC_NMEM + c + 1], None, ALU.mult, None,
               ["memTb", "pcol"], [CT, "memTl"])
        chk(l * 10 + 8.25)
        for kv in range(1 if _os.environ.get("DBG_KV0") else 2):
            for c in range(int(_os.environ.get("DBG_NC", "8"))):
                wt, wk = wtile(w_xkv[l], 0, 8, kv * D + c * 128, 128)
                if kv == 0 and not _os.environ.get("DBG_X1"):
                    ps, pk = PS()
                    for kc in range(8):
                        mm(ps[:, 0:256], wt[:, kc, :], memTl[:, kc, :], kc == 0, kc == 7, [wk, "memTl", CT], [pk])
                    cp("act", KTp[:, c, :], ps[:, 0:256], [pk], [CT, "KTp"])
                for mc in range(0 if _os.environ.get("DBG_X2") else 2):
                    ps, pk = PS()
                    for kc in range(8):
                        mm(ps[:, 0:128], memTl[:, kc, mc * 128:(mc + 1) * 128], wt[:, kc, :], kc == 0, kc == 7, [wk, "memTl", CT], [pk])
                    s4 = (c * 2 + mc) % 2
                    cp("dve", oms[:, s4, 0:128], ps[:, 0:128], [pk], [("oms", s4)])
                    dst = (omk if kv == 0 else omv)[l, mc * 128:(mc + 1) * 128, c * 128:(c + 1) * 128]
                    if not _os.environ.get("DBG_NOOM"):
                        dma(dst, oms[:, s4, 0:128], [("oms", s4)], [], "oms%d" % s4)
                    if kv == 1:
                        cp("act", Vp[:, mc, c * 128:(c + 1) * 128], oms[:, s4, 0:128], [("oms", s4)], [CT, "Vp"])
        chk(l * 10 + 8.3)
        QK = [("u", c) for c in range(8)]
        for tcx in range(16):
            ta = tcx * 128
            for h in range(4):
                ps, pk = PS()
                for dc in range(2):
                    mm(ps[:, 0:256], big2[:, h * 2 + dc, ta:ta + 128], KTp[:, h * 2 + dc, :], dc == 0, dc == 1, QK + ["KTp", CT], [pk])
                P.add("dve", lambda e, o=smalls[:, 12:13], i=ps[:, 0:256]: e.tensor_reduce(o, i, AX.X, ALU.max), r=[pk], w=["xa0"])
                ts("dve", smalls[:, 13:14], smalls[:, 12:13], -1.0, None, ALU.mult, None, ["xa0"], ["xa1"])
                act(pexp[:, 0:256], ps[:, 0:256], AF.Exp, [pk, "xa1"], [CT, "pexp", "xa2"], bias=smalls[:, 13:14], accum=smalls[:, 14:15])
                pst, pkt = PS()
                pstb = pst[:, :].bitcast(BF16)
                for mc in range(2):
                    P.add("pe", lambda e, o=pstb[:, mc * 128:(mc + 1) * 128], i=pexp[:, mc * 128:(mc + 1) * 128]:
                          e.transpose(o, i, identb[:, :]), r=["pexp", CT, "constsb_i"], w=[pkt])
                cp("act", pT[:, 0:256], pstb[:, 0:256], [pkt], [CT, "pT"])
                pso, pko = PS()
                for mc in range(2):
                    mm(pso[:, 0:256], pT[:, mc * 128:(mc + 1) * 128], Vp[:, mc, h * 256:(h + 1) * 256], mc == 0, mc == 1, ["pT", "Vp", CT], [pko])
                P.add("dve", lambda e, a=smalls[:, 15:16], i=smalls[:, 14:15]: e.reciprocal(a, i), r=["xa2"], w=["xa3"])
                ts("dve", aot[:, h * 256:(h + 1) * 256], pso[:, 0:256], smalls[:, 15:16], None, ALU.mult, None, [pko, "xa3"], ["aot"])
            pst, pkt = PS()
            pstb = pst[:, :].bitcast(BF16)
            for c in range(8):
                P.add("pe", lambda e, o=pstb[:, c * 128:(c + 1) * 128], i=aot[:, c * 128:(c + 1) * 128]:
                      e.transpose(o, i, identb[:, :]), r=["aot", "constsb_i"], w=[pkt])
            cp("act", aoT[:, :, ta:ta + 128], _rs(pstb, "p (c t) -> p c t", c=8), [pkt], [CT, "aoT"])
        chk(l * 10 + 8.4)
        for h in range(4):
            pss, pks = PSL()
            for dc in range(2):
                tt("pool", pTm[:, dc, :, :], big2[:, h * 2 + dc, PT:PT + 64].unsqueeze(1).to_broadcast([128, NB, 64]),
                   _rs(mrowb[:, :], "p (b t) -> p b t", b=NB), ALU.mult, QK + ["mrowb"], [CT, "pTm"])
            for b in range(NB):
                dma(_rs(kvst[:, 0:512], "p (c m) -> p c m", c=2), _rs(kT_d[l, b, h], "(c p) m -> p c m", p=128), [], [CT, "kvst"], "kvst")
                cp("pool", kvbf[:, 0:512], kvst[:, 0:512], ["kvst", CT], [CT, "kvbf"])
                for dc in range(2):
                    mm(pss[0:64, 0:256], pTm[:, dc, b, :], kvbf[:, dc * 256:(dc + 1) * 256], (b == 0 and dc == 0), (b == NB - 1 and dc == 1),
                       ["pTm", "kvbf", CT], [pks])
            P.add("dve", lambda e, o=smalls[0:64, 12:13], i=pss[0:64, 0:256]: e.tensor_reduce(o, i, AX.X, ALU.max), r=[pks], w=["xa0"])
            ts("dve", smalls[0:64, 13:14], smalls[0:64, 12:13], -1.0, None, ALU.mult, None, ["xa0"], ["xa1"])
            act(pexp[0:64, 0:256], pss[0:64, 0:256], AF.Exp, [pks, "xa1"], [CT, "pexp", "xa2"], bias=smalls[0:64, 13:14], accum=smalls[0:64, 14:15])
            pst, pkt = PS()
            pstb = pst[:, :].bitcast(BF16)
            for mc in range(2):
                P.add("pe", lambda e, o=pstb[:, mc * 64:(mc + 1) * 64], i=pexp[0:64, mc * 128:(mc + 1) * 128]:
                      e.transpose(o, i, identb[0:64, 0:64]), r=["pexp", CT, "constsb_i"], w=[pkt])
            cp("act", pT[:, 0:128], pstb[:, 0:128], [pkt], [CT, "pT"])
            for mc in range(2):
                tt("pool", pTm[:, mc, :, :], pT[:, mc * 64:(mc + 1) * 64].unsqueeze(1).to_broadcast([128, NB, 64]),
                   _rs(mrowb[:, :], "p (b t) -> p b t", b=NB), ALU.mult, ["pT", "mrowb", CT], [CT, "pTm"])
            pso, pko = PSL()
            for b in range(NB):
                dma(_rs(kvst[:, 0:512], "p (c e) -> p c e", c=2),
                    _rs(v_d[l, b, :, h * 256:(h + 1) * 256], "(c p) e -> p c e", p=128), [], [CT, "kvst"], "kvst")
                cp("pool", kvbf[:, 0:512], kvst[:, 0:512], ["kvst", CT], [CT, "kvbf"])
                for mc in range(2):
                    mm(pso[0:64, 0:256], pTm[:, mc, b, :], kvbf[:, mc * 256:(mc + 1) * 256], (b == 0 and mc == 0), (b == NB - 1 and mc == 1),
                       ["pTm", "kvbf", CT], [pko])
            P.add("dve", lambda e, a=smalls[0:64, 15:16], i=smalls[0:64, 14:15]: e.reciprocal(a, i), r=["xa2"], w=["xa3"])
            ts("dve", aot[0:64, h * 256:(h + 1) * 256], pso[0:64, 0:256], smalls[0:64, 15:16], None, ALU.mult, None, [pko, "xa3"], ["aot"])
        pst, pkt = PS()
        pstb = pst[:, :].bitcast(BF16)
        for c in range(8):
            P.add("pe", lambda e, o=pstb[:, c * 128:c * 128 + 64], i=aot[0:64, c * 128:(c + 1) * 128]:
                  e.transpose(o, i, identb[0:64, 0:64]), r=["aot", "constsb_i"], w=[pkt])
        cp("act", aoT[:, :, PT:PT + 64], _rs(pstb, "p (c t) -> p c t", c=8)[:, :, 0:64], [pkt], [CT, "aoT"])
        chk(l * 10 + 8.5)
        fence()
        residual(w_xo[l], 8, lambda kc, t0, n: aoT[:, kc, t0:t0 + n], [CT, "aoT"])
        fence()

        chk(l * 10 + 9)
        rmsnorm(xcur, xk, pc + PC_NFFN, hT_dst)
        fence()
        for g0, gn in ((0, 8), (8, 8), (16, 6)):
            for i in range(gn):
                hc = g0 + i
                wtg, wkg = wtile(w_fi[l], 0, 8, hc * 128, 128)
                wtu, wku = wtile(w_fi[l], 0, 8, DFF + hc * 128, 128)
                for ti, (t0, n) in enumerate(TT):
                    psG, pkG = PS()
                    for kc in range(8):
                        mm(psG[:, 0:n], wtg[:, kc, :], hsrc(kc, t0, n), kc == 0, kc == 7, [wkg] + HK, [pkG])
                    sg, sgk = T5()
                    act(sg[:, 0:n], psG[:, 0:n], AF.Silu, [pkG], [sgk])
                    psU, pkU = PS()
                    for kc in range(8):
                        mm(psU[:, 0:n], wtu[:, kc, :], hsrc(kc, t0, n), kc == 0, kc == 7, [wku] + HK, [pkU])
                    tt("dve", big2[:, i, t0:t0 + n], psU[:, 0:n], sg[:, 0:n], ALU.mult, [pkU, sgk], [("u", i)])
            nonlocal_src = w_fo[l][g0 * 128:(g0 + gn) * 128, :]
            residual(nonlocal_src, gn, lambda kc, t0, n: big2[:, kc, t0:t0 + n], [("u", c) for c in range(8)])

    stopped = False
    try:
        chk(0)
        layers()
    except _Stop:
        stopped = True
    P.epoch = L

    def y_dst(c, t0, n):
        return None
    for ti, (t0, n) in enumerate([] if stopped else TT):
        xin, xik = xin2[ti % 2], ("xin", ti % 2)
        dma(xin[:, :, 0:n], xcur[:, :, t0:t0 + n], [(xk, ti)], [xik], "xin%d" % (ti % 2))
        ps, pk = PS()
        for c in range(8):
            act(sqb[:, c % 2, 0:n], xin[:, c, 0:n], AF.Square, [xik], [("sqb", c % 2)])
            mm(ps[:, 0:n], onesb[:, :], sqb[:, c % 2, 0:n], c == 0, c == 7, [("sqb", c % 2)], [pk])
        rs, rk = T5()
        act(rs[:, 0:n], ps[:, 0:n], AF.Sqrt, [pk], [rk], bias=EPS, scale=1.0 / D)
        P.add("dve", lambda e, a=rs[:, 0:n]: e.reciprocal(a, a), r=[rk], w=[rk])
        for c in range(8):
            stt(xin[:, c, 0:n], xin[:, c, 0:n], pcol[:, PC_FIN + c:PC_FIN + c + 1], rs[:, 0:n], ALU.mult, ALU.mult,
                [xik, rk, "pcol"], [xik])
        dma(yT[:, :, t0:t0 + n], xin[:, :, 0:n], [xik], [], "yout%d" % (ti % 2))

    P.finalize()
    sem_names = set()
    for op in P.ops:
        if op.dma is not None:
            sem_names.add(("d", op.dma))
        elif op.signal:
            sem_names.add(("c", op.eng, op.epoch))
    sems = {}
    for i, s in enumerate(sorted(sem_names, key=str)):
        sems[s] = nc.semaphore("s%d" % i).__enter__()
    by_eng = {"pe": [], "act": [], "dve": [], "pool": [], "sp": []}
    for op in P.ops:
        by_eng[op.eng].append(op)
    final_waits = [(("d", k), v) for k, v in P.dma_cnt.items()]

    def run(e, ops, final=False):
        for op in ops:
            for s, v in op.waits:
                e.wait_ge(sems[s], v)
            ins = op.fn(e)
            if op.dma is not None:
                ins.then_inc(sems[("d", op.dma)], 16)
            elif op.signal:
                ins.then_inc(sems[("c", op.eng, op.epoch)], 1)
        if final:
            for s, v in final_waits:
                e.wait_ge(sems[s], v)

    with nc.allow_non_contiguous_dma(reason="small strided state rows"), nc.Block() as block:
        @block.sync
        def _(e):
            run(e, by_eng["sp"], final=True)

        @block.tensor
        def _(e):
            run(e, by_eng["pe"])

        @block.scalar
        def _(e):
            run(e, by_eng["act"])

        @block.vector
        def _(e):
            run(e, by_eng["dve"])

        @block.gpsimd
        def _(e):
            run(e, by_eng["pool"])
    return nc, len(P.ops)


def _consts():
    c = np.zeros((128, 1540), np.float32)
    c[:, 0:128] = np.eye(128)
    c[:, 128:256] = 1.0
    s = np.arange(128)
    c[:, 256:384] = np.where(s[None, :] >= s[:, None], 0.0, -30000.0)
    s6 = np.arange(64)
    same = (s6[:, None] // 4) == (s6[None, :] // 4)
    c[0:64, 384:448] = np.where(same & (s6[None, :] >= s6[:, None]), 0.0, -30000.0)
    c[0:64, 448:464] = (s6[:, None] // 4 == np.arange(16)[None, :]).astype(np.float32)
    c[0:4, 464:468] = np.eye(4)
    for h in range(4):
        c[h, 512 + h * 128:512 + (h + 1) * 128] = -1.0
        c[h, 1024 + h * 128:1024 + (h + 1) * 128] = 1.0
    mr = (np.arange(16)[:, None] == (s6[None, :] // 4)).astype(np.float32).reshape(-1)
    c[:, 1536] = 1.0
    c[:, 1537] = 0.0
    c[:, 1538] = -1.0
    return c, np.ascontiguousarray(np.broadcast_to(mr[None, :], (128, 1024))).astype(np.float32)


def _col(v):
    return np.ascontiguousarray(v.reshape(-1, 128).T)


_CACHE = {}


def _prep(inp):
    f = lambda k: np.asarray(inp[k], dtype=np.float32)
    cst, mrow = _consts()
    pcol = np.zeros((128, L * NPC), np.float32)
    for l in range(L):
        b = l * NPC
        pcol[:, b + PC_NMIX:b + PC_NMIX + 8] = _col(f("norm_mix_g")[l])
        pcol[:, b + PC_NX:b + PC_NX + 8] = _col(f("norm_x_g")[l])
        pcol[:, b + PC_NFFN:b + PC_NFFN + 8] = _col(f("norm_ffn_g")[l])
        pcol[:, b + PC_NMEM:b + PC_NMEM + 8] = _col(f("norm_mem_g")[l])
        pcol[:, b + PC_FIN:b + PC_FIN + 8] = _col(f("final_norm_g"))
        caw = f("conv_a_w")[l]
        cbw = f("conv_b_w")[l]
        for j in range(4):
            pcol[:, b + PC_CAW + j * 3:b + PC_CAW + (j + 1) * 3] = caw[:, j * 128:(j + 1) * 128].T
            pcol[:, b + PC_CBW + j * 31:b + PC_CBW + (j + 1) * 31] = cbw[:, j * 128:(j + 1) * 128].T
        pcol[:, b + PC_CBB:b + PC_CBB + 4] = _col(f("conv_b_b")[l])
        pcol[:, b + PC_LNG:b + PC_LNG + 4] = _col(f("ln_b_g")[l])
        pcol[:, b + PC_LNB:b + PC_LNB + 4] = _col(f("ln_b_b")[l])
    pcol[:, PC_FIN:PC_FIN + 8] = _col(f("final_norm_g"))
    gnorm = np.ascontiguousarray(np.broadcast_to(f("mlstm_norm_g")[:, None, :], (L, 128, D)))
    bif = np.ascontiguousarray(f("b_if").reshape(L, 2, 4).transpose(2, 0, 1).reshape(4, L * 2))
    xp, xs = f("x_prompt"), f("x_sample")
    shared = {k: f(k) for k in ("w_in", "w_out_a", "w_out_b", "w_out_c", "w_o", "w_xq", "w_xkv", "w_xo", "w_ffn_in", "w_ffn_out")}
    in_maps = []
    for i in range(8):
        bs = slice(i * NB, (i + 1) * NB)
        xt = np.concatenate([xp[i], xs[bs].reshape(ST, D)], axis=0)
        xT = np.ascontiguousarray(xt.T.reshape(8, 128, T).transpose(1, 0, 2))
        memtok = np.ascontiguousarray(f("mem_prompt")[i].reshape(2, 128, D).transpose(1, 0, 2))
        sca = f("state_conv_a")[:, bs]
        sca = np.ascontiguousarray(sca.reshape(L, NB, 2, 4, 128).transpose(4, 0, 3, 1, 2).reshape(128, -1))
        scb = f("state_conv_b")[:, bs]
        scb = np.ascontiguousarray(scb.reshape(L, NB, 30, 4, 128).transpose(4, 0, 3, 1, 2).reshape(128, -1))
        cn = np.ascontiguousarray(f("state_mlstm_c")[:, bs])
        ctr = np.ascontiguousarray(cn.transpose(0, 1, 2, 4, 3))
        nn = f("state_mlstm_n")[:, bs]
        nTi = np.ascontiguousarray(nn.reshape(L, NB, 4, 2, 128).transpose(4, 0, 3, 1, 2).reshape(128, -1))
        mi = np.ascontiguousarray(f("state_mlstm_m")[:, bs].transpose(2, 0, 1).reshape(4, -1))
        kTc = np.ascontiguousarray(f("cache_mem_k")[:, bs].transpose(0, 1, 3, 4, 2))
        vc = np.ascontiguousarray(f("cache_mem_v")[:, bs].reshape(L, NB, 256, D))
        m = {"xT_in": xT, "memtok": memtok, "pcol": pcol, "gnorm": gnorm, "bif": bif, "sca": sca, "scb": scb,
             "cnat": cn, "ctr": ctr, "nTin": nTi, "min": mi, "kTc": kTc, "vc": vc, "cst": cst, "mrow": mrow}
        m.update(shared)
        in_maps.append(m)
    return in_maps


def _post(R):
    y_p = np.zeros((8, PT, D), np.float32)
    y_s = np.zeros((128, 4, D), np.float32)
    p_a = np.zeros((L, 8, 2, 512), np.float32)
    p_b = np.zeros((L, 8, 30, 512), np.float32)
    p_c = np.zeros((L, 8, 4, 256, 256), np.float32)
    p_n = np.zeros((L, 8, 4, 256), np.float32)
    p_m = np.zeros((L, 8, 4), np.float32)
    p_k = np.zeros((L, 8, 256, 4, 256), np.float32)
    p_v = np.zeros((L, 8, 256, 4, 256), np.float32)
    s_a = np.zeros((L, 128, 2, 512), np.float32)
    s_b = np.zeros((L, 128, 30, 512), np.float32)
    s_c = np.zeros((L, 128, 4, 256, 256), np.float32)
    s_n = np.zeros((L, 128, 4, 256), np.float32)
    s_m = np.zeros((L, 128, 4), np.float32)
    for i in range(8):
        r = R[i]
        bs = slice(i * NB, (i + 1) * NB)
        yt = r["yT"].transpose(2, 1, 0).reshape(T, D)
        y_p[i] = yt[:PT]
        y_s[bs] = yt[PT:].reshape(NB, 4, D)
        a = r["oca"].reshape(128, L, 4, 17, 2).transpose(1, 3, 4, 2, 0).reshape(L, 17, 2, 512)
        p_a[:, i] = a[:, 0]
        s_a[:, bs] = a[:, 1:]
        b_ = r["ocb"].reshape(128, L, 4, 17, 30).transpose(1, 3, 4, 2, 0).reshape(L, 17, 30, 512)
        p_b[:, i] = b_[:, 0]
        s_b[:, bs] = b_[:, 1:]
        p_c[:, i] = r["oC"][:, 0]
        s_c[:, bs] = r["oC"][:, 1:]
        n_ = r["on"].reshape(128, L, 2, 68).transpose(1, 3, 2, 0).reshape(L, 68, 256)
        p_n[:, i] = n_[:, 0:4]
        s_n[:, bs] = n_[:, 4:].reshape(L, NB, 4, 256)
        m_ = r["om"].reshape(4, L, 17).transpose(1, 2, 0)
        p_m[:, i] = m_[:, 0]
        s_m[:, bs] = m_[:, 1:]
        p_k[:, i] = r["omk"].reshape(L, 256, 4, 256)
        p_v[:, i] = r["omv"].reshape(L, 256, 4, 256)
    return (y_p, y_s, p_a, p_b, p_c, p_n, p_m, p_k, p_v, s_a, s_b, s_c, s_n, s_m)


def kernel(**inp):
    if "nc" not in _CACHE:
        _CACHE["nc"] = build_nc()
    nc, nops = _CACHE["nc"]
    in_maps = _prep(inp)
    res = run_bass_kernel_spmd(nc, in_maps, core_ids=list(range(8)))
    return _post(res.results)
```

```python
import os as _os
import numpy as np
import concourse.bass as bass
import concourse.mybir as mybir
from concourse.bass_utils import run_bass_kernel_spmd

F32 = mybir.dt.float32
BF16 = mybir.dt.bfloat16
AF = mybir.ActivationFunctionType
ALU = mybir.AluOpType
AX = mybir.AxisListType

L = 4
D = 1024
T = 2112
PT = 2048
ST = 64
NB = 16
TT = [(0, 512), (512, 512), (1024, 512), (1536, 512), (2048, 64)]
INW = 9736
DFF = 2816
EPS = 1e-6
C_AB, C_AC, C_AX, C_BV, C_BG = 0, 512, 1024, 1536, 2048
C_Q, C_K, C_V, C_O, C_IF = 2560, 3584, 4608, 5632, 6656
C_GA, C_GB, C_GC = 6664, 7688, 8712
PC_NMIX, PC_NX, PC_NFFN, PC_NMEM, PC_FIN = 0, 8, 16, 24, 32
PC_CAW, PC_CBW, PC_CBB, PC_LNG, PC_LNB = 40, 52, 176, 180, 184
NPC = 188


_CTKEYS = {"ctmp", "memtS", "memn", "mrowst", "xin", "vab", "abuf", "ucv", "dg", "scbS", "bconv", "qTs", "kTs", "ktok",
           "vtok", "gob", "vct", "qsTm", "vcTs", "aoT", "kvst", "kvbf", "pTm", "KTp", "Vp", "memTl", "pexp", "pT", "cns", "ctsf", "xb", "oms", "aot"}


class Op:
    __slots__ = ("eng", "fn", "deps", "dma", "dval", "signal", "sigval", "waits", "epoch", "idx")


class Prog:
    def __init__(self):
        self.ops = []
        self.lastw = {}
        self.rd_eng = {}
        self.rd_dma = {}
        self.dma_cnt = {}
        self.epoch = 0

    def add(self, eng, fn, r=(), w=(), dma=None, fence=False):
        if not fence:
            def _isct(k):
                n = k[0] if isinstance(k, tuple) else k
                return n in _CTKEYS
            if any(_isct(k) for k in list(r) + list(w)):
                r = [k for k in r if k != "ctmp"] + ["ctmp"]
                w = [k for k in w if k != "ctmp"]
        op = Op()
        op.eng, op.fn, op.dma, op.epoch = eng, fn, dma, self.epoch
        op.idx = len(self.ops)
        op.signal = False
        op.sigval = 0
        op.dval = 0
        deps = set()
        for k in r:
            if k in self.lastw:
                deps.add(self.lastw[k])
        for k in w:
            if k in self.lastw:
                deps.add(self.lastw[k])
            for e, i in self.rd_eng.get(k, {}).items():
                deps.add(i)
            for i in self.rd_dma.get(k, ()):
                deps.add(i)
        for k in r:
            if dma is not None:
                self.rd_dma.setdefault(k, []).append(op.idx)
            else:
                self.rd_eng.setdefault(k, {})[eng] = op.idx
        for k in w:
            self.lastw[k] = op.idx
            self.rd_eng[k] = {}
            self.rd_dma[k] = []
        if dma is not None:
            c = self.dma_cnt.get(dma, 0) + 16
            self.dma_cnt[dma] = c
            op.dval = c
        deps.discard(op.idx)
        op.deps = deps
        self.ops.append(op)
        return op

    def finalize(self):
        ops = self.ops
        for op in ops:
            for d in op.deps:
                y = ops[d]
                if y.dma is None and not (y.eng == op.eng and op.dma is None and op.eng == "pe"):
                    y.signal = True
        cnt = {}
        for op in ops:
            if op.signal:
                k = (op.eng, op.epoch)
                cnt[k] = cnt.get(k, 0) + 1
                op.sigval = cnt[k]
        waited = {}
        for op in ops:
            need = {}
            for d in op.deps:
                y = ops[d]
                if y.dma is not None:
                    s, v = ("d", y.dma), y.dval
                elif y.eng == op.eng and op.dma is None and op.eng == "pe":
                    continue
                else:
                    s, v = ("c", y.eng, y.epoch), y.sigval
                if need.get(s, 0) < v:
                    need[s] = v
            ws = []
            wd = waited.setdefault(op.eng, {})
            for s, v in need.items():
                if wd.get(s, 0) < v:
                    wd[s] = v
                    ws.append((s, v))
            op.waits = ws
        return cnt


def _rs(a, pat, **kw):
    return a.rearrange(pat, **kw)


class _Stop(Exception):
    pass


def build_nc(stop=10 ** 9):
    def chk(i):
        if i >= stop:
            raise _Stop()
    nc = bass.Bass("TRN2", target_bir_lowering=False)
    P = Prog()

    def din(name, shape):
        return nc.dram_tensor(name, list(shape), F32, kind="ExternalInput").ap()

    def dout(name, shape):
        return nc.dram_tensor(name, list(shape), F32, kind="ExternalOutput").ap()

    xT_in = din("xT_in", [128, 8, T])
    memtok = din("memtok", [128, 2, D])
    pcol_d = din("pcol", [128, L * NPC])
    gnorm_d = din("gnorm", [L, 128, D])
    bif_d = din("bif", [4, L * 2])
    sca_d = din("sca", [128, L * 4 * NB * 2])
    scb_d = din("scb", [128, L * 4 * NB * 30])
    cnat_d = din("cnat", [L, NB, 4, 256, 256])
    ctr_d = din("ctr", [L, NB, 4, 256, 256])
    nT_d = din("nTin", [128, L * 2 * 64])
    m_d = din("min", [4, L * NB])
    kT_d = din("kTc", [L, NB, 4, 256, 256])
    v_d = din("vc", [L, NB, 256, D])
    cst_d = din("cst", [128, 1540])
    mrow_d = din("mrow", [128, 1024])
    w_in = din("w_in", [L, D, INW])
    w_oa = din("w_out_a", [L, 512, D])
    w_ob = din("w_out_b", [L, 512, D])
    w_oc = din("w_out_c", [L, D, D])
    w_o = din("w_o", [L, D, D])
    w_xq = din("w_xq", [L, D, D])
    w_xkv = din("w_xkv", [L, D, 2 * D])
    w_xo = din("w_xo", [L, D, D])
    w_fi = din("w_ffn_in", [L, D, 2 * DFF])
    w_fo = din("w_ffn_out", [L, DFF, D])

    yT = dout("yT", [128, 8, T])
    oca = dout("oca", [128, L * 4, 17 * 2])
    ocb = dout("ocb", [128, L * 4, 17 * 30])
    oC = dout("oC", [L, 17, 4, 256, 256])
    on = dout("on", [128, L, 2 * 68])
    om = dout("om", [4, L * 17])
    omk = dout("omk", [L, 256, D])
    omv = dout("omv", [L, 256, D])
    xsA = nc.dram_tensor("xsA", [128, 8, T], F32).ap()
    xsB = nc.dram_tensor("xsB", [128, 8, T], F32).ap()

    def sb(name, shape, dt=F32):
        return nc.alloc_sbuf_tensor(name, list(shape), dt) if False else nc.sbuf_tensor(name, list(shape), dt).__enter__()

    hT = sb("hT", [128, 8, T], BF16)
    big2 = sb("big2", [128, 8, T], BF16)
    ctmp = sb("ctmp", [128, 16384], F32)
    wst = sb("wst", [128, 3, 1024], F32)
    wbf = sb("wbf", [128, 3, 1024], BF16)
    cst = sb("cstsb", [128, 1540], F32)
    pcol = sb("pcolsb", [128, L * NPC], F32)
    gnb = sb("gnb", [128, D], F32)
    sqb = sb("sqb", [128, 2, 512], BF16)
    t512 = sb("t512", [128, 4, 512], F32)
    rows = sb("rows", [4, 8, 128], F32)
    bif = sb("bifsb", [4, L * 2], F32)
    gcol = sb("gcol", [128, 16], F32)
    smalls = sb("smalls", [128, 32], F32)
    wprb = sb("wprb", [128, 64], F32)
    wTt4 = sb("wTt4", [128, 4, 128], F32)
    smT4 = sb("smT4", [128, 4, 128], BF16)
    qsT4 = sb("qsT4", [128, 4, 256], BF16)
    wxs = sb("wxs", [4, 64], F32)
    eps4 = sb("eps4", [128, 16], F32)
    cnp = sb("cnp", [128, 4, 512], F32)
    lnbuf = cnp[:, 0:2, :]
    ctp = sb("ctp", [128, 4, 2 * 258], BF16)
    cts = sb("cts", [128, 2, 2 * 258], BF16)
    nT = sb("nT", [128, 2, 68], F32)
    msb = sb("msb", [4, 64], F32)
    vwb4 = sb("vwb4", [128, 4, 256], BF16)
    wlm4 = sb("wlm4", [128, 4, 16], F32)
    wlmb4 = sb("wlmb4", [128, 4, 16], BF16)
    ostA = sb("ostA", [128, 34], F32)
    ostB = sb("ostB", [128, 17 * 30], F32)
    scaS = sb("scaS", [128, 4 * NB * 2], F32)
    identb = sb("identb", [128, 128], BF16)
    onesb = sb("onesb", [128, 128], BF16)
    mrowb = sb("mrowb", [128, NB * 64], BF16)

    ident = cst[:, 0:128]
    onesf = cst[:, 128:256]
    maskP = cst[:, 256:384]
    maskS = cst[0:64, 384:448]
    maskcol = cst[0:64, 448:464]
    I4 = cst[0:4, 464:468]
    selneg = cst[0:4, 512:1024]
    selpos = cst[0:4, 1024:1536]

    def cv(off, nbytes, dt):
        a = ctmp[:, off // 4:(off + nbytes) // 4]
        return a.bitcast(dt) if dt != F32 else a

    xin2 = [_rs(cv(0, 16384, F32), "p (c t) -> p c t", c=8), _rs(cv(16384, 16384, F32), "p (c t) -> p c t", c=8)]
    xbufs = [cv(33792, 8448, F32), cv(42240, 8448, F32)]
    mrow_st = cv(16384, 4096, F32)
    cns = _rs(cv(55360, 4096, F32), "p (s n) -> p s n", s=2)
    cts_f = _rs(cv(59456, 4096, F32), "p (s n) -> p s n", s=2)
    vab = _rs(cv(0, 16896, BF16), "p (c t) -> p c t", c=4)
    abuf = cv(16896, 4224, BF16)
    ucv = [cv(21120, 5248, BF16), cv(26368, 5248, BF16)]
    dg = _rs(cv(31616, 7936, BF16), "p (k n) -> p k n", k=31)
    scbS = cv(39552, 7680, F32)
    bconv = _rs(cv(47232, 16896, BF16), "p (c t) -> p c t", c=4)
    qTs = _rs(cv(0, 8192, BF16), "p (c t) -> p c t", c=8)
    kTs = _rs(cv(8192, 8192, BF16), "p (c t) -> p c t", c=8)
    ktok = _rs(cv(16384, 8192, BF16), "p (c n) -> p c n", c=4)
    vtok = _rs(cv(24576, 8256, BF16), "p (c h n) -> p c h n", c=4, h=4)
    gob = _rs(cv(32832, 8192, BF16), "p (c n) -> p c n", c=4)
    vct = cv(41024, 2048, BF16)
    qsTm = _rs(cv(43072, 4096, BF16), "p (c b t) -> p c b t", c=2, b=NB)
    vcTs = _rs(cv(47168, 8192, BF16), "p (c t) -> p c t", c=8)
    aoT = _rs(cv(0, 33792, BF16), "p (c t) -> p c t", c=8)
    kvst = cv(33792, 2048, F32)
    oms = _rs(cv(35840, 2048, F32), "p (s n) -> p s n", s=2)
    aot = cv(37888, 2048, BF16)
    kvbf = cv(41984, 4096, BF16)
    pTm = _rs(cv(46080, 4096, BF16), "p (c b t) -> p c b t", c=2, b=NB)
    KTp = _rs(cv(50176, 4096, BF16), "p (c m) -> p c m", c=8)
    Vp = _rs(cv(54272, 4096, BF16), "p (c n) -> p c n", c=2)
    memTl = _rs(cv(58368, 4096, BF16), "p (c m) -> p c m", c=8)
    pexp = cv(62464, 2048, BF16)
    pT = cv(64512, 1024, BF16)
    memtS = _rs(cv(0, 8192, F32), "p (c n) -> p c n", c=2)
    memn = _rs(cv(8192, 8192, F32), "p (c n) -> p c n", c=2)
    memTb = sb("memTb", [128, 8, 256], BF16)

    psb = [nc.psum_tensor("ps%d" % i, [128, 512], F32).__enter__() for i in range(8)]
    ps_ctr = [0]

    ps_nb = [6]

    def PS():
        i = ps_ctr[0] % ps_nb[0]
        ps_ctr[0] += 1
        return psb[i], ("ps", i)

    psl_ctr = [0]

    def PSL():
        if ps_nb[0] == 6:
            i = 6 + psl_ctr[0] % 2
        else:
            i = 4 + psl_ctr[0] % 4
        psl_ctr[0] += 1
        return psb[i], ("ps", i)

    t5_ctr = [0]

    def T5():
        i = t5_ctr[0] % 4
        t5_ctr[0] += 1
        return t512[:, i, :], ("t5", i)

    CT = "ctmp"

    def fence():
        P.add("pool", lambda e: e.memset(smalls[:, 31:32], 0.0), r=[], w=[CT, "fencebyte"], fence=True)

    w_ctr = [0]

    def wtile(src2d, r0, nkc, c0, ncols):
        s = w_ctr[0] % 3
        w_ctr[0] += 1
        n = nkc * ncols
        src = _rs(src2d[r0:r0 + nkc * 128, c0:c0 + ncols], "(kc p) n -> p kc n", p=128)
        dst = _rs(wst[:, s, 0:n], "p (kc n) -> p kc n", kc=nkc)
        P.add("sp", lambda e: e.dma_start(out=dst, in_=src), r=[], w=[("wst", s)], dma="ws%d_%d" % (s, P.epoch))
        P.add("pool", lambda e: e.tensor_copy(out=wbf[:, s, 0:n], in_=wst[:, s, 0:n]), r=[("wst", s)], w=[("wbf", s)])
        return _rs(wbf[:, s, 0:n], "p (kc n) -> p kc n", kc=nkc), ("wbf", s)

    def mm(out, lhsT, rhs, start, stop, r, w):
        P.add("pe", lambda e: e.matmul(out, lhsT=lhsT, rhs=rhs, start=start, stop=stop), r=r, w=w)

    def proj(wt, wk, col0, src, srck, nkc, tiles, consume):
        for ti, (t0, n) in enumerate(tiles):
            ps, pk = PS()
            for kc in range(nkc):
                mm(ps[:, 0:n], wt[:, kc, col0:col0 + 128], src(kc, t0, n), kc == 0, kc == nkc - 1,
                   [wk] + srck, [pk])
            consume(ti, t0, n, ps, pk)

    def act(out, in_, func, r, w, bias=0.0, scale=1.0, accum=None):
        if accum is None:
            P.add("act", lambda e: e.activation(out, in_, func, bias=bias, scale=scale), r=r, w=w)
        else:
            P.add("act", lambda e: e.activation(out, in_, func, bias=bias, scale=scale, accum_out=accum), r=r, w=w)

    def tt(eng, out, a, b, op, r, w):
        P.add(eng, lambda e: e.tensor_tensor(out, a, b, op), r=r, w=w)

    def stt(out, a, s, b, op0, op1, r, w):
        P.add("dve", lambda e: e.scalar_tensor_tensor(out, a, s, b, op0, op1), r=r, w=w)

    def ts(eng, out, a, s1, s2, op0, op1, r, w):
        if s2 is None:
            P.add(eng, lambda e: e.tensor_scalar(out, a, s1, None, op0), r=r, w=w)
        else:
            P.add(eng, lambda e: e.tensor_scalar(out, a, s1, s2, op0, op1), r=r, w=w)

    def cp(eng, out, a, r, w):
        if eng == "act":
            P.add("act", lambda e: e.activation(out, a, AF.Copy), r=r, w=w)
        else:
            P.add(eng, lambda e: e.tensor_copy(out=out, in_=a), r=r, w=w)

    def dma(out, in_, r, w, sem):
        P.add("sp", lambda e: e.dma_start(out=out, in_=in_), r=r, w=w, dma=sem)

    HK = [("hT", c) for c in range(8)]

    def rmsnorm(xsrc, xsk, gcolbase, dst_fn):
        for ti, (t0, n) in enumerate(TT):
            xin, xik = xin2[ti % 2], ("xin", ti % 2)
            dma(xin[:, :, 0:n], xsrc[:, :, t0:t0 + n], [(xsk, ti)], [xik], "xin%d" % (ti % 2))
            ps, pk = PS()
            for c in range(8):
                act(sqb[:, c % 2, 0:n], xin[:, c, 0:n], AF.Square, [xik], [("sqb", c % 2)])
                mm(ps[:, 0:n], onesb[:, :], sqb[:, c % 2, 0:n], c == 0, c == 7, [("sqb", c % 2), "constsb_o"], [pk])
            rs, rk = T5()
            act(rs[:, 0:n], ps[:, 0:n], AF.Sqrt, [pk], [rk], bias=EPS, scale=1.0 / D)
            P.add("dve", lambda e, a=rs[:, 0:n]: e.reciprocal(a, a), r=[rk], w=[rk])
            for c in range(8):
                o, ok = dst_fn(c, t0, n)
                stt(o, xin[:, c, 0:n], pcol[:, gcolbase + c:gcolbase + c + 1], rs[:, 0:n], ALU.mult, ALU.mult,
                    [xik, rk, "pcol"], ok)

    def hT_dst(c, t0, n):
        return hT[:, c, t0:t0 + n], [("hT", c)]

    def hsrc(kc, t0, n):
        return hT[:, kc, t0:t0 + n]

    dma(cst[:, :], cst_d, [], ["consts"], "init1")
    dma(pcol[:, :], pcol_d, [], ["pcol"], "init2")
    dma(bif[:, :], bif_d, [], ["bif"], "init3")
    dma(memtS, memtok, [], [CT, "memtS"], "init4")
    cp("dve", identb[:, :], ident, ["consts"], ["constsb_i"])
    cp("dve", onesb[:, :], onesf, ["consts"], ["constsb_o"])
    dma(mrow_st, mrow_d, [], [CT, "mrowst"], "init5")
    cp("dve", mrowb[:, :], mrow_st, ["mrowst", CT], ["mrowb"])
    P.add("pool", lambda e: e.memset(_rs(vtok, "p c h n -> p (c h) n")[:, :, 256:258], 1.0), r=[], w=[CT])
    for mc in range(2):
        jk, jkk = T5()
        act(jk[:, 0:512], memtS[:, mc, 0:512], AF.Square, [CT, "memtS"], [jkk], accum=smalls[:, mc * 2:mc * 2 + 1])
        jk2, jkk2 = T5()
        act(jk2[:, 0:512], memtS[:, mc, 512:1024], AF.Square, [CT, "memtS"], [jkk2, "sm0"], accum=smalls[:, mc * 2 + 1:mc * 2 + 2])
        tt("dve", smalls[:, 4 + mc:5 + mc], smalls[:, mc * 2:mc * 2 + 1], smalls[:, mc * 2 + 1:mc * 2 + 2], ALU.add,
           [jkk, jkk2, "sm0"], ["sm1"])
        act(smalls[:, 6 + mc:7 + mc], smalls[:, 4 + mc:5 + mc], AF.Sqrt, ["sm1"], ["sm2"], bias=EPS, scale=1.0 / D)
        P.add("dve", lambda e, a=smalls[:, 6 + mc:7 + mc]: e.reciprocal(a, a), r=["sm2"], w=["sm3"])
        ts("dve", memn[:, mc, :], memtS[:, mc, :], smalls[:, 6 + mc:7 + mc], None, ALU.mult, None, ["sm3", CT, "memtS"], [CT, "memn"])
        for g in range(2):
            ps, pk = PS()
            for q in range(4):
                c = g * 4 + q
                P.add("pe", lambda e, o=ps[:, q * 128:(q + 1) * 128], i=memn[:, mc, c * 128:(c + 1) * 128]:
                      e.transpose(o, i, ident), r=["memn", "consts"], w=[pk])
            cp("dve", memTb[:, g * 4:(g + 1) * 4, mc * 128:(mc + 1) * 128],
               _rs(ps[:, :], "p (q t) -> p q t", q=4), [pk], ["memTb"])
    P.add("pool", lambda e: e.memset(nT[:, :, 0:4], 0.0), r=[], w=["nT"])
    fence()

    xcur, xk = xT_in, "x0"
    xdsts = [(xsA, "xA"), (xsB, "xB")]
    xflip = [0]

    xb_ctr = [0]

    def residual(wsrc2d, nk, srcfn, srckeys):
        nonlocal xcur, xk
        xn, xnk = xdsts[xflip[0] % 2]
        xflip[0] += 1
        xsrc_, xsk_ = xcur, xk

        def pre(j):
            wt, wk = wtile(wsrc2d, 0, nk, j * 128, 128)
            i = xb_ctr[0] % 2
            xb_ctr[0] += 1
            dma(xbufs[i][:, :], xsrc_[:, j, :], [(xsk_, t) for t in range(5)], [("xb", i)], "xbl%d" % i)
            return wt, wk, i
        nxt = pre(0)
        for j in range(8):
            wt, wk, i = nxt
            if j + 1 < 8:
                nxt = pre(j + 1)

            def consume(ti, t0, n, ps, pk, i=i):
                tt("dve", xbufs[i][:, t0:t0 + n], ps[:, 0:n], xbufs[i][:, t0:t0 + n], ALU.add, [pk, ("xb", i)], [("xb", i)])
            proj(wt, wk, 0, srcfn, srckeys, nk, TT, consume)
            dma(xn[:, j, :], xbufs[i][:, :], [("xb", i)], [(xnk, t) for t in range(5)], "xbs%d" % i)
        xcur, xk = xn, xnk

    def layers():
      for l in range(L):
        layer(l)

    def layer(l):
        nonlocal xcur, xk
        P.epoch = l
        pc = l * NPC
        Win = w_in[l]
        chk(l * 10 + 1)
        rmsnorm(xcur, xk, pc + PC_NMIX, hT_dst)
        fence()
        dma(gnb[:, :], gnorm_d[l], [], ["gnb"], "ld0_1")
        dma(scaS[:, :], sca_d[:, l * 128:(l + 1) * 128], [], ["scaS"], "ld0_2")
        dma(scbS, scb_d[:, l * 1920:(l + 1) * 1920], [], [CT, "scbS"], "ld0_3")
        dma(nT[:, :, 4:68], _rs(nT_d[:, l * 128:(l + 1) * 128], "p (c n) -> p c n", c=2), [], ["nT"], "ld0_4")
        dma(msb[:, 0:16], m_d[:, l * NB:(l + 1) * NB], [], ["msb"], "ld0_5")
        wg, wgk0 = None, None

        def conv_stage(K, colbase, halo_src, j, ub, ubk, consume):
            wv = pcol[:, colbase + j * K:colbase + (j + 1) * K]
            P.add("pool", lambda e: e.tensor_tensor(dg[:, 0:K, :], ident.unsqueeze(1).to_broadcast([128, K, 128]),
                                                    wv.unsqueeze(2).to_broadcast([128, K, 128]), ALU.mult),
                  r=["pcol", "consts"], w=[CT, "dg"])
            for ti, (t0, n) in enumerate(TT):
                ps, pk = PS()
                for k in range(K):
                    off = 30 - (K - 1) + k
                    if ti < 4:
                        rhs = ub[:, t0 + off:t0 + off + n]
                    else:
                        rhs = _rs(ub[:, 2078:2078 + NB * 34], "p (b t) -> p b t", b=NB)[:, :, off:off + 4]
                    mm(ps[:, 0:n], dg[:, k, :], rhs, k == 0, k == K - 1, ["dg", ubk, CT], [pk])
                consume(ti, t0, n, ps, pk)

        def ustage(colP, colG, gfunc, j, ub, ubk, halo_view, Kh, tailout):
            wtG, wkG = wtile(Win, 0, 8, colG + j * 128, 128)
            wtP, wkP = wtile(Win, 0, 8, colP + j * 128, 128)
            ubs = _rs(ub[:, 2078:2078 + NB * 34], "p (b t) -> p b t", b=NB)
            cp("pool", ubs[:, :, 30 - Kh:30], halo_view, ["scaS", "scbS", CT], [ubk, CT])
            for ti, (t0, n) in enumerate(TT):
                psG, pkG = PS()
                for kc in range(8):
                    mm(psG[:, 0:n], wtG[:, kc, :], hsrc(kc, t0, n), kc == 0, kc == 7, [wkG] + HK, [pkG])
                tg, tgk = T5()
                act(tg[:, 0:n], psG[:, 0:n], gfunc, [pkG], [tgk])
                psP, pkP = PS()
                for kc in range(8):
                    mm(psP[:, 0:n], wtP[:, kc, :], hsrc(kc, t0, n), kc == 0, kc == 7, [wkP] + HK, [pkP])
                if ti < 4:
                    tt("dve", ub[:, 30 + t0:30 + t0 + n], psP[:, 0:n], tg[:, 0:n], ALU.mult, [pkP, tgk], [ubk, CT])
                    if ti == 3:
                        tt("dve", tailout[:, 0:Kh], psP[:, 512 - Kh:512], tg[:, 512 - Kh:512], ALU.mult,
                           [pkP, tgk], ["ost"])
                else:
                    tt("dve", ubs[:, :, 30:34], _rs(psP[:, 0:64], "p (b t) -> p b t", b=NB),
                       _rs(tg[:, 0:64], "p (b t) -> p b t", b=NB), ALU.mult, [pkP, tgk], [ubk, CT])
                    so = _rs(tailout[:, Kh:17 * Kh], "p (b t) -> p b t", b=NB)
                    if Kh >= 4:
                        tt("dve", so[:, :, Kh - 4:Kh], _rs(psP[:, 0:64], "p (b t) -> p b t", b=NB),
                           _rs(tg[:, 0:64], "p (b t) -> p b t", b=NB), ALU.mult, [pkP, tgk], ["ost"])
                    else:
                        tt("dve", so[:, :, 0:Kh], _rs(psP[:, 0:64], "p (b t) -> p b t", b=NB)[:, :, 4 - Kh:4],
                           _rs(tg[:, 0:64], "p (b t) -> p b t", b=NB)[:, :, 4 - Kh:4], ALU.mult, [pkP, tgk], ["ost"])

        chk(l * 10 + 2)
        P.add("pool", lambda e: e.memset(ucv[0][:, 0:30], 0.0), r=[], w=[CT, ("ucv", 0)])
        P.add("pool", lambda e: e.memset(ucv[1][:, 0:30], 0.0), r=[], w=[CT, ("ucv", 1)])
        for j in range(4):
            ub, ubk = ucv[j % 2], ("ucv", j % 2)
            hv = _rs(scaS[:, j * 32:(j + 1) * 32], "p (b t) -> p b t", b=NB)
            ustage(C_AX, C_AC, AF.Copy, j, ub, ubk, hv, 2, ostA)
            dma(oca[:, l * 4 + j, :], ostA[:, :], ["ost"], [], "oca")
            wtB, wkB = wtile(Win, 0, 8, C_AB + j * 128, 128)
            for ti, (t0, n) in enumerate(TT):
                psb_, pkb = PS()
                for kc in range(8):
                    mm(psb_[:, 0:n], wtB[:, kc, :], hsrc(kc, t0, n), kc == 0, kc == 7, [wkB] + HK, [pkb])
                cp("act", abuf[:, t0:t0 + n], psb_[:, 0:n], [pkb], [CT, "abuf"])

            def consA(ti, t0, n, ps, pk, j=j):
                tt("dve", vab[:, j, t0:t0 + n], ps[:, 0:n], abuf[:, t0:t0 + n], ALU.mult, [pk, "abuf"], [CT, ("vab", j)])
            conv_stage(3, pc + PC_CAW, None, j, ub, ubk, consA)

        def merge_stage(first, gcol0, wsrc2d, nk, srcfn, srckeys, tiles):
            for j in range(8):
                wtG, wkG = wtile(Win, 0, 8, gcol0 + j * 128, 128)
                wtY, wkY = wtile(wsrc2d, 0, nk, j * 128, 128)
                for ti, (t0, n) in enumerate(tiles):
                    psG, pkG = PS()
                    for kc in range(8):
                        mm(psG[:, 0:n], wtG[:, kc, :], hsrc(kc, t0, n), kc == 0, kc == 7, [wkG] + HK, [pkG])
                    sg, sgk = T5()
                    act(sg[:, 0:n], psG[:, 0:n], AF.Sigmoid, [pkG], [sgk])
                    psY, pkY = PS()
                    for kc in range(nk):
                        mm(psY[:, 0:n], wtY[:, kc, :], srcfn(kc, t0, n), kc == 0, kc == nk - 1, [wkY] + srckeys, [pkY])
                    if first:
                        tt("dve", big2[:, j, t0:t0 + n], psY[:, 0:n], sg[:, 0:n], ALU.mult, [pkY, sgk], [("u", j)])
                    else:
                        tt("dve", sg[:, 0:n], psY[:, 0:n], sg[:, 0:n], ALU.mult, [pkY, sgk], [sgk])
                        tt("dve", big2[:, j, t0:t0 + n], big2[:, j, t0:t0 + n], sg[:, 0:n], ALU.add, [sgk, ("u", j)], [("u", j)])

        chk(l * 10 + 3)
        merge_stage(True, C_GA, w_oa[l], 4, lambda kc, t0, n: vab[:, kc, t0:t0 + n], [CT] + [("vab", c) for c in range(4)], TT)

        chk(l * 10 + 4)
        for j in range(4):
            ub, ubk = ucv[j % 2], ("ucv", j % 2)
            hv = _rs(scbS[:, j * 480:(j + 1) * 480], "p (b t) -> p b t", b=NB)
            cp("pool", _rs(ostB[:, 30:510], "p (b t) -> p b t", b=NB)[:, :, 0:26], hv[:, :, 4:30], ["scbS", CT], ["ost"])
            ustage(C_BV, C_BG, AF.Sigmoid, j, ub, ubk, hv, 30, ostB)
            dma(ocb[:, l * 4 + j, :], ostB[:, :], ["ost"], [], "ocb")

            def consB(ti, t0, n, ps, pk, j=j):
                act(bconv[:, j, t0:t0 + n], ps[:, 0:n], AF.Identity, [pk, "pcol"], [CT, ("bconv", j)],
                    bias=pcol[:, pc + PC_CBB + j:pc + PC_CBB + j + 1])
            conv_stage(31, pc + PC_CBW, None, j, ub, ubk, consB)
        BK = [("bconv", c) for c in range(4)]
        for ti, (t0, n) in enumerate(TT):
            ps1, pk1 = PS()
            for c in range(4):
                mm(ps1[:, 0:n], onesb[:, :], bconv[:, c, t0:t0 + n], c == 0, c == 3, BK + [CT, "constsb_o"], [pk1])
            ps2, pk2 = PS()
            for c in range(4):
                act(sqb[:, c % 2, 0:n], bconv[:, c, t0:t0 + n], AF.Square, BK + [CT], [("sqb", c % 2)])
                mm(ps2[:, 0:n], onesb[:, :], sqb[:, c % 2, 0:n], c == 0, c == 3, [("sqb", c % 2)], [pk2])
            mu, muk = lnbuf[:, 0, :], ("cnp", 0)
            act(mu[:, 0:n], ps1[:, 0:n], AF.Copy, [pk1], [muk], scale=1.0 / 512)
            rs, rk = lnbuf[:, 1, :], ("cnp", 1)
            tt("dve", rs[:, 0:n], mu[:, 0:n], mu[:, 0:n], ALU.mult, [muk], [rk])
            stt(rs[:, 0:n], ps2[:, 0:n], 1.0 / 512, rs[:, 0:n], ALU.mult, ALU.subtract, [pk2, rk], [rk])
            act(rs[:, 0:n], rs[:, 0:n], AF.Sqrt, [rk], [rk], bias=EPS)
            P.add("dve", lambda e, a=rs[:, 0:n]: e.reciprocal(a, a), r=[rk], w=[rk])
            for c in range(4):
                xc, xck = T5()
                tt("dve", xc[:, 0:n], bconv[:, c, t0:t0 + n], mu[:, 0:n], ALU.subtract, BK + [CT, muk], [xck])
                stt(xc[:, 0:n], xc[:, 0:n], pcol[:, pc + PC_LNG + c:pc + PC_LNG + c + 1], rs[:, 0:n], ALU.mult, ALU.mult,
                    [xck, rk, "pcol"], [xck])
                act(vab[:, c, t0:t0 + n], xc[:, 0:n], AF.Silu, [xck, "pcol"], [CT, ("vab", c)],
                    bias=pcol[:, pc + PC_LNB + c:pc + PC_LNB + c + 1])
        chk(l * 10 + 5)
        merge_stage(False, C_GB, w_ob[l], 4, lambda kc, t0, n: vab[:, kc, t0:t0 + n], [CT] + [("vab", c) for c in range(4)], TT)
        fence()

        chk(l * 10 + 6)
        P.add("pool", lambda e: e.memset(_rs(vtok, "p c h n -> p (c h) n")[:, :, 256:258], 1.0), r=[], w=[CT, "vtok"])
        P.add("pool", lambda e: e.memset(cnp[:, :, :], 0.0), r=[], w=[("cnp", h_) for h_ in range(4)])
        P.add("pool", lambda e: e.memset(ctp[:, :, :], 0.0), r=[], w=[("ctp", h_) for h_ in range(4)])
        P.add("pool", lambda e: e.memset(nT[:, :, 0:4], 0.0), r=[], w=["nT"])
        P.add("pool", lambda e: e.memset(msb[:, 16:20], 0.0), r=[], w=["carry"])
        wgt, wgk = wtile(Win, 0, 8, C_IF, 8)
        wgb = sb("wgb%d" % l, [128, 8, 8], BF16)
        cp("pool", wgb[:, :, :], wgt, [wgk], [("wgb", l)])
        R = lambda i: rows[:, i, :]
        for sc in range(5):
            t0s, ns = TT[sc]
            samp = sc == 4
            Lc = 64 if samp else 128
            ntc = 1 if samp else 4
            nseq = NB if samp else 1
            tiles_sc = [(t0s, ns)]
            for c in range(8):
                wtq, wkq = wtile(Win, 0, 8, C_Q + c * 128, 128)

                def cq(ti, t0, n, ps, pk, c=c):
                    cp("act", qTs[:, c, 0:n], ps[:, 0:n], [pk], [CT, "qTs"])
                proj(wtq, wkq, 0, hsrc, HK, 8, tiles_sc, cq)
                wtk, wkk = wtile(Win, 0, 8, C_K + c * 128, 128)

                def ck(ti, t0, n, ps, pk, c=c):
                    act(kTs[:, c, 0:n], ps[:, 0:n], AF.Copy, [pk], [CT, "kTs"], scale=0.0625)
                proj(wtk, wkk, 0, hsrc, HK, 8, tiles_sc, ck)
                for kind, col in (("k", C_K), ("v", C_V), ("o", C_O)):
                    if kind == "k":
                        wtt, wkt = wtk, wkk
                    else:
                        wtt, wkt = wtile(Win, 0, 8, col + c * 128, 128)
                    for tc in range(ntc):
                        ps, pk = PS()
                        ta = t0s + tc * 128
                        for kc in range(8):
                            mm(ps[0:Lc, 0:128], hT[:, kc, ta:ta + Lc], wtt[:, kc, :], kc == 0, kc == 7, [wkt] + HK, [pk])
                        if kind == "k":
                            act(ktok[0:Lc, tc, c * 128:(c + 1) * 128], ps[0:Lc, 0:128], AF.Copy, [pk], [CT, "ktok"], scale=0.0625)
                        elif kind == "v":
                            cp("dve", vtok[0:Lc, tc, c // 2, (c % 2) * 128:(c % 2) * 128 + 128], ps[0:Lc, 0:128], [pk], [CT, "vtok"])
                        else:
                            so_, sok = T5()
                            act(so_[0:Lc, 0:128], ps[0:Lc, 0:128], AF.Sigmoid, [pk], [sok])
                            tt("pool", gob[0:Lc, tc, c * 128:(c + 1) * 128], so_[0:Lc, 0:128], gnb[0:Lc, c * 128:(c + 1) * 128],
                               ALU.mult, [sok, "gnb"], [CT, "gob"])
            for tc in range(ntc):
                ta = t0s + tc * 128
                lo = tc * 128
                for gi_, (ro, co, bo) in enumerate(((0, 0, 0), (1, 4, 1))):
                    ps, pk = PS()
                    for kc in range(8):
                        mm(ps[0:4, 0:Lc], wgb[:, kc, co:co + 4], hT[:, kc, ta:ta + Lc], kc == 0, kc == 7, [("wgb", l)] + HK, [pk])
                    act(R(ro)[:, 0:Lc], ps[0:4, 0:Lc], AF.Identity, [pk, "bif"], [("row", ro)], bias=bif[:, l * 2 + bo:l * 2 + bo + 1])
                ts("dve", R(7)[:, 0:Lc], R(1)[:, 0:Lc], -1.0, None, ALU.mult, None, [("row", 1)], [("row", 7)])
                tt("dve", R(7)[:, 0:Lc], R(7)[:, 0:Lc], R(1)[:, 0:Lc], ALU.max, [("row", 1), ("row", 7)], [("row", 7)])
                act(R(7)[:, 0:Lc], R(7)[:, 0:Lc], AF.Exp, [("row", 7)], [("row", 7)], scale=-1.0)
                act(R(7)[:, 0:Lc], R(7)[:, 0:Lc], AF.Ln, [("row", 7)], [("row", 7)], bias=1.0)
                stt(R(1)[:, 0:Lc], R(1)[:, 0:Lc], 0.0, R(7)[:, 0:Lc], ALU.min, ALU.subtract, [("row", 1), ("row", 7)], [("row", 1)])
                if not samp:
                    P.add("dve", lambda e: e.tensor_tensor_scan(R(2)[:, 0:128], cst[0:4, 1536:1537].to_broadcast([4, 128]), R(1)[:, 0:128],
                                                                msb[:, 16:17], ALU.mult, ALU.add), r=[("row", 1), "carry", "consts"], w=[("row", 2)])
                    tt("dve", R(0)[:, 0:128], R(0)[:, 0:128], R(2)[:, 0:128], ALU.subtract, [("row", 0), ("row", 2)], [("row", 0)])
                    P.add("dve", lambda e: e.tensor_tensor_scan(R(3)[:, 0:128], cst[0:4, 1537:1538].to_broadcast([4, 128]), R(0)[:, 0:128],
                                                                msb[:, 17:18], ALU.add, ALU.max), r=[("row", 0), "carry", "consts"], w=[("row", 3)])
                    Mend = R(3)[:, 127:128].to_broadcast([4, 128])
                    Mprev = msb[:, 17:18].to_broadcast([4, 128])
                    tt("dve", msb[:, 20:21], msb[:, 17:18], R(3)[:, 127:128], ALU.subtract, ["carry", ("row", 3)], ["wpr"])
                    npair = 1
                else:
                    v3 = lambda i: _rs(R(i)[:, 0:64], "p (b t) -> p b t", b=NB)
                    cp("dve", v3(2)[:, :, 0:1], v3(1)[:, :, 0:1], [("row", 1)], [("row", 2)])
                    for t_ in range(1, 4):
                        tt("dve", v3(2)[:, :, t_:t_ + 1], v3(2)[:, :, t_ - 1:t_], v3(1)[:, :, t_:t_ + 1], ALU.add, [("row", 1), ("row", 2)], [("row", 2)])
                    tt("dve", R(0)[:, 0:64], R(0)[:, 0:64], R(2)[:, 0:64], ALU.subtract, [("row", 0), ("row", 2)], [("row", 0)])
                    m0 = msb[:, 0:16].unsqueeze(2)
                    tt("dve", v3(3)[:, :, 0:1], v3(0)[:, :, 0:1], m0, ALU.max, [("row", 0), "msb"], [("row", 3)])
                    for t_ in range(1, 4):
                        tt("dve", v3(3)[:, :, t_:t_ + 1], v3(3)[:, :, t_ - 1:t_], v3(0)[:, :, t_:t_ + 1], ALU.max, [("row", 0), ("row", 3)], [("row", 3)])
                    Mend = v3(3)[:, :, 3:4].to_broadcast([4, NB, 4])
                    Mprev = m0.to_broadcast([4, NB, 4])
                    tt("dve", R(7)[:, 64:80].unsqueeze(2), m0, v3(3)[:, :, 3:4], ALU.subtract, ["msb", ("row", 3)], ["wpr"])
                    npair = NB
                Lv = (lambda i: R(i)[:, 0:Lc]) if not samp else (lambda i: _rs(R(i)[:, 0:64], "p (b t) -> p b t", b=NB))
                tt("dve", Lv(4), Lv(0), Mend, ALU.subtract, [("row", 0), ("row", 3)], [("row", 4)])
                act(R(4)[:, 0:Lc], R(4)[:, 0:Lc], AF.Exp, [("row", 4)], [("row", 4)])
                tt("dve", R(5)[:, 0:Lc], R(2)[:, 0:Lc], R(3)[:, 0:Lc], ALU.add, [("row", 2), ("row", 3)], [("row", 5)])
                if samp:
                    cp("dve", msb[:, 32:48].unsqueeze(2), _rs(R(5)[:, 0:64], "p (b t) -> p b t", b=NB)[:, :, 3:4], [("row", 5)], ["mout"])
                    dma(om[:, l * 17 + 1:l * 17 + 17], msb[:, 32:48], ["mout"], [], "om_s")
                elif sc == 3 and tc == 3:
                    cp("dve", msb[:, 24:25], R(5)[:, 127:128], [("row", 5)], ["moutp"])
                    dma(om[:, l * 17:l * 17 + 1], msb[:, 24:25], ["moutp"], [], "om_p")
                act(R(5)[:, 0:Lc], R(5)[:, 0:Lc], AF.Exp, [("row", 5)], [("row", 5)], scale=-1.0)
                tt("dve", Lv(6), Mprev, Lv(3), ALU.subtract, [("row", 3), "carry", "msb"], [("row", 6)])
                act(R(6)[:, 0:Lc], R(6)[:, 0:Lc], AF.Exp, [("row", 6)], [("row", 6)])
                if not samp:
                    act(msb[:, 20:21], msb[:, 20:21], AF.Exp, ["wpr"], ["wpr"])
                    ts("dve", R(7)[:, 96:100], I4, msb[:, 20:21], None, ALU.mult, None, ["wpr", "consts"], ["wprx"])
                    ps, pk = PS()
                    mm(ps[:, 0:4], onesf[0:4, :], R(7)[:, 96:100], True, True, ["wprx", "consts"], [pk])
                    cp("dve", wprb[:, 0:4], ps[:, 0:4], [pk], ["wprb"])
                    cp("dve", msb[:, 16:17], R(2)[:, 127:128], [("row", 2)], ["carry"])
                    cp("dve", msb[:, 17:18], R(3)[:, 127:128], [("row", 3), ("row", 6), "wpr"], ["carry"])
                else:
                    act(R(7)[:, 64:80], R(7)[:, 64:80], AF.Exp, ["wpr"], ["wpr"])
                    wx = _rs(wxs[0:4, 0:64], "p (b h) -> p b h", b=NB)
                    tt("dve", wx, R(7)[:, 64:80].unsqueeze(2).to_broadcast([4, NB, 4]), I4.unsqueeze(1).to_broadcast([4, NB, 4]),
                       ALU.mult, ["wpr", "consts"], ["wprx"])
                    ps, pk = PS()
                    mm(ps[:, 0:64], onesf[0:4, :], wxs[0:4, 0:64], True, True, ["wprx", "consts"], [pk])
                    cp("dve", wprb[:, 0:64], ps[:, 0:64], [pk], ["wprb"])
                ps, pk = PS()
                for q, ri in enumerate((0, 4, 5, 6)):
                    mm(ps[0:Lc, q * 4:q * 4 + 4], R(ri)[:, 0:Lc], I4, True, True, [("row", ri), "consts"], [pk])
                cp("dve", gcol[0:Lc, :], ps[0:Lc, 0:16], [pk], ["gcol"])
                def head_gen(h):
                    wT_ = wTt4[0:Lc, h, 0:Lc]
                    smT_ = smT4[0:Lc, h, 0:Lc]
                    qsT_ = _rs(qsT4[:, h, :], "p (c t) -> p c t", c=2)
                    wlm_ = wlm4[:, h, :]
                    wlmb_ = wlmb4[:, h, :]
                    e0 = h * 4
                    K = lambda nm: (nm, h)
                    psq, pkq = PS()
                    for dc in range(2):
                        mm(psq[0:Lc, 0:Lc], kTs[:, h * 2 + dc, lo:lo + Lc], qTs[:, h * 2 + dc, lo:lo + Lc], dc == 0, dc == 1,
                           ["qTs", "kTs", CT], [pkq])
                    psm, pkm = PS()
                    mm(psm[0:Lc, 0:Lc], selneg[:, h * 128:h * 128 + Lc], R(3)[:, 0:Lc], True, False, [("row", 3), "consts"], [pkm])
                    mm(psm[0:Lc, 0:Lc], ident[0:Lc, 0:Lc], (maskS if samp else maskP), False, True, ["consts"], [pkm])
                    act(wT_, psm[0:Lc, 0:Lc], AF.Exp, [pkm, "gcol"], [K("wTt")], bias=gcol[0:Lc, h:h + 1])
                    tt("dve", smT_, psq[0:Lc, 0:Lc], wT_, ALU.mult, [pkq, K("wTt")], [K("smT")])
                    yield
                    psw, pkw = PS()
                    mm(psw[:, 0:Lc], selpos[:, h * 128:(h + 1) * 128], R(6)[:, 0:Lc], True, True, [("row", 6), "consts"], [pkw])
                    for dc in range(2):
                        tt("dve", qsT_[:, dc, 0:Lc], qTs[:, h * 2 + dc, lo:lo + Lc], psw[:, 0:Lc], ALU.mult, [pkw, "qTs", CT], [K("qsT")])
                    if samp:
                        for dc in range(2):
                            tt("pool", qsTm[:, dc, :, :], qsT_[:, dc, 0:64].unsqueeze(1).to_broadcast([128, NB, 64]),
                               _rs(mrowb[:, :], "p (b t) -> p b t", b=NB), ALU.mult, [K("qsT"), "mrowb"], [CT, "qsTm"])
                        ts("dve", wlm_[0:64, :], maskcol, gcol[0:64, 4 + h:5 + h], None, ALU.mult, None, ["gcol", "consts"], [K("wlm")])
                        cp("dve", wlmb_[0:64, :], wlm_[0:64, :], [K("wlm")], [K("wlmb")])
                    else:
                        cp("dve", wlmb_[0:128, 0:1], gcol[0:128, 4 + h:5 + h], ["gcol"], [K("wlmb")])
                    yield
                    pso, pko = PSL()
                    mm(pso[0:Lc, 0:257], smT_, vtok[0:Lc, tc, h, 0:257], True, False, [K("smT"), "vtok", CT], [pko])
                    for b in range(nseq):
                        if samp:
                            s2 = (b + h * NB) % 2
                            pidx = 4 + b * 4 + h
                            dma(_rs(cts_f[:, s2, :], "p (c e) -> p c e", c=2), _rs(ctr_d[l, b, h], "(c p) e -> p c e", p=128),
                                [], [("ctsf", s2), CT], "ctsf%d" % s2)
                            dma(_rs(cns[:, s2, :], "p (c e) -> p c e", c=2), _rs(cnat_d[l, b, h], "(c p) e -> p c e", p=128),
                                [], [("cns", s2), CT], "cns%d" % s2)
                            ctv = _rs(cts[:, s2, :], "p (c e) -> p c e", c=2)
                            cp("act", ctv[:, :, 0:256], _rs(cts_f[:, s2, :], "p (c e) -> p c e", c=2), [("ctsf", s2)], [("cts", s2)])
                            cp("pool", ctv[:, :, 256:257], nT[:, :, pidx:pidx + 1], ["nT"], [("cts", s2)])
                            ctk = ("cts", s2)
                            cnv, cnk = cns[:, s2, :], ("cns", s2)
                            lhs_q = lambda dc, b=b: qsTm[:, dc, b, :]
                            qk_ = ["qsTm", CT]
                            wpc = wprb[:, b * 4 + h:b * 4 + h + 1]
                            wl_col = wlm_[0:64, b:b + 1]
                            wl_colb = wlmb_[0:64, b:b + 1]
                        else:
                            pidx = h
                            ctv = _rs(ctp[:, h, :], "p (c e) -> p c e", c=2)
                            ctk = ("ctp", h)
                            cnv, cnk = cnp[:, h, :], ("cnp", h)
                            lhs_q = lambda dc: qsT_[:, dc, 0:128]
                            qk_ = [K("qsT")]
                            wpc = wprb[:, h:h + 1]
                            wl_col = gcol[0:128, 4 + h:5 + h]
                            wl_colb = wlmb_[0:128, 0:1]
                        for dc in range(2):
                            mm(pso[0:Lc, 0:257], lhs_q(dc), ctv[:, dc, 0:257], False, (b == nseq - 1 and dc == 1), qk_ + [ctk], [pko])
                        if not samp:
                            yield
                        ts("dve", vwb4[0:Lc, h, :], vtok[0:Lc, tc, h, 0:256], wl_col, None, ALU.mult, None, ["vtok", CT, "gcol", K("wlm")], [K("vwb")])
                        psu, pku = PS()
                        for ec in range(2):
                            mm(psu[:, ec * 256:(ec + 1) * 256], vwb4[0:Lc, h, ec * 128:(ec + 1) * 128], ktok[0:Lc, tc, h * 256:(h + 1) * 256],
                               True, True, [K("vwb"), "ktok", CT], [pku])
                        stt(cnv, cnv, wpc, psu[:, :], ALU.mult, ALU.add, [cnk, "wprb", pku], [cnk])
                        psn, pkn = PS()
                        for dc in range(2):
                            mm(psn[:, dc:dc + 1], ktok[0:Lc, tc, h * 256 + dc * 128:h * 256 + dc * 128 + 128], wl_colb, True, True,
                               ["ktok", CT, K("wlmb")], [pkn])
                        stt(nT[:, :, pidx], nT[:, :, pidx], wpc, psn[:, 0:2], ALU.mult, ALU.add, ["nT", "wprb", pkn, ctk], ["nT"])
                        if samp:
                            dma(_rs(oC[l, 1 + b, h], "(c p) d -> p c d", p=128), _rs(cnv, "p (c d) -> p c d", c=2), [cnk], [], "oc%d" % s2)
                        else:
                            yield
                            for dcc in range(2):
                                pst, pkt = PS()
                                for ec in range(2):
                                    P.add("pe", lambda e, o=pst[:, ec * 128:(ec + 1) * 128], i=cnp[:, h, ec * 256 + dcc * 128:ec * 256 + dcc * 128 + 128]:
                                          e.transpose(o, i, ident), r=[("cnp", h), "consts"], w=[pkt])
                                cp("act", ctv[:, dcc, 0:256], pst[:, 0:256], [pkt], [("ctp", h)])
                            cp("pool", ctv[:, :, 256:257], nT[:, :, h:h + 1], ["nT"], [("ctp", h)])
                            if sc == 3 and tc == 3:
                                dma(_rs(oC[l, 0, h], "(c p) d -> p c d", p=128), _rs(cnv, "p (c d) -> p c d", c=2), [cnk], [], "ocp%d" % h)
                            yield
                    sm = eps4
                    ts("dve", sm[0:Lc, e0:e0 + 1], pso[0:Lc, 256:257], -1.0, None, ALU.mult, None, [pko], [K("ep0")])
                    tt("dve", sm[0:Lc, e0:e0 + 1], sm[0:Lc, e0:e0 + 1], pso[0:Lc, 256:257], ALU.max, [pko, K("ep0")], [K("ep0")])
                    tt("dve", sm[0:Lc, e0:e0 + 1], sm[0:Lc, e0:e0 + 1], gcol[0:Lc, 8 + h:9 + h], ALU.max, ["gcol", K("ep0")], [K("ep0")])
                    P.add("dve", lambda e, a=sm[0:Lc, e0:e0 + 1]: e.reciprocal(a, a), r=[K("ep0")], w=[K("ep0")])
                    yield
                    jk, jkk = T5()
                    act(jk[0:Lc, 0:256], pso[0:Lc, 0:256], AF.Square, [pko, K("ep0")], [jkk, K("ep1")], scale=sm[0:Lc, e0:e0 + 1], accum=sm[0:Lc, e0 + 1:e0 + 2])
                    act(sm[0:Lc, e0 + 2:e0 + 3], sm[0:Lc, e0 + 1:e0 + 2], AF.Sqrt, [K("ep1")], [K("ep2")], bias=EPS, scale=1.0 / 256)
                    yield
                    P.add("dve", lambda e, a=sm[0:Lc, e0 + 2:e0 + 3]: e.reciprocal(a, a), r=[K("ep2")], w=[K("ep2")])
                    tt("dve", sm[0:Lc, e0 + 3:e0 + 4], sm[0:Lc, e0 + 2:e0 + 3], sm[0:Lc, e0:e0 + 1], ALU.mult, [K("ep2"), K("ep0")], [K("ep3")])
                    stt(vct[0:Lc, h * 256:(h + 1) * 256], pso[0:Lc, 0:256], sm[0:Lc, e0 + 3:e0 + 4], gob[0:Lc, tc, h * 256:(h + 1) * 256],
                        ALU.mult, ALU.mult, [pko, K("ep3"), "gob", CT], [CT, "vct"])

                ps_nb[0] = 4
                gens = [head_gen(h) for h in range(4)]
                if samp:
                    for g_ in gens:
                        for _ in g_:
                            pass
                    gens = []
                while gens:
                    for g_ in list(gens):
                        try:
                            next(g_)
                        except StopIteration:
                            gens.remove(g_)
                ps_nb[0] = 6
                pst, pkt = PS()
                pstb = pst[:, :].bitcast(BF16)
                for c in range(8):
                    P.add("pe", lambda e, o=pstb[:, c * 128:c * 128 + Lc], i=vct[0:Lc, c * 128:(c + 1) * 128], idn=identb[0:Lc, 0:Lc]:
                          e.transpose(o, i, idn), r=["vct", CT, "constsb_i"], w=[pkt])
                cp("act", vcTs[:, :, lo:lo + Lc], _rs(pstb, "p (c t) -> p c t", c=8)[:, :, 0:Lc], [pkt], [CT, "vcTs"])
            merge_stage(False, C_GC, w_oc[l], 8, lambda kc, t0, n, t0s=t0s: vcTs[:, kc, t0 - t0s:t0 - t0s + n], [CT, "vcTs"], tiles_sc)
        dma(on[:, l, :], _rs(nT[:, :, :], "p c n -> p (c n)"), ["nT"], [], "on")
        fence()
        chk(l * 10 + 7)
        residual(w_o[l], 8, lambda kc, t0, n: big2[:, kc, t0:t0 + n], [("u", c) for c in range(8)])

        chk(l * 10 + 8)
        rmsnorm(xcur, xk, pc + PC_NX, hT_dst)
        fence()
        chk(l * 10 + 8.1)
        for c in range(8):
            wt, wk = wtile(w_xq[l], 0, 8, c * 128, 128)

            def cxq(ti, t0, n, ps, pk, c=c):
                act(big2[:, c, t0:t0 + n], ps[:, 0:n], AF.Copy, [pk], [("u", c)], scale=0.0625)
            proj(wt, wk, 0, hsrc, HK, 8, TT, cxq)
        chk(l * 10 + 8.2)
        for c in range(8):
            ts("dve", memTl[:, c, :], memTb[:, c, :], pcol[:, pc + PC_NMEM + c:pc + PC_NMEM + c + 1], None, ALU.mult, None,
               ["memTb", "pcol"], [CT, "memTl"])
        chk(l * 10 + 8.25)
        for kv in range(1 if _os.environ.get("DBG_KV0") else 2):
            for c in range(int(_os.environ.get("DBG_NC", "8"))):
                wt, wk = wtile(w_xkv[l], 0, 8, kv * D + c * 128, 128)
                if kv == 0 and not _os.environ.get("DBG_X1"):
                    ps, pk = PS()
                    for kc in range(8):
                        mm(ps[:, 0:256], wt[:, kc, :], memTl[:, kc, :], kc == 0, kc == 7, [wk, "memTl", CT], [pk])
                    cp("act", KTp[:, c, :], ps[:, 0:256], [pk], [CT, "KTp"])
                for mc in range(0 if _os.environ.get("DBG_X2") else 2):
                    ps, pk = PS()
                    for kc in range(8):
                        mm(ps[:, 0:128], memTl[:, kc, mc * 128:(mc + 1) * 128], wt[:, kc, :], kc == 0, kc == 7, [wk, "memTl", CT], [pk])
                    s4 = (c * 2 + mc) % 2
                    cp("dve", oms[:, s4, 0:128], ps[:, 0:128], [pk], [("oms", s4)])
                    dst = (omk if kv == 0 else omv)[l, mc * 128:(mc + 1) * 128, c * 128:(c + 1) * 128]
                    if not _os.environ.get("DBG_NOOM"):
                        dma(dst, oms[:, s4, 0:128], [("oms", s4)], [], "oms%d" % s4)
                    if kv == 1:
                        cp("act", Vp[:, mc, c * 128:(c + 1) * 128], oms[:, s4, 0:128], [("oms", s4)], [CT, "Vp"])
        chk(l * 10 + 8.3)
        QK = [("u", c) for c in range(8)]
        for tcx in range(16):
            ta = tcx * 128
            for h in range(4):
                ps, pk = PS()
                for dc in range(2):
                    mm(ps[:, 0:256], big2[:, h * 2 + dc, ta:ta + 128], KTp[:, h * 2 + dc, :], dc == 0, dc == 1, QK + ["KTp", CT], [pk])
                P.add("dve", lambda e, o=smalls[:, 12:13], i=ps[:, 0:256]: e.tensor_reduce(o, i, AX.X, ALU.max), r=[pk], w=["xa0"])
                ts("dve", smalls[:, 13:14], smalls[:, 12:13], -1.0, None, ALU.mult, None, ["xa0"], ["xa1"])
                act(pexp[:, 0:256], ps[:, 0:256], AF.Exp, [pk, "xa1"], [CT, "pexp", "xa2"], bias=smalls[:, 13:14], accum=smalls[:, 14:15])
                pst, pkt = PS()
                pstb = pst[:, :].bitcast(BF16)
                for mc in range(2):
                    P.add("pe", lambda e, o=pstb[:, mc * 128:(mc + 1) * 128], i=pexp[:, mc * 128:(mc + 1) * 128]:
                          e.transpose(o, i, identb[:, :]), r=["pexp", CT, "constsb_i"], w=[pkt])
                cp("act", pT[:, 0:256], pstb[:, 0:256], [pkt], [CT, "pT"])
                pso, pko = PS()
                for mc in range(2):
                    mm(pso[:, 0:256], pT[:, mc * 128:(mc + 1) * 128], Vp[:, mc, h * 256:(h + 1) * 256], mc == 0, mc == 1, ["pT", "Vp", CT], [pko])
                P.add("dve", lambda e, a=smalls[:, 15:16], i=smalls[:, 14:15]: e.reciprocal(a, i), r=["xa2"], w=["xa3"])
                ts("dve", aot[:, h * 256:(h + 1) * 256], pso[:, 0:256], smalls[:, 15:16], None, ALU.mult, None, [pko, "xa3"], ["aot"])
            pst, pkt = PS()
            pstb = pst[:, :].bitcast(BF16)
            for c in range(8):
                P.add("pe", lambda e, o=pstb[:, c * 128:(c + 1) * 128], i=aot[:, c * 128:(c + 1) * 128]:
                      e.transpose(o, i, identb[:, :]), r=["aot", "constsb_i"], w=[pkt])
            cp("act", aoT[:, :, ta:ta + 128], _rs(pstb, "p (c t) -> p c t", c=8), [pkt], [CT, "aoT"])
        chk(l * 10 + 8.4)
        for h in range(4):
            pss, pks = PSL()
            for dc in range(2):
                tt("pool", pTm[:, dc, :, :], big2[:, h * 2 + dc, PT:PT + 64].unsqueeze(1).to_broadcast([128, NB, 64]),
                   _rs(mrowb[:, :], "p (b t) -> p b t", b=NB), ALU.mult, QK + ["mrowb"], [CT, "pTm"])
            for b in range(NB):
                dma(_rs(kvst[:, 0:512], "p (c m) -> p c m", c=2), _rs(kT_d[l, b, h], "(c p) m -> p c m", p=128), [], [CT, "kvst"], "kvst")
                cp("pool", kvbf[:, 0:512], kvst[:, 0:512], ["kvst", CT], [CT, "kvbf"])
                for dc in range(2):
                    mm(pss[0:64, 0:256], pTm[:, dc, b, :], kvbf[:, dc * 256:(dc + 1) * 256], (b == 0 and dc == 0), (b == NB - 1 and dc == 1),
                       ["pTm", "kvbf", CT], [pks])
            P.add("dve", lambda e, o=smalls[0:64, 12:13], i=pss[0:64, 0:256]: e.tensor_reduce(o, i, AX.X, ALU.max), r=[pks], w=["xa0"])
            ts("dve", smalls[0:64, 13:14], smalls[0:64, 12:13], -1.0, None, ALU.mult, None, ["xa0"], ["xa1"])
            act(pexp[0:64, 0:256], pss[0:64, 0:256], AF.Exp, [pks, "xa1"], [CT, "pexp", "xa2"], bias=smalls[0:64, 13:14], accum=smalls[0:64, 14:15])
            pst, pkt = PS()
            pstb = pst[:, :].bitcast(BF16)
            for mc in range(2):
                P.add("pe", lambda e, o=pstb[:, mc * 64:(mc + 1) * 64], i=pexp[0:64, mc * 128:(mc + 1) * 128]:
                      e.transpose(o, i, identb[0:64, 0:64]), r=["pexp", CT, "constsb_i"], w=[pkt])
            cp("act", pT[:, 0:128], pstb[:, 0:128], [pkt], [CT, "pT"])
            for mc in range(2):
                tt("pool", pTm[:, mc, :, :], pT[:, mc * 64:(mc + 1) * 64].unsqueeze(1).to_broadcast([128, NB, 64]),
                   _rs(mrowb[:, :], "p (b t) -> p b t", b=NB), ALU.mult, ["pT", "mrowb", CT], [CT, "pTm"])
            pso, pko = PSL()
            for b in range(NB):
                dma(_rs(kvst[:, 0:512], "p (c e) -> p c e", c=2),
                    _rs(v_d[l, b, :, h * 256:(h + 1) * 256], "(c p) e -> p c e", p=128), [], [CT, "kvst"], "kvst")
                cp("pool", kvbf[:, 0:512], kvst[:, 0:512], ["kvst", CT], [CT, "kvbf"])
                for mc in range(2):
                    mm(pso[0:64, 0:256], pTm[:, mc, b, :], kvbf[:, mc * 256:(mc + 1) * 256], (b == 0 and mc == 0), (b == NB - 1 and mc == 1),
                       ["pTm", "kvbf", CT], [pko])
            P.add("dve", lambda e, a=smalls[0:64, 15:16], i=smalls[0:64, 14:15]: e.reciprocal(a, i), r=["xa2"], w=["xa3"])
            ts("dve", aot[0:64, h * 256:(h + 1) * 256], pso[0:64, 0:256], smalls[0:64, 15:16], None, ALU.mult, None, [pko, "xa3"], ["aot"])
        pst, pkt = PS()
        pstb = pst[:, :].bitcast(BF16)
        for c in range(8):
            P.add("pe", lambda e, o=pstb[:, c * 128:c * 128 + 64], i=aot[0:64, c * 128:(c + 1) * 128]:
                  e.transpose(o, i, identb[0:64, 0:64]), r=["aot", "constsb_i"], w=[pkt])
        cp("act", aoT[:, :, PT:PT + 64], _rs(pstb, "p (c t) -> p c t", c=8)[:, :, 0:64], [pkt], [CT, "aoT"])
        chk(l * 10 + 8.5)
        fence()
        residual(w_xo[l], 8, lambda kc, t0, n: aoT[:, kc, t0:t0 + n], [CT, "aoT"])
        fence()

        chk(l * 10 + 9)
        rmsnorm(xcur, xk, pc + PC_NFFN, hT_dst)
        fence()
        for g0, gn in ((0, 8), (8, 8), (16, 6)):
            for i in range(gn):
                hc = g0 + i
                wtg, wkg = wtile(w_fi[l], 0, 8, hc * 128, 128)
                wtu, wku = wtile(w_fi[l], 0, 8, DFF + hc * 128, 128)
                for ti, (t0, n) in enumerate(TT):
                    psG, pkG = PS()
                    for kc in range(8):
                        mm(psG[:, 0:n], wtg[:, kc, :], hsrc(kc, t0, n), kc == 0, kc == 7, [wkg] + HK, [pkG])
                    sg, sgk = T5()
                    act(sg[:, 0:n], psG[:, 0:n], AF.Silu, [pkG], [sgk])
                    psU, pkU = PS()
                    for kc in range(8):
                        mm(psU[:, 0:n], wtu[:, kc, :], hsrc(kc, t0, n), kc == 0, kc == 7, [wku] + HK, [pkU])
                    tt("dve", big2[:, i, t0:t0 + n], psU[:, 0:n], sg[:, 0:n], ALU.mult, [pkU, sgk], [("u", i)])
            nonlocal_src = w_fo[l][g0 * 128:(g0 + gn) * 128, :]
            residual(nonlocal_src, gn, lambda kc, t0, n: big2[:, kc, t0:t0 + n], [("u", c) for c in range(8)])

    stopped = False
    try:
        chk(0)
        layers()
    except _Stop:
        stopped = True
    P.epoch = L

    def y_dst(c, t0, n):
        return None
    for ti, (t0, n) in enumerate([] if stopped else TT):
        xin, xik = xin2[ti % 2], ("xin", ti % 2)
        dma(xin[:, :, 0:n], xcur[:, :, t0:t0 + n], [(xk, ti)], [xik], "xin%d" % (ti % 2))
        ps, pk = PS()
        for c in range(8):
            act(sqb[:, c % 2, 0:n], xin[:, c, 0:n], AF.Square, [xik], [("sqb", c % 2)])
            mm(ps[:, 0:n], onesb[:, :], sqb[:, c % 2, 0:n], c == 0, c == 7, [("sqb", c % 2)], [pk])
        rs, rk = T5()
        act(rs[:, 0:n], ps[:, 0:n], AF.Sqrt, [pk], [rk], bias=EPS, scale=1.0 / D)
        P.add("dve", lambda e, a=rs[:, 0:n]: e.reciprocal(a, a), r=[rk], w=[rk])
        for c in range(8):
            stt(xin[:, c, 0:n], xin[:, c, 0:n], pcol[:, PC_FIN + c:PC_FIN + c + 1], rs[:, 0:n], ALU.mult, ALU.mult,
                [xik, rk, "pcol"], [xik])
        dma(yT[:, :, t0:t0 + n], xin[:, :, 0:n], [xik], [], "yout%d" % (ti % 2))

    P.finalize()
    sem_names = set()
    for op in P.ops:
        if op.dma is not None:
            sem_names.add(("d", op.dma))
        elif op.signal:
            sem_names.add(("c", op.eng, op.epoch))
    sems = {}
    for i, s in enumerate(sorted(sem_names, key=str)):
        sems[s] = nc.semaphore("s%d" % i).__enter__()
    by_eng = {"pe": [], "act": [], "dve": [], "pool": [], "sp": []}
    for op in P.ops:
        by_eng[op.eng].append(op)
    final_waits = [(("d", k), v) for k, v in P.dma_cnt.items()]

    def run(e, ops, final=False):
        for op in ops:
            for s, v in op.waits:
                e.wait_ge(sems[s], v)
            ins = op.fn(e)
            if op.dma is not None:
                ins.then_inc(sems[("d", op.dma)], 16)
            elif op.signal:
                ins.then_inc(sems[("c", op.eng, op.epoch)], 1)
        if final:
            for s, v in final_waits:
                e.wait_ge(sems[s], v)

    with nc.allow_non_contiguous_dma(reason="small strided state rows"), nc.Block() as block:
        @block.sync
        def _(e):
            run(e, by_eng["sp"], final=True)

        @block.tensor
        def _(e):
            run(e, by_eng["pe"])

        @block.scalar
        def _(e):
            run(e, by_eng["act"])

        @block.vector
        def _(e):
            run(e, by_eng["dve"])

        @block.gpsimd
        def _(e):
            run(e, by_eng["pool"])
    return nc, len(P.ops)


def _consts():
    c = np.zeros((128, 1540), np.float32)
    c[:, 0:128] = np.eye(128)
    c[:, 128:256] = 1.0
    s = np.arange(128)
    c[:, 256:384] = np.where(s[None, :] >= s[:, None], 0.0, -30000.0)
    s6 = np.arange(64)
    same = (s6[:, None] // 4) == (s6[None, :] // 4)
    c[0:64, 384:448] = np.where(same & (s6[None, :] >= s6[:, None]), 0.0, -30000.0)
    c[0:64, 448:464] = (s6[:, None] // 4 == np.arange(16)[None, :]).astype(np.float32)
    c[0:4, 464:468] = np.eye(4)
    for h in range(4):
        c[h, 512 + h * 128:512 + (h + 1) * 128] = -1.0
        c[h, 1024 + h * 128:1024 + (h + 1) * 128] = 1.0
    mr = (np.arange(16)[:, None] == (s6[None, :] // 4)).astype(np.float32).reshape(-1)
    c[:, 1536] = 1.0
    c[:, 1537] = 0.0
    c[:, 1538] = -1.0
    return c, np.ascontiguousarray(np.broadcast_to(mr[None, :], (128, 1024))).astype(np.float32)


def _col(v):
    return np.ascontiguousarray(v.reshape(-1, 128).T)


_CACHE = {}


def _prep(inp):
    f = lambda k: np.asarray(inp[k], dtype=np.float32)
    cst, mrow = _consts()
    pcol = np.zeros((128, L * NPC), np.float32)
    for l in range(L):
        b = l * NPC
        pcol[:, b + PC_NMIX:b + PC_NMIX + 8] = _col(f("norm_mix_g")[l])
        pcol[:, b + PC_NX:b + PC_NX + 8] = _col(f("norm_x_g")[l])
        pcol[:, b + PC_NFFN:b + PC_NFFN + 8] = _col(f("norm_ffn_g")[l])
        pcol[:, b + PC_NMEM:b + PC_NMEM + 8] = _col(f("norm_mem_g")[l])
        pcol[:, b + PC_FIN:b + PC_FIN + 8] = _col(f("final_norm_g"))
        caw = f("conv_a_w")[l]
        cbw = f("conv_b_w")[l]
        for j in range(4):
            pcol[:, b + PC_CAW + j * 3:b + PC_CAW + (j + 1) * 3] = caw[:, j * 128:(j + 1) * 128].T
            pcol[:, b + PC_CBW + j * 31:b + PC_CBW + (j + 1) * 31] = cbw[:, j * 128:(j + 1) * 128].T
        pcol[:, b + PC_CBB:b + PC_CBB + 4] = _col(f("conv_b_b")[l])
        pcol[:, b + PC_LNG:b + PC_LNG + 4] = _col(f("ln_b_g")[l])
        pcol[:, b + PC_LNB:b + PC_LNB + 4] = _col(f("ln_b_b")[l])
    pcol[:, PC_FIN:PC_FIN + 8] = _col(f("final_norm_g"))
    gnorm = np.ascontiguousarray(np.broadcast_to(f("mlstm_norm_g")[:, None, :], (L, 128, D)))
    bif = np.ascontiguousarray(f("b_if").reshape(L, 2, 4).transpose(2, 0, 1).reshape(4, L * 2))
    xp, xs = f("x_prompt"), f("x_sample")
    shared = {k: f(k) for k in ("w_in", "w_out_a", "w_out_b", "w_out_c", "w_o", "w_xq", "w_xkv", "w_xo", "w_ffn_in", "w_ffn_out")}
    in_maps = []
    for i in range(8):
        bs = slice(i * NB, (i + 1) * NB)
        xt = np.concatenate([xp[i], xs[bs].reshape(ST, D)], axis=0)
        xT = np.ascontiguousarray(xt.T.reshape(8, 128, T).transpose(1, 0, 2))
        memtok = np.ascontiguousarray(f("mem_prompt")[i].reshape(2, 128, D).transpose(1, 0, 2))
        sca = f("state_conv_a")[:, bs]
        sca = np.ascontiguousarray(sca.reshape(L, NB, 2, 4, 128).transpose(4, 0, 3, 1, 2).reshape(128, -1))
        scb = f("state_conv_b")[:, bs]
        scb = np.ascontiguousarray(scb.reshape(L, NB, 30, 4, 128).transpose(4, 0, 3, 1, 2).reshape(128, -1))
        cn = np.ascontiguousarray(f("state_mlstm_c")[:, bs])
        ctr = np.ascontiguousarray(cn.transpose(0, 1, 2, 4, 3))
        nn = f("state_mlstm_n")[:, bs]
        nTi = np.ascontiguousarray(nn.reshape(L, NB, 4, 2, 128).transpose(4, 0, 3, 1, 2).reshape(128, -1))
        mi = np.ascontiguousarray(f("state_mlstm_m")[:, bs].transpose(2, 0, 1).reshape(4, -1))
        kTc = np.ascontiguousarray(f("cache_mem_k")[:, bs].transpose(0, 1, 3, 4, 2))
        vc = np.ascontiguousarray(f("cache_mem_v")[:, bs].reshape(L, NB, 256, D))
        m = {"xT_in": xT, "memtok": memtok, "pcol": pcol, "gnorm": gnorm, "bif": bif, "sca": sca, "scb": scb,
             "cnat": cn, "ctr": ctr, "nTin": nTi, "min": mi, "kTc": kTc, "vc": vc, "cst": cst, "mrow": mrow}
        m.update(shared)
        in_maps.append(m)
    return in_maps


def _post(R):
    y_p = np.zeros((8, PT, D), np.float32)
    y_s = np.zeros((128, 4, D), np.float32)
    p_a = np.zeros((L, 8, 2, 512), np.float32)
    p_b = np.zeros((L, 8, 30, 512), np.float32)
    p_c = np.zeros((L, 8, 4, 256, 256), np.float32)
    p_n = np.zeros((L, 8, 4, 256), np.float32)
    p_m = np.zeros((L, 8, 4), np.float32)
    p_k = np.zeros((L, 8, 256, 4, 256), np.float32)
    p_v = np.zeros((L, 8, 256, 4, 256), np.float32)
    s_a = np.zeros((L, 128, 2, 512), np.float32)
    s_b = np.zeros((L, 128, 30, 512), np.float32)
    s_c = np.zeros((L, 128, 4, 256, 256), np.float32)
    s_n = np.zeros((L, 128, 4, 256), np.float32)
    s_m = np.zeros((L, 128, 4), np.float32)
    for i in range(8):
        r = R[i]
        bs = slice(i * NB, (i + 1) * NB)
        yt = r["yT"].transpose(2, 1, 0).reshape(T, D)
        y_p[i] = yt[:PT]
        y_s[bs] = yt[PT:].reshape(NB, 4, D)
        a = r["oca"].reshape(128, L, 4, 17, 2).transpose(1, 3, 4, 2, 0).reshape(L, 17, 2, 512)
        p_a[:, i] = a[:, 0]
        s_a[:, bs] = a[:, 1:]
        b_ = r["ocb"].reshape(128, L, 4, 17, 30).transpose(1, 3, 4, 2, 0).reshape(L, 17, 30, 512)
        p_b[:, i] = b_[:, 0]
        s_b[:, bs] = b_[:, 1:]
        p_c[:, i] = r["oC"][:, 0]
        s_c[:, bs] = r["oC"][:, 1:]
        n_ = r["on"].reshape(128, L, 2, 68).transpose(1, 3, 2, 0).reshape(L, 68, 256)
        p_n[:, i] = n_[:, 0:4]
        s_n[:, bs] = n_[:, 4:].reshape(L, NB, 4, 256)
        m_ = r["om"].reshape(4, L, 17).transpose(1, 2, 0)
        p_m[:, i] = m_[:, 0]
        s_m[:, bs] = m_[:, 1:]
        p_k[:, i] = r["omk"].reshape(L, 256, 4, 256)
        p_v[:, i] = r["omv"].reshape(L, 256, 4, 256)
    return (y_p, y_s, p_a, p_b, p_c, p_n, p_m, p_k, p_v, s_a, s_b, s_c, s_n, s_m)


def kernel(**inp):
    if "nc" not in _CACHE:
        _CACHE["nc"] = build_nc()
    nc, nops = _CACHE["nc"]
    in_maps = _prep(inp)
    res = run_bass_kernel_spmd(nc, in_maps, core_ids=list(range(8)))
    return _post(res.results)
```
